# Optimizing a Trainium2 kernel written in Bass

```python
import math
import jax, jax.numpy as jnp
from jax import lax
import numpy as np

D_MODEL = 2048
BATCH = 2
SEQ = 16384
DEPTH = 2

HEAD_DIM = 128
N_HEADS = D_MODEL // HEAD_DIM
N_HEADS_A = N_HEADS // 2
N_HEADS_B = N_HEADS - N_HEADS_A
WIDTH_A = N_HEADS_A * HEAD_DIM
WIDTH_B = N_HEADS_B * HEAD_DIM
DILATED_BRANCHES = ((128, 1), (512, 4), (2048, 16))
N_IDX_HEADS = 16
IDX_DIM = 64
TOPK_MAX = 256
Q_BLOCK = 128
D_FF = 5632
N_BUCKETS = 32
REL_MAX_DIST = 2048
EPS = 1e-6
PROJ_SIZES = (WIDTH_A, WIDTH_A, WIDTH_A, WIDTH_B, WIDTH_B, WIDTH_B,
              N_IDX_HEADS * IDX_DIM, IDX_DIM, N_IDX_HEADS)
PROJ_WIDTH = sum(PROJ_SIZES)
SPLIT_POINTS = tuple(int(c) for c in np.cumsum(PROJ_SIZES)[:-1])

kernel_name = "hybrid_dilated_dsa_macaron"


def rms_norm(x, g):
    xf = x.astype(jnp.float32)
    y = xf * lax.rsqrt(jnp.mean(xf * xf, axis=-1, keepdims=True) + EPS)
    return (y * g.astype(jnp.float32)).astype(x.dtype)


def swiglu(h, w_in, w_out):
    gate, up = jnp.split(h @ w_in, 2, axis=-1)
    return (jax.nn.silu(gate) * up) @ w_out


def rel_bucket(dist):
    exact = N_BUCKETS // 2
    df = jnp.maximum(dist, 1).astype(jnp.float32)
    large = exact + (jnp.log(df / exact) / math.log(REL_MAX_DIST / exact)
                     * (N_BUCKETS - exact)).astype(jnp.int32)
    large = jnp.minimum(large, N_BUCKETS - 1)
    return jnp.where(dist < exact, dist, large)


def dilated_branch(q, k, v, bias_tab, window, dil):
    B, S, H, E = q.shape
    blk = window // dil
    span = blk * dil
    s_pad = -(-S // span) * span
    nb = s_pad // span

    def to_blocks(t):
        t = jnp.pad(t, ((0, 0), (0, s_pad - S), (0, 0), (0, 0)))
        return t.reshape(B, nb, blk, dil, H, E)

    def with_prev(t):
        prev = jnp.pad(t, ((0, 0), (1, 0), (0, 0), (0, 0), (0, 0), (0, 0)))[:, :-1]
        return jnp.concatenate([prev, t], axis=2)

    qb = to_blocks(q)
    kw = with_prev(to_blocks(k))
    vw = with_prev(to_blocks(v))
    i = jnp.arange(blk)[:, None]
    j = jnp.arange(2 * blk)[None, :]
    step = blk + i - j
    band = (step >= 0) & (step <= blk)
    first = j >= blk
    nblock = jnp.arange(nb)[:, None, None, None, None]
    valid = band[None, None, None] & ((nblock > 0) | first[None, None, None])
    bias = bias_tab[rel_bucket(jnp.clip(step, 0, blk) * dil)]
    bias = jnp.transpose(bias, (2, 0, 1)).astype(jnp.float32)
    logits = jnp.einsum('bnidhe,bnjdhe->bndhij', qb, kw).astype(jnp.float32) + bias
    logits = jnp.where(valid, logits, -jnp.inf)
    lse = jax.nn.logsumexp(logits, axis=-1)
    p = jnp.exp(logits - lse[..., None])
    o = jnp.einsum('bndhij,bnjdhe->bnidhe', p.astype(v.dtype), vw)
    o = o.reshape(B, s_pad, H, E)[:, :S]
    lse = jnp.transpose(lse, (0, 1, 4, 2, 3)).reshape(B, s_pad, H)[:, :S]
    return o, lse


def dilated_mixer(q, k, v, bias_tab):
    outs, lses = [], []
    for window, dil in DILATED_BRANCHES:
        o, l = dilated_branch(q, k, v, bias_tab, window, dil)
        outs.append(o)
        lses.append(l)
    wts = jax.nn.softmax(jnp.stack(lses), axis=0)
    out = jnp.einsum('nbsh,nbshe->bshe', wts, jnp.stack(outs).astype(jnp.float32))
    return out.astype(q.dtype)


def dsa_mixer(q, k, v, q_idx, k_idx, w_idx, bias_tab):
    B, S, H, E = q.shape
    n_sel = min(TOPK_MAX, S // 4)
    nqb = S // Q_BLOCK
    key_pos = jnp.arange(S)
    idx_scale = (IDX_DIM ** -0.5) * (N_IDX_HEADS ** -0.5)

    def blocks(t):
        return jnp.moveaxis(t.reshape((B, nqb, Q_BLOCK) + t.shape[2:]), 1, 0)

    def one_block(args):
        qb, qib, wb, start = args
        t_pos = start + jnp.arange(Q_BLOCK)
        rel = jax.nn.relu(jnp.einsum('bthe,bse->bths', qib, k_idx).astype(jnp.float32))
        score = jnp.einsum('bths,bth->bts', rel, wb.astype(jnp.float32)) * idx_scale
        causal = key_pos[None, :] <= t_pos[:, None]
        score = jnp.where(causal, score, -jnp.inf)
        _, sel = lax.top_k(score, n_sel)
        valid = sel <= t_pos[None, :, None]
        kg = jax.vmap(lambda kk, ii: kk[ii])(k, sel)
        vg = jax.vmap(lambda vv, ii: vv[ii])(v, sel)
        logits = jnp.einsum('bthe,btkhe->bhtk', qb, kg).astype(jnp.float32)
        dist = jnp.maximum(t_pos[None, :, None] - sel, 0)
        bias = bias_tab[rel_bucket(dist)]
        logits = logits + jnp.transpose(bias, (0, 3, 1, 2)).astype(jnp.float32)
        logits = jnp.where(valid[:, None], logits, -jnp.inf)
        p = jax.nn.softmax(logits, axis=-1)
        return jnp.einsum('bhtk,btkhe->bthe', p.astype(v.dtype), vg)

    starts = jnp.arange(nqb, dtype=jnp.int32) * Q_BLOCK
    out = lax.map(one_block, (blocks(q), blocks(q_idx), blocks(w_idx), starts))
    return jnp.moveaxis(out, 0, 1).reshape(B, S, H, E)


def setup_inputs(seed: int = 0) -> dict:
    key = jax.random.key(seed)
    ks = jax.random.split(key, 16)
    nrm = lambda k, shape, fan_in: jax.random.normal(k, shape, jnp.float32) * (fan_in ** -0.5)
    gain = lambda k, shape: 1.0 + 0.05 * jax.random.normal(k, shape, jnp.float32)
    return {
        'x': jax.random.normal(ks[0], (BATCH, SEQ, D_MODEL), jnp.float32),
        'rel_bias': 0.2 * jax.random.normal(ks[1], (N_BUCKETS, N_HEADS), jnp.float32),
        'norm_ffn1': gain(ks[2], (DEPTH, D_MODEL)),
        'w_ffn1_in': nrm(ks[3], (DEPTH, D_MODEL, 2 * D_FF), D_MODEL),
        'w_ffn1_out': nrm(ks[4], (DEPTH, D_FF, D_MODEL), D_FF),
        'norm_mix': gain(ks[5], (DEPTH, D_MODEL)),
        'w_in': nrm(ks[6], (DEPTH, D_MODEL, PROJ_WIDTH), D_MODEL),
        'q_norm_a': gain(ks[7], (DEPTH, HEAD_DIM)),
        'k_norm_a': gain(ks[8], (DEPTH, HEAD_DIM)),
        'q_norm_b': gain(ks[9], (DEPTH, HEAD_DIM)),
        'k_norm_b': gain(ks[10], (DEPTH, HEAD_DIM)),
        'w_out': nrm(ks[11], (DEPTH, WIDTH_A + WIDTH_B, D_MODEL), WIDTH_A + WIDTH_B),
        'norm_ffn2': gain(ks[12], (DEPTH, D_MODEL)),
        'w_ffn2_in': nrm(ks[13], (DEPTH, D_MODEL, 2 * D_FF), D_MODEL),
        'w_ffn2_out': nrm(ks[14], (DEPTH, D_FF, D_MODEL), D_FF),
    }


def reference(x, rel_bias, norm_ffn1, w_ffn1_in, w_ffn1_out, norm_mix, w_in,
              q_norm_a, k_norm_a, q_norm_b, k_norm_b, w_out,
              norm_ffn2, w_ffn2_in, w_ffn2_out):
    B, S, _ = x.shape
    bias_a = rel_bias[:, :N_HEADS_A]
    bias_b = rel_bias[:, N_HEADS_A:]
    scale = HEAD_DIM ** -0.5
    heads = lambda t: t.reshape(B, S, -1, HEAD_DIM)
    for l in range(DEPTH):
        x = x + 0.5 * swiglu(rms_norm(x, norm_ffn1[l]), w_ffn1_in[l], w_ffn1_out[l])
        h = rms_norm(x, norm_mix[l])
        proj = h @ w_in[l]
        qa, ka, va, qb, kb, vb, qi, ki, wi = jnp.split(proj, SPLIT_POINTS, axis=-1)
        qa = rms_norm(heads(qa), q_norm_a[l]) * scale
        ka = rms_norm(heads(ka), k_norm_a[l])
        qb = rms_norm(heads(qb), q_norm_b[l]) * scale
        kb = rms_norm(heads(kb), k_norm_b[l])
        mix_a = dilated_mixer(qa, ka, heads(va), bias_a)
        mix_b = dsa_mixer(qb, kb, heads(vb), qi.reshape(B, S, N_IDX_HEADS, IDX_DIM),
                          ki, wi, bias_b)
        mixed = jnp.concatenate([mix_a.reshape(B, S, WIDTH_A), mix_b.reshape(B, S, WIDTH_B)], axis=-1)
        x = x + mixed @ w_out[l]
        x = x + 0.5 * swiglu(rms_norm(x, norm_ffn2[l]), w_ffn2_in[l], w_ffn2_out[l])
    return x
```

```python
import numpy as np
from contextlib import ExitStack
import ml_dtypes
import concourse.bass as bass
import concourse.mybir as mybir
from concourse.bass_utils import run_bass_kernel_spmd

F32 = mybir.dt.float32
BF16 = mybir.dt.bfloat16
ALU = mybir.AluOpType
AF = mybir.ActivationFunctionType
NPBF = ml_dtypes.bfloat16
EPS = 1e-6


class Cfg:
    def __init__(s, D=2048, F=5632, HA=8, HB=8, NIH=16, B=2, S=16384, CPB=4, DEPTH=2):
        s.D, s.F, s.HA, s.HB, s.NIH, s.B, s.S, s.CPB, s.DEPTH = D, F, HA, HB, NIH, B, S, CPB, DEPTH
        s.DC, s.FC = D // 128, F // 128
        s.NCORES = B * CPB
        s.UNIT = 2048
        s.NU = S // s.UNIT
        assert s.NU == 2 * CPB
        s.T = 2 * s.UNIT
        s.G = 1024
        s.WA, s.WB, s.QI = HA * 128, HB * 128, NIH * 64
        s.PW = 3 * s.WA + 3 * s.WB + s.QI + 64 + NIH
        s.NCC = (s.PW + 127) // 128
        s.o_qa, s.o_ka, s.o_va = 0, s.WA, 2 * s.WA
        s.o_qb, s.o_kb, s.o_vb = 3 * s.WA, 3 * s.WA + s.WB, 3 * s.WA + 2 * s.WB
        s.o_qi = 3 * s.WA + 3 * s.WB
        s.o_ki = s.o_qi + s.QI
        s.o_wi = s.o_ki + 64
        assert s.o_ki % 128 == 0
        s.NQI = s.QI // 128


class Buf:
    __slots__ = ("lw", "rd")

    def __init__(self):
        self.lw = None
        self.rd = []


class EngS:
    def __init__(self, name):
        self.name = name
        self.ops = []
        self.count = 0
        self.waited = {}


class Prog:
    NDMASEM = 12

    def __init__(self, nc, es):
        self.nc = nc
        self.E = {n: EngS(n) for n in ("pe", "act", "dve", "pool", "sp")}
        self.sems = {}
        for n in self.E:
            self.sems[("e", n)] = es.enter_context(nc.semaphore("s_" + n))
        self.dq = {}
        for q in ("sp", "pool"):
            lst = []
            for i in range(self.NDMASEM):
                k = ("d", q, i)
                self.sems[k] = es.enter_context(nc.semaphore("d_%s_%d" % (q, i)))
                lst.append(k)
            self.dq[q] = {"keys": lst, "n": 0, "vals": [0] * self.NDMASEM}

    def _deps(self, reads, writes, extra=()):
        deps = list(extra)
        for b in reads:
            if b.lw is not None:
                deps.append(b.lw)
        for b in writes:
            if b.lw is not None:
                deps.append(b.lw)
            deps.extend(b.rd)
        return deps

    def _prune(self, e, deps):
        best = {}
        for (k, v) in deps:
            if e.name == "pe" and k == ("e", "pe"):
                continue
            if e.waited.get(k, 0) >= v:
                continue
            if best.get(k, 0) < v:
                best[k] = v
        for k, v in best.items():
            e.waited[k] = v
        return list(best.items())

    def _commit(self, tok, reads, writes):
        for b in reads:
            b.rd.append(tok)
        for b in writes:
            b.lw = tok
            b.rd = []

    def op(self, eng, fn, reads=(), writes=()):
        e = self.E[eng]
        waits = self._prune(e, self._deps(reads, writes))
        e.count += 1
        tok = (("e", eng), e.count)
        e.ops.append((waits, fn, (("e", eng), 1)))
        self._commit(tok, reads, writes)
        return tok

    def dma(self, q, out, in_, reads=(), writes=()):
        e = self.E[q]
        d = self.dq[q]
        i = d["n"] % self.NDMASEM
        d["n"] += 1
        key = d["keys"][i]
        prev = d["vals"][i]
        extra = [(key, prev)] if prev > 0 else []
        waits = self._prune(e, self._deps(reads, writes, extra))
        d["vals"][i] = prev + 16
        tok = (key, prev + 16)
        e.ops.append((waits, (lambda t, out=out, in_=in_: t.dma_start(out=out, in_=in_)), (key, 16)))
        self._commit(tok, reads, writes)
        return tok

    def all_tokens(self):
        toks = []
        for n, e in self.E.items():
            if e.count:
                toks.append((("e", n), e.count))
        for q, d in self.dq.items():
            for k, v in zip(d["keys"], d["vals"]):
                if v:
                    toks.append((k, v))
        return toks

    def barrier(self):
        toks = self.all_tokens()
        for n, e in self.E.items():
            waits = self._prune(e, toks)
            if waits:
                e.ops.append((waits, None, None))

    def finish(self):
        self.barrier()
        nc, sems, E = self.nc, self.sems, self.E

        def replay(engname, engobj):
            for (waits, fn, inc) in E[engname].ops:
                for (k, v) in waits:
                    engobj.wait_ge(sems[k], v)
                if fn is not None:
                    fn(engobj).then_inc(sems[inc[0]], inc[1])

        with nc.Block() as block:
            @block.tensor
            def _(t):
                replay("pe", t)

            @block.scalar
            def _(t):
                replay("act", t)

            @block.vector
            def _(t):
                replay("dve", t)

            @block.gpsimd
            def _(t):
                replay("pool", t)

            @block.sync
            def _(t):
                replay("sp", t)


class Ctx:
    def __init__(self, cfg):
        self.cfg = cfg
        nc = bass.Bass("TRN2", target_bir_lowering=False)
        nc.dge_precook = False
        self.nc = nc
        self.es = ExitStack()
        self.P = Prog(nc, self.es)
        self.banks = [self.es.enter_context(nc.psum_tensor("bank%d" % i, [128, 512], F32)) for i in range(8)]
        self.bankb = [Buf() for _ in range(8)]
        self.ones = self.es.enter_context(nc.sbuf_tensor("ones", [128, 128], BF16))
        self.onesb = Buf()
        self.P.op("dve", lambda t: t.memset(self.ones[:], 1.0), writes=[self.onesb])
        self.xbufs = {}
        self.n_in = {}

    def din(self, name, shape, dt):
        return self.nc.dram_tensor(name, list(shape), dt, kind="ExternalInput").ap()

    def dout(self, name, shape, dt):
        return self.nc.dram_tensor(name, list(shape), dt, kind="ExternalOutput").ap()

    def dint(self, name, shape, dt):
        return self.nc.dram_tensor(name, list(shape), dt, kind="Internal").ap()

    def sb(self, es, name, shape, dt):
        return es.enter_context(self.nc.sbuf_tensor(name, list(shape), dt))

    def xb(self, name, dc, tt):
        k = (name, dc, tt)
        if k not in self.xbufs:
            self.xbufs[k] = Buf()
        return self.xbufs[k]


def alloc_dense(cx, es):
    cfg = cx.cfg
    W = {}
    NCH = max(cfg.FC, cfg.DC)
    W["hT"] = cx.sb(es, "hT", [128, cfg.DC, cfg.G], BF16)
    W["hTb"] = [Buf() for _ in range(cfg.G // 256)]
    W["actT"] = cx.sb(es, "actT", [128, NCH, cfg.G], BF16)
    W["actTb"] = [Buf() for _ in range(NCH)]
    W["w1"] = [cx.sb(es, "w1_%d" % i, [128, cfg.DC * 256], BF16) for i in range(2)]
    W["w1b"] = [Buf(), Buf()]
    wbsz = max(NCH * 128, 4 * cfg.DC * 128)
    W["wb"] = [cx.sb(es, "wb_%d" % i, [128, wbsz], BF16) for i in range(2)]
    W["wbb"] = [Buf(), Buf()]
    W["xn"] = cx.sb(es, "xn", [128, cfg.DC, 256], F32)
    W["xnb"] = Buf()
    W["sq"] = [cx.sb(es, "sq%d" % i, [128, 512], BF16) for i in range(2)]
    W["sqb"] = [Buf(), Buf()]
    W["rstd"] = cx.sb(es, "rstd", [128, 512], F32)
    W["rstdb"] = Buf()
    W["sg"] = [cx.sb(es, "sg%d" % i, [128, 512], F32) for i in range(2)]
    W["sgb"] = [Buf(), Buf()]
    W["xe"] = [cx.sb(es, "xe%d" % i, [128, 512], F32) for i in range(2)]
    W["xeb"] = [Buf(), Buf()]
    W["xo"] = [cx.sb(es, "xo%d" % i, [128, 512], F32) for i in range(2)]
    W["xob"] = [Buf(), Buf()]
    W["ot"] = [cx.sb(es, "ot%d" % i, [128, 512], BF16) for i in range(2)]
    W["otb"] = [Buf(), Buf()]
    W["wis"] = [cx.sb(es, "wis%d" % i, [128, 16], F32) for i in range(2)]
    W["wisb"] = [Buf(), Buf()]
    W["cnt"] = {"g": 0, "y": 0, "w1": 0, "wb": 0, "sq": 0, "sg": 0, "xe": 0, "ot": 0, "wis": 0}
    return W


def load_vec(cx, es, name, dram_ap, ncol):
    t = cx.sb(es, name, [128, ncol], F32)
    b = Buf()
    cx.P.dma("sp", t[:], dram_ap, writes=[b])
    return t, b


def rstd_from_ssq(cx, W, ssq_ap, ssq_buf, n, width):
    P = cx.P
    rs = W["rstd"]
    P.op("act", lambda t: t.activation(out=rs[:, :width], in_=ssq_ap, func=AF.Sqrt, scale=1.0 / n, bias=cx.epsb[:, 0:1]),
         reads=[ssq_buf, cx.epsbb], writes=[W["rstdb"]])
    P.op("dve", lambda t: t.reciprocal(out=rs[:, :width], in_=rs[:, :width]), reads=[W["rstdb"]], writes=[W["rstdb"]])


def norm_phase(cx, W, xname, xap, gain, gainb, tok0):
    cfg, P = cx.cfg, cx.P
    xv = xap.rearrange("(c p) t -> p c t", p=128)
    for q in range(cfg.G // 256):
        t0 = tok0 + q * 256
        tt = t0 // 512
        xn = W["xn"]
        P.dma("sp", xn[:], xv[:, :, t0:t0 + 256], reads=[cx.xb(xname, dc, tt) for dc in range(cfg.DC)], writes=[W["xnb"]])
        ssq = cx.banks[6]
        for c in range(cfg.DC):
            i = W["cnt"]["sq"] % 2
            W["cnt"]["sq"] += 1
            sq = W["sq"][i]
            P.op("act", lambda t, sq=sq, c=c: t.activation(out=sq[:, :256], in_=xn[:, c, :], func=AF.Square),
                 reads=[W["xnb"]], writes=[W["sqb"][i]])
            P.op("pe", lambda t, sq=sq, c=c: t.matmul(ssq[:, :256], lhsT=cx.ones[:], rhs=sq[:, :256], start=(c == 0), stop=(c == cfg.DC - 1)),
                 reads=[W["sqb"][i], cx.onesb], writes=[cx.bankb[6]])
        rstd_from_ssq(cx, W, ssq[:, :256], cx.bankb[6], cfg.D, 256)
        for c in range(cfg.DC):
            P.op("dve", lambda t, c=c, q=q: t.scalar_tensor_tensor(out=W["hT"][:, c, q * 256:(q + 1) * 256], in0=xn[:, c, :], scalar=gain[:, c:c + 1],
                                                                 in1=W["rstd"][:, :256], op0=ALU.mult, op1=ALU.mult),
                 reads=[W["xnb"], W["rstdb"], gainb], writes=[W["hTb"][q]])


def inproj_phase(cx, W, w1t):
    cfg, P = cx.cfg, cx.P
    ntt = cfg.G // 512
    for f in range(cfg.FC):
        wi = W["cnt"]["w1"] % 2
        W["cnt"]["w1"] += 1
        w1 = W["w1"][wi]
        P.dma("pool", w1[:], w1t[f], writes=[W["w1b"][wi]])
        for tt in range(ntt):
            gi = W["cnt"]["g"] % 2
            W["cnt"]["g"] += 1
            pg, pu = cx.banks[gi], cx.banks[2 + gi]
            hb = [W["hTb"][2 * tt], W["hTb"][2 * tt + 1]]
            for gu, pb, bi in ((0, pg, gi), (1, pu, 2 + gi)):
                for kc in range(cfg.DC):
                    P.op("pe", lambda t, pb=pb, w1=w1, kc=kc, gu=gu, tt=tt: t.matmul(
                        pb[:], lhsT=w1[:, kc * 256 + gu * 128: kc * 256 + gu * 128 + 128], rhs=W["hT"][:, kc, tt * 512:(tt + 1) * 512],
                        start=(kc == 0), stop=(kc == cfg.DC - 1)), reads=[W["w1b"][wi]] + hb, writes=[cx.bankb[bi]])
            si = W["cnt"]["sg"] % 2
            W["cnt"]["sg"] += 1
            sg = W["sg"][si]
            P.op("act", lambda t, sg=sg, pg=pg: t.activation(out=sg[:], in_=pg[:], func=AF.Silu), reads=[cx.bankb[gi]], writes=[W["sgb"][si]])
            P.op("dve", lambda t, sg=sg, pu=pu, f=f, tt=tt: t.tensor_tensor(out=W["actT"][:, f, tt * 512:(tt + 1) * 512], in0=sg[:], in1=pu[:], op=ALU.mult),
                 reads=[W["sgb"][si], cx.bankb[2 + gi]], writes=[W["actTb"][f]])


def linres_phase(cx, W, w2t, nch, xsname, xs, xdname, xd, tok0, scale):
    cfg, P = cx.cfg, cx.P
    ntt = cfg.G // 512
    for dc in range(cfg.DC):
        wi = W["cnt"]["wb"] % 2
        W["cnt"]["wb"] += 1
        wb = W["wb"][wi]
        P.dma("pool", wb[:, :nch * 128], w2t[dc], writes=[W["wbb"][wi]])
        for tt in range(ntt):
            yi = W["cnt"]["y"] % 2
            W["cnt"]["y"] += 1
            py = cx.banks[4 + yi]
            gt = (tok0 + tt * 512) // 512
            ei = W["cnt"]["xe"] % 2
            W["cnt"]["xe"] += 1
            xe, xo = W["xe"][ei], W["xo"][ei]
            P.dma("sp", xe[:], xs[dc * 128:(dc + 1) * 128, tok0 + tt * 512: tok0 + (tt + 1) * 512],
                  reads=[cx.xb(xsname, dc, gt)], writes=[W["xeb"][ei]])
            for f in range(nch):
                P.op("pe", lambda t, py=py, wb=wb, f=f, tt=tt: t.matmul(py[:], lhsT=wb[:, f * 128:(f + 1) * 128], rhs=W["actT"][:, f, tt * 512:(tt + 1) * 512],
                                                                      start=(f == 0), stop=(f == nch - 1)),
                     reads=[W["wbb"][wi], W["actTb"][f]], writes=[cx.bankb[4 + yi]])
            P.op("dve", lambda t, py=py, xe=xe, xo=xo: t.scalar_tensor_tensor(out=xo[:], in0=py[:], scalar=float(scale), in1=xe[:], op0=ALU.mult, op1=ALU.add),
                 reads=[cx.bankb[4 + yi], W["xeb"][ei]], writes=[W["xob"][ei]])
            P.dma("sp", xd[dc * 128:(dc + 1) * 128, tok0 + tt * 512: tok0 + (tt + 1) * 512], xo[:],
                  reads=[W["xob"][ei]], writes=[cx.xb(xdname, dc, gt)])


def load_actT(cx, W, src, nch, tok0):
    cfg, P = cx.cfg, cx.P
    sv = src.rearrange("(c p) t -> p c t", p=128)
    for c in range(nch):
        P.dma("sp", W["actT"][:, c, :], sv[:, c, tok0:tok0 + cfg.G], writes=[W["actTb"][c]])


def proj_phase(cx, W, wpt, gains, outs, tok0):
    cfg, P = cx.cfg, cx.P
    ntt = cfg.G // 512
    nsub = cfg.G // 128
    allh = W["hTb"]
    kinds = {}
    for h in range(cfg.HA):
        kinds[cfg.o_qa // 128 + h] = ("n", "qa", h, 0)
        kinds[cfg.o_ka // 128 + h] = ("n", "ka", h, 1)
    for h in range(cfg.HB):
        kinds[cfg.o_qb // 128 + h] = ("n", "qb", h, 2)
        kinds[cfg.o_kb // 128 + h] = ("n", "kb", h, 3)
    for j in range(cfg.NQI):
        kinds[cfg.o_qi // 128 + j] = ("p", "qi", j, 128)
    kinds[cfg.o_ki // 128] = ("p", "ki", 0, 64)
    for v0, nm, wdt in ((cfg.o_va // 128, "va", cfg.WA), (cfg.o_vb // 128, "vb", cfg.WB)):
        for j in range(wdt // 128):
            kinds[v0 + j] = ("v", nm, j, 0)
    gain_t, gain_b = gains
    for cc0 in range(0, cfg.NCC, 4):
        ncl = min(4, cfg.NCC - cc0)
        wi = W["cnt"]["wb"] % 2
        W["cnt"]["wb"] += 1
        wb = W["wb"][wi]
        wbv = wb[:, :4 * cfg.DC * 128].rearrange("p (j k c) -> p j k c", j=4, k=cfg.DC)
        P.dma("pool", wbv[:, :ncl], wpt[cc0:cc0 + ncl].rearrange("j p (k c) -> p j k c", k=cfg.DC), writes=[W["wbb"][wi]])
        j = 0
        while j < ncl:
            cc = cc0 + j
            kind = kinds[cc]
            if kind[0] == "v":
                j1 = j
                while j1 < ncl and kinds[cc0 + j1][0] == "v" and kinds[cc0 + j1][1] == kind[1]:
                    j1 += 1
                nv = j1 - j
                for sub in range(nsub):
                    gi = W["cnt"]["g"] % 2
                    W["cnt"]["g"] += 1
                    pb = cx.banks[gi]
                    for kc in range(cfg.DC):
                        P.op("pe", lambda t, pb=pb, kc=kc, sub=sub, j=j, nv=nv, wbv=wbv: t.matmul(
                            pb[:, :nv * 128], lhsT=W["hT"][:, kc, sub * 128:(sub + 1) * 128], rhs=wbv[:, j:j + nv, kc, :],
                            start=(kc == 0), stop=(kc == cfg.DC - 1)), reads=[W["wbb"][wi], allh[sub // 2]], writes=[cx.bankb[gi]])
                    oi = W["cnt"]["ot"] % 2
                    W["cnt"]["ot"] += 1
                    ot = W["ot"][oi]
                    P.op("act", lambda t, ot=ot, pb=pb, nv=nv: t.activation(out=ot[:, :nv * 128], in_=pb[:, :nv * 128], func=AF.Copy),
                         reads=[cx.bankb[gi]], writes=[W["otb"][oi]])
                    c0 = kind[2] * 128
                    P.dma("sp", outs[kind[1]][tok0 + sub * 128: tok0 + (sub + 1) * 128, c0:c0 + nv * 128], ot[:, :nv * 128], reads=[W["otb"][oi]])
                j = j1
                continue
            M = 128 if kind[0] == "n" else kind[3]
            for tt in range(ntt):
                gi = W["cnt"]["g"] % 2
                W["cnt"]["g"] += 1
                pb = cx.banks[gi]
                hb = [allh[2 * tt], allh[2 * tt + 1]]
                for kc in range(cfg.DC):
                    P.op("pe", lambda t, pb=pb, kc=kc, tt=tt, j=j, M=M, wbv=wbv: t.matmul(
                        pb[:M, :], lhsT=wbv[:, j, kc, :M], rhs=W["hT"][:, kc, tt * 512:(tt + 1) * 512],
                        start=(kc == 0), stop=(kc == cfg.DC - 1)), reads=[W["wbb"][wi]] + hb, writes=[cx.bankb[gi]])
                oi = W["cnt"]["ot"] % 2
                W["cnt"]["ot"] += 1
                ot = W["ot"][oi]
                tsl = slice(tok0 + tt * 512, tok0 + (tt + 1) * 512)
                if kind[0] == "n":
                    si = W["cnt"]["sq"] % 2
                    W["cnt"]["sq"] += 1
                    sq = W["sq"][si]
                    P.op("act", lambda t, sq=sq, pb=pb: t.activation(out=sq[:], in_=pb[:], func=AF.Square), reads=[cx.bankb[gi]], writes=[W["sqb"][si]])
                    ssq = cx.banks[6]
                    P.op("pe", lambda t, sq=sq: t.matmul(ssq[:], lhsT=cx.ones[:], rhs=sq[:], start=True, stop=True),
                         reads=[W["sqb"][si], cx.onesb], writes=[cx.bankb[6]])
                    rstd_from_ssq(cx, W, ssq[:], cx.bankb[6], 128, 512)
                    gcol = kind[3]
                    P.op("dve", lambda t, ot=ot, pb=pb, gcol=gcol: t.scalar_tensor_tensor(out=ot[:], in0=pb[:], scalar=gain_t[:, gcol:gcol + 1], in1=W["rstd"][:],
                                                                                        op0=ALU.mult, op1=ALU.mult),
                         reads=[cx.bankb[gi], W["rstdb"], gain_b], writes=[W["otb"][oi]])
                    P.dma("sp", outs[kind[1]][kind[2], :, tsl], ot[:], reads=[W["otb"][oi]])
                else:
                    P.op("act", lambda t, ot=ot, pb=pb, M=M: t.activation(out=ot[:M, :], in_=pb[:M, :], func=AF.Copy), reads=[cx.bankb[gi]], writes=[W["otb"][oi]])
                    if kind[1] == "qi":
                        P.dma("sp", outs["qi"][kind[2], :, tsl], ot[:], reads=[W["otb"][oi]])
                    else:
                        P.dma("sp", outs["ki"][:, tsl], ot[:64, :], reads=[W["otb"][oi]])
            if kind[1] == "ki":
                for sub in range(nsub):
                    gi = W["cnt"]["g"] % 2
                    W["cnt"]["g"] += 1
                    pb = cx.banks[gi]
                    for kc in range(cfg.DC):
                        P.op("pe", lambda t, pb=pb, kc=kc, sub=sub, j=j, wbv=wbv: t.matmul(
                            pb[:, :cfg.NIH], lhsT=W["hT"][:, kc, sub * 128:(sub + 1) * 128], rhs=wbv[:, j, kc, 64:64 + cfg.NIH],
                            start=(kc == 0), stop=(kc == cfg.DC - 1)), reads=[W["wbb"][wi], allh[sub // 2]], writes=[cx.bankb[gi]])
                    oi = W["cnt"]["wis"] % 2
                    W["cnt"]["wis"] += 1
                    ws = W["wis"][oi]
                    P.op("dve", lambda t, ws=ws, pb=pb: t.tensor_copy(out=ws[:, :cfg.NIH], in_=pb[:, :cfg.NIH]), reads=[cx.bankb[gi]], writes=[W["wisb"][oi]])
                    P.dma("sp", outs["wi"][tok0 + sub * 128: tok0 + (sub + 1) * 128, :], ws[:, :cfg.NIH], reads=[W["wisb"][oi]])
            j += 1


def common_consts(cx):
    es = cx.es
    cx.epsb = cx.sb(es, "epsb", [128, 1], F32)
    cx.epsbb = Buf()
    cx.P.op("dve", lambda t: t.memset(cx.epsb[:], EPS), writes=[cx.epsbb])


def declare_dense_inputs(cx, pre, with_ffn=True, with_proj=True):
    cfg = cx.cfg
    d = {}
    if with_ffn:
        d["g1"] = cx.din(pre + "g1", [128, cfg.DC], F32)
        d["w1t"] = cx.din(pre + "w1t", [cfg.FC, 128, cfg.DC * 256], F32)
        d["w2t"] = cx.din(pre + "w2t", [cfg.DC, 128, cfg.FC * 128], F32)
    if with_proj:
        d["gm"] = cx.din(pre + "gm", [128, cfg.DC], F32)
        d["wpt"] = cx.din(pre + "wpt", [cfg.NCC, 128, cfg.DC * 128], F32)
        d["hg"] = cx.din(pre + "hg", [128, 4], F32)
    return d


def declare_proj_outs(cx):
    cfg = cx.cfg
    o = {}
    o["qa"] = cx.dout("o_qa", [cfg.HA, 128, cfg.T], BF16)
    o["ka"] = cx.dout("o_ka", [cfg.HA, 128, cfg.T], BF16)
    o["va"] = cx.dout("o_va", [cfg.T, cfg.WA], BF16)
    o["qb"] = cx.dout("o_qb", [cfg.HB, 128, cfg.T], BF16)
    o["kb"] = cx.dout("o_kb", [cfg.HB, 128, cfg.T], BF16)
    o["vb"] = cx.dout("o_vb", [cfg.T, cfg.WB], BF16)
    o["qi"] = cx.dout("o_qi", [cfg.NQI, 128, cfg.T], BF16)
    o["ki"] = cx.dout("o_ki", [64, cfg.T], BF16)
    o["wi"] = cx.dout("o_wi", [cfg.T, cfg.NIH], F32)
    return o


def head_gains(cx, es, hg_ap):
    t, b = load_vec(cx, es, "hgs", hg_ap, 4)
    sc = 128.0 ** -0.5
    for col in (0, 2):
        cx.P.op("dve", lambda tt, col=col: tt.tensor_scalar(out=t[:, col:col + 1], in0=t[:, col:col + 1], scalar1=sc, scalar2=None, op0=ALU.mult),
                reads=[b], writes=[b])
    return t, b


def build_L1(cfg):
    cx = Ctx(cfg)
    common_consts(cx)
    x_in = cx.din("xT", [cfg.D, cfg.T], F32)
    di = declare_dense_inputs(cx, "")
    x_out = cx.dout("xT_out", [cfg.D, cfg.T], F32)
    outs = declare_proj_outs(cx)
    with ExitStack() as es2:
        W = alloc_dense(cx, es2)
        g1 = load_vec(cx, es2, "g1s", di["g1"], cfg.DC)
        gm = load_vec(cx, es2, "gms", di["gm"], cfg.DC)
        hg = head_gains(cx, es2, di["hg"])
        for g in range(cfg.T // cfg.G):
            tok0 = g * cfg.G
            norm_phase(cx, W, "xin", x_in, g1[0], g1[1], tok0)
            inproj_phase(cx, W, di["w1t"])
            linres_phase(cx, W, di["w2t"], cfg.FC, "xin", x_in, "xout", x_out, tok0, 0.5)
            norm_phase(cx, W, "xout", x_out, gm[0], gm[1], tok0)
            proj_phase(cx, W, di["wpt"], hg, outs, tok0)
        cx.P.finish()
    return cx


def tile_w1(w, cfg):
    a = w.reshape(cfg.DC, 128, 2, cfg.FC, 128).transpose(3, 1, 0, 2, 4)
    return np.ascontiguousarray(a).reshape(cfg.FC, 128, cfg.DC * 256)


def tile_w2(w, nch, cfg):
    a = w.reshape(nch, 128, cfg.DC, 128).transpose(2, 1, 0, 3)
    return np.ascontiguousarray(a).reshape(cfg.DC, 128, nch * 128)


def tile_wp(w, cfg):
    wpad = np.zeros((cfg.D, cfg.NCC * 128), np.float32)
    wpad[:, :cfg.PW] = w
    a = wpad.reshape(cfg.DC, 128, cfg.NCC, 128).transpose(2, 1, 0, 3)
    return np.ascontiguousarray(a).reshape(cfg.NCC, 128, cfg.DC * 128)


def vec_pc(v, cfg):
    return np.ascontiguousarray(v.reshape(cfg.DC, 128).T)


NBIS = 20
PEN = -30000.0


class DB:
    pass


class CtxA(Ctx):
    def __init__(self, cfg):
        self.cfg = cfg
        nc = bass.Bass("TRN2", target_bir_lowering=False)
        nc.dge_precook = False
        self.nc = nc
        self.es = ExitStack()
        self.P = Prog(nc, self.es)
        self.dbank = [self.es.enter_context(nc.psum_tensor("dbank%d" % i, [128, 1024], F32)) for i in range(4)]
        self.dbb = [Buf() for _ in range(4)]
        self.hb = [[Buf(), Buf()] for _ in range(4)]
        self.ones = self.es.enter_context(nc.sbuf_tensor("ones", [128, 128], BF16))
        self.onesb = Buf()
        self.P.op("dve", lambda t: t.memset(self.ones[:], 1.0), writes=[self.onesb])
        self.xbufs = {}


def dsa_phase(cx, es, I, out_mb):
    cfg, P = cx.cfg, cx.P
    HB, NIH, CPB = cfg.HB, cfg.NIH, cfg.CPB
    KT = 128 * CPB
    NQB = cfg.S // KT
    NN = 12 + CPB
    HW = HB * 128
    sb = lambda n, s, d: cx.sb(es, n, s, d)
    scores = sb("scores", [128, cfg.S], F32)
    scb = Buf()
    maskb = sb("maskb", [128, cfg.S], BF16)
    mkb = Buf()
    ki2 = sb("ki2", [128, cfg.S], BF16)
    ki2b = Buf()
    P.dma("sp", ki2[:], I["ki2"], writes=[ki2b])
    ident = sb("ident", [128, 128], BF16)
    identb = Buf()
    P.dma("sp", ident[:], I["ident"], writes=[identb])
    pen = sb("pen", [128, KT], F32)
    penb = Buf()
    qrel = sb("qrel", [128, 1], F32)
    P.dma("sp", qrel[:], I["qrel"], writes=[penb])
    P.dma("sp", pen[:], I["iota"], writes=[penb])
    P.op("dve", lambda t: t.tensor_scalar(out=pen[:], in0=pen[:], scalar1=qrel[:, 0:1], scalar2=PEN, op0=ALU.is_gt, op1=ALU.mult), reads=[penb], writes=[penb])
    cbt = sb("cbt", [128, HW], F32)
    cbb = Buf()
    P.dma("sp", cbt[:], I["cb"], writes=[cbb])
    qbt = [sb("qbt%d" % i, [128, HW], BF16) for i in range(2)]
    qbtb = [Buf(), Buf()]
    qit = [sb("qit%d" % i, [128, cfg.NQI * 128], BF16) for i in range(2)]
    qitb = [Buf(), Buf()]
    wit = [sb("wit%d" % i, [128, 3 * NIH], F32) for i in range(2)]
    witb = [Buf(), Buf()]
    R = [sb("R%d" % i, [128, KT], F32) for i in range(4)]
    Rb = [Buf() for _ in range(4)]
    sm = sb("sm", [128, 8], F32)
    smb = Buf()
    sma = sb("sma", [128, 2], F32)
    smab = Buf()
    midb = Buf()
    mkb2 = Buf()
    ptmp = [sb("ptmp%d" % i, [128, KT], F32) for i in range(2)]
    ptb = [Buf(), Buf()]
    kt_ = [sb("kt%d" % i, [128, HW], BF16) for i in range(2)]
    ktb = [Buf(), Buf()]
    vt_ = [sb("vt%d" % i, [128, HW], BF16) for i in range(2)]
    vtb = [Buf(), Buf()]
    E = [sb("E%d" % i, [128, HW], F32) for i in range(2)]
    Eb = [Buf(), Buf()]
    PT = [sb("PT%d" % i, [128, HW], BF16) for i in range(2)]
    PTb = [Buf(), Buf()]
    Ehb = [[Buf(), Buf()] for _ in range(2)]
    PThb = [[Buf(), Buf()] for _ in range(2)]
    tbt = [sb("tbt%d" % i, [128, HW], F32) for i in range(2)]
    tbb = [Buf(), Buf()]
    ob = sb("ob", [128, HW], BF16)
    obb = Buf()
    rl = sb("rl", [128, HW], F32)
    rlb = Buf()
    cnt = {"R": 0, "kv": 0, "E": 0, "tb": 0, "ps": 0, "pt": 0}
    for m in range(NQB):
        qi_ = m % 2
        P.dma("sp", qbt[qi_][:], I["qb"][m], writes=[qbtb[qi_]])
        P.dma("sp", qit[qi_][:], I["qi"][m], writes=[qitb[qi_]])
        wt = wit[qi_]
        P.dma("sp", wt[:, :NIH], I["wi"][m], writes=[witb[qi_]])
        P.op("dve", lambda t, wt=wt: t.scalar_tensor_tensor(out=wt[:, NIH:2 * NIH], in0=wt[:, :NIH], scalar=-1.0, in1=wt[:, :NIH], op0=ALU.mult, op1=ALU.max), reads=[witb[qi_]], writes=[witb[qi_]])
        P.op("dve", lambda t, wt=wt: t.tensor_scalar(out=wt[:, 2 * NIH:3 * NIH], in0=wt[:, :NIH], scalar1=0.0, scalar2=2.0, op0=ALU.is_ge, op1=ALU.mult), reads=[witb[qi_]], writes=[witb[qi_]])
        P.op("dve", lambda t, wt=wt: t.tensor_scalar(out=wt[:, 2 * NIH:3 * NIH], in0=wt[:, 2 * NIH:3 * NIH], scalar1=-1.0, scalar2=None, op0=ALU.add), reads=[witb[qi_]], writes=[witb[qi_]])
        L = KT * (m + 1)
        for kt in range(m + 1):
            k0 = kt * KT
            for h in range(NIH):
                bi = cnt["ps"] % 4
                cnt["ps"] += 1
                pb = cx.dbank[bi // 2][:, (bi % 2) * 512:(bi % 2) * 512 + KT]
                pbb = cx.hb[bi // 2][bi % 2]
                p0 = (h % 2) * 64
                P.op("pe", lambda t, pb=pb, h=h, p0=p0, k0=k0, qi_=qi_: t.matmul(pb, lhsT=qit[qi_][p0:p0 + 64, (h // 2) * 128:(h // 2) * 128 + 128], rhs=ki2[p0:p0 + 64, k0:k0 + KT],
                                                                              start=True, stop=True), reads=[qitb[qi_], ki2b], writes=[pbb, cx.dbb[bi // 2]])
                ri = cnt["R"] % 4
                cnt["R"] += 1
                Rt = R[ri]
                P.op("act", lambda t, Rt=Rt, pb=pb, wt=wt, h=h: t.activation(out=Rt[:], in_=pb, func=AF.Relu, scale=wt[:, NIH + h:NIH + h + 1]),
                     reads=[pbb, witb[qi_]], writes=[Rb[ri]])
                sc = scores[:, k0:k0 + KT]
                hsplit = NIH // 2
                if h < hsplit:
                    if h == 0:
                        P.op("dve", lambda t, Rt=Rt, sc=sc, wt=wt, h=h: t.tensor_scalar(out=sc, in0=Rt[:], scalar1=wt[:, 2 * NIH + h:2 * NIH + h + 1], scalar2=None, op0=ALU.mult),
                             reads=[Rb[ri], witb[qi_]], writes=[scb])
                    else:
                        P.op("dve", lambda t, Rt=Rt, sc=sc, wt=wt, h=h: t.scalar_tensor_tensor(out=sc, in0=Rt[:], scalar=wt[:, 2 * NIH + h:2 * NIH + h + 1], in1=sc, op0=ALU.mult, op1=ALU.add),
                             reads=[Rb[ri], witb[qi_], scb], writes=[scb])
                else:
                    pi = cnt["pt"] % 2
                    pt = ptmp[pi]
                    if h == hsplit:
                        P.op("pool", lambda t, Rt=Rt, pt=pt, wt=wt, h=h: t.tensor_scalar(out=pt[:], in0=Rt[:], scalar1=wt[:, 2 * NIH + h:2 * NIH + h + 1], scalar2=None, op0=ALU.mult),
                             reads=[Rb[ri], witb[qi_]], writes=[ptb[pi]])
                    else:
                        P.op("pool", lambda t, Rt=Rt, wt=wt, h=h: t.tensor_scalar(out=Rt[:], in0=Rt[:], scalar1=wt[:, 2 * NIH + h:2 * NIH + h + 1], scalar2=None, op0=ALU.mult),
                             reads=[Rb[ri], witb[qi_]], writes=[Rb[ri]])
                        P.op("pool", lambda t, Rt=Rt, pt=pt: t.tensor_tensor(out=pt[:], in0=pt[:], in1=Rt[:], op=ALU.add),
                             reads=[Rb[ri], ptb[pi]], writes=[ptb[pi]])
                    if h == NIH - 1:
                        P.op("dve", lambda t, pt=pt, sc=sc: t.tensor_tensor(out=sc, in0=sc, in1=pt[:], op=ALU.add), reads=[ptb[pi], scb], writes=[scb])
                        cnt["pt"] += 1
        P.op("dve", lambda t, L=L: t.tensor_reduce(out=sm[:, 0:1], in_=scores[:, :L], axis=mybir.AxisListType.X, op=ALU.min), reads=[scb], writes=[smb])
        P.op("dve", lambda t, L=L: t.tensor_tensor(out=scores[:, L - KT:L], in0=scores[:, L - KT:L], in1=pen[:], op=ALU.add), reads=[scb, penb], writes=[scb])
        P.op("dve", lambda t, L=L: t.tensor_reduce(out=sm[:, 5:6], in_=scores[:, :L], axis=mybir.AxisListType.X, op=ALU.max), reads=[scb, smb], writes=[smb])
        P.op("dve", lambda t: t.scalar_tensor_tensor(out=sm[:, 1:2], in0=sm[:, 5:6], scalar=1e-6, in1=sm[:, 0:1], op0=ALU.add, op1=ALU.subtract), reads=[smb], writes=[smb])
        La = L // 2
        nact = L - La
        for it in range(NBIS):
            f = 2.0 ** -(it + 1)
            P.op("dve", lambda t, f=f: t.scalar_tensor_tensor(out=sm[:, 2:3], in0=sm[:, 1:2], scalar=f, in1=sm[:, 0:1], op0=ALU.mult, op1=ALU.add), reads=[smb], writes=[smb, midb])
            P.op("act", lambda t, L=L, La=La: t.activation(out=maskb[:, La:L], in_=scores[:, La:L], func=AF.Sign, scale=-1.0, bias=sm[:, 2:3], accum_out=sma[:, 0:1]),
                 reads=[scb, midb], writes=[mkb2, smab])
            P.op("dve", lambda t, La=La: t.tensor_scalar(out=maskb[:, :La], in0=scores[:, :La], scalar1=sm[:, 2:3], scalar2=None, op0=ALU.is_ge, op1=ALU.add, accum_out=sm[:, 3:4]),
                 reads=[scb, smb], writes=[mkb, smb])
            P.op("dve", lambda t: t.scalar_tensor_tensor(out=sm[:, 3:4], in0=sma[:, 0:1], scalar=-0.5, in1=sm[:, 3:4], op0=ALU.mult, op1=ALU.add), reads=[smb, smab], writes=[smb])
            P.op("dve", lambda t, f=f, nact=nact: t.tensor_scalar(out=sm[:, 4:5], in0=sm[:, 3:4], scalar1=255.5 - 0.5 * nact, scalar2=f, op0=ALU.is_ge, op1=ALU.mult), reads=[smb], writes=[smb])
            P.op("dve", lambda t: t.scalar_tensor_tensor(out=sm[:, 0:1], in0=sm[:, 4:5], scalar=sm[:, 1:2], in1=sm[:, 0:1], op0=ALU.mult, op1=ALU.add), reads=[smb], writes=[smb])
        P.op("dve", lambda t, L=L: t.tensor_scalar(out=maskb[:, :L], in0=scores[:, :L], scalar1=sm[:, 0:1], scalar2=None, op0=ALU.is_ge), reads=[scb, smb], writes=[mkb, mkb2])
        nkb = CPB * (m + 1)
        oacc, lacc, sbk, mtb = cx.dbank[2], cx.dbank[3], cx.dbank[0], cx.dbank[1]
        mt16 = mtb[:].bitcast(BF16)
        for kb in range(nkb):
            ki_ = cnt["kv"] % 2
            cnt["kv"] += 1
            P.dma("sp", kt_[ki_][:], I["kb"][kb], writes=[ktb[ki_]])
            P.dma("sp", vt_[ki_][:], I["vb"][kb], writes=[vtb[ki_]])
            mi = kb % 2
            mts = mt16[:, mi * 1024:mi * 1024 + 128]
            P.op("pe", lambda t, mts=mts, kb=kb: t.transpose(out=mts, in_=maskb[:, kb * 128:(kb + 1) * 128], identity=ident[:]),
                 reads=[mkb, mkb2, identb], writes=[cx.hb[1][mi]])
            ei = cnt["E"] % 2
            cnt["E"] += 1
            Et, PTt = E[ei], PT[ei]
            delta = (nkb - 1) - kb
            tb = None
            if delta < NN:
                ti = cnt["tb"] % 2
                cnt["tb"] += 1
                tb = tbt[ti]
                P.dma("sp", tb[:], I["tb"][delta], writes=[tbb[ti]])
                P.op("dve", lambda t, tb=tb: t.tensor_tensor(out=tb[:], in0=tb[:], in1=cbt[:], op=ALU.subtract), reads=[tbb[ti], cbb], writes=[tbb[ti]])
                P.op("act", lambda t, tb=tb: t.activation(out=tb[:], in_=tb[:], func=AF.Exp), reads=[tbb[ti]], writes=[tbb[ti]])
            for hh in range((HB + 3) // 4):
                nh = min(4, HB - 4 * hh)
                c0, c1 = hh * 512, hh * 512 + nh * 128
                for h in range(4 * hh, 4 * hh + nh):
                    P.op("pe", lambda t, h=h, ki_=ki_, qi_=qi_: t.matmul(sbk[:, h * 128:(h + 1) * 128], lhsT=kt_[ki_][:, h * 128:(h + 1) * 128], rhs=qbt[qi_][:, h * 128:(h + 1) * 128],
                                                                      start=True, stop=True), reads=[ktb[ki_], qbtb[qi_]], writes=[cx.hb[0][hh]])
                P.op("act", lambda t, Et=Et, c0=c0, c1=c1: t.activation(out=Et[:, c0:c1], in_=sbk[:, c0:c1], func=AF.Exp), reads=[cx.hb[0][hh]], writes=[Ehb[ei][hh]])
                if tb is not None:
                    P.op("dve", lambda t, tb=tb, Et=Et, c0=c0, c1=c1: t.tensor_tensor(out=Et[:, c0:c1], in0=Et[:, c0:c1], in1=tb[:, c0:c1], op=ALU.mult),
                         reads=[tbb[ti], Ehb[ei][hh]], writes=[Ehb[ei][hh]])
                mtbc = mts.unsqueeze(1).to_broadcast([128, nh, 128])
                P.op("dve", lambda t, Et=Et, PTt=PTt, mtbc=mtbc, c0=c0, c1=c1, nh=nh: t.tensor_tensor(out=PTt[:, c0:c1].rearrange("p (h t) -> p h t", h=nh), in0=Et[:, c0:c1].rearrange("p (h t) -> p h t", h=nh), in1=mtbc, op=ALU.mult),
                     reads=[Ehb[ei][hh], cx.hb[1][mi]], writes=[PThb[ei][hh]])
                for h in range(4 * hh, 4 * hh + nh):
                    P.op("pe", lambda t, h=h, ki_=ki_, PTt=PTt, kb=kb, nkb=nkb: t.matmul(oacc[:, h * 128:(h + 1) * 128], lhsT=vt_[ki_][:, h * 128:(h + 1) * 128], rhs=PTt[:, h * 128:(h + 1) * 128],
                                                                                      start=(kb == 0 and h % 4 == 0), stop=(kb == nkb - 1), skip_group_check=True), reads=[vtb[ki_], PThb[ei][hh]], writes=[cx.dbb[2]])
                P.op("pe", lambda t, PTt=PTt, kb=kb, c0=c0, c1=c1, nkb=nkb: t.matmul(lacc[:, c0:c1], lhsT=cx.ones[:], rhs=PTt[:, c0:c1], start=(kb == 0), stop=(kb == nkb - 1)),
                     reads=[cx.onesb, PThb[ei][hh]], writes=[cx.dbb[3]])
        P.op("dve", lambda t: t.reciprocal(out=rl[:], in_=lacc[:, :HW]), reads=[cx.dbb[3]], writes=[rlb])
        P.op("dve", lambda t: t.tensor_tensor(out=ob[:], in0=oacc[:, :HW], in1=rl[:], op=ALU.mult), reads=[cx.dbb[2], rlb], writes=[obb])
        P.dma("sp", out_mb[:, :, m * 128:(m + 1) * 128], ob[:].rearrange("p (h t) -> p h t", h=HB), reads=[obb])


def dil_phase(cx, es, I, out_ma):
    cfg, P = cx.cfg, cx.P
    HA = cfg.HA
    HW = HA * 128
    sb = lambda n, s, d: cx.sb(es, n, s, d)
    npf = sb("npf", [128, 1], F32)
    npfb = Buf()
    P.dma("sp", npf[:], I["npf"], writes=[npfb])
    EBA = [[sb("eba%d_%d" % (br, v), [128, HW], F32) for v in range(3)] for br in range(3)]
    ebab = Buf()
    tmpv = sb("aE0", [128, HW], F32)
    for br in range(3):
        for pc in range(2):
            e_ = EBA[br][pc]
            P.dma("sp", e_[:], I["tb"][br, pc], writes=[ebab])
            P.dma("sp", tmpv[:], I["vm"][br, pc], reads=[ebab], writes=[ebab])
            P.op("act", lambda t, e_=e_: t.activation(out=e_[:], in_=e_[:], func=AF.Exp), reads=[ebab], writes=[ebab])
            P.op("dve", lambda t, e_=e_: t.tensor_tensor(out=e_[:], in0=e_[:], in1=tmpv[:], op=ALU.mult), reads=[ebab], writes=[ebab])
        P.op("dve", lambda t, br=br: t.tensor_scalar(out=EBA[br][2][:], in0=EBA[br][0][:], scalar1=npf[:, 0:1], scalar2=None, op0=ALU.mult), reads=[ebab, npfb], writes=[ebab])
    oaccT = sb("oaccT", [128, HA, 2048], F32)
    laccT = sb("laccT", [128, HA, 2048], F32)
    accb = Buf()
    qt = [sb("aq%d" % i, [128, HW], BF16) for i in range(2)]
    kt = [sb("ak%d" % i, [128, 2, HW], BF16) for i in range(2)]
    vt = [sb("av%d" % i, [128, 2, HW], BF16) for i in range(2)]
    qkvb = [Buf(), Buf()]
    E0 = tmpv
    E = [E0, E0]
    Eb0 = ebab
    Eb = [Eb0, Eb0]
    PT = [sb("aPT%d" % i, [128, HW], BF16) for i in range(2)]
    PTb = [Buf(), Buf()]
    ob = [sb("aob%d" % i, [128, HA, 256], BF16) for i in range(2)]
    obb = [Buf(), Buf()]
    n = 0
    ne = 0
    for u in range(2):
        for br, dil in enumerate((1, 4, 16)):
            span = 128 * dil
            for ti in range(16):
                np_, r = ti // dil, ti % dil
                bi = n % 2
                n += 1
                P.dma("sp", qt[bi][:], I["q"][br, u, ti], writes=[qkvb[bi]])
                P.dma("sp", kt[bi][:], I["k"][br, u, ti].rearrange("c p x -> p c x"), writes=[qkvb[bi]])
                P.dma("sp", vt[bi][:], I["v"][br, u, ti].rearrange("c p x -> p c x"), writes=[qkvb[bi]])
                sbk = cx.dbank[bi]
                oacc, lacc = cx.dbank[2], cx.dbank[3]
                for pc in range(2):
                    for h in range(HA):
                        P.op("pe", lambda t, h=h, bi=bi, pc=pc, sbk=sbk: t.matmul(sbk[:, h * 128:(h + 1) * 128], lhsT=kt[bi][:, pc, h * 128:(h + 1) * 128], rhs=qt[bi][:, h * 128:(h + 1) * 128],
                                                                              start=True, stop=True), reads=[qkvb[bi]], writes=[cx.dbb[bi]])
                    ei = ne % 2
                    ne += 1
                    Et, PTt = E[ei], PT[ei]
                    P.op("act", lambda t, Et=Et, sbk=sbk: t.activation(out=Et[:], in_=sbk[:, :HW], func=AF.Exp), reads=[cx.dbb[bi]], writes=[Eb[ei]])
                    ev = EBA[br][2] if (pc == 0 and u == 0 and np_ == 0) else EBA[br][pc]
                    P.op("dve", lambda t, Et=Et, PTt=PTt, ev=ev: t.tensor_tensor(out=PTt[:], in0=Et[:], in1=ev[:], op=ALU.mult), reads=[Eb[ei], ebab], writes=[PTb[ei]])
                    for h in range(HA):
                        P.op("pe", lambda t, h=h, bi=bi, pc=pc, PTt=PTt: t.matmul(oacc[:, h * 128:(h + 1) * 128], lhsT=vt[bi][:, pc, h * 128:(h + 1) * 128], rhs=PTt[:, h * 128:(h + 1) * 128],
                                                                              start=(pc == 0 and h % 4 == 0), stop=(pc == 1), skip_group_check=True), reads=[qkvb[bi], PTb[ei]], writes=[cx.dbb[2]])
                    for c0 in range(0, HW, 512):
                        c1 = min(HW, c0 + 512)
                        P.op("pe", lambda t, PTt=PTt, pc=pc, c0=c0, c1=c1: t.matmul(lacc[:, c0:c1], lhsT=cx.ones[:], rhs=PTt[:, c0:c1], start=(pc == 0), stop=(pc == 1)),
                             reads=[cx.onesb, PTb[ei]], writes=[cx.dbb[3]])
                s0 = np_ * span + r
                dst_o = oaccT[:, :, s0:s0 + 127 * dil + 1:dil]
                dst_l = laccT[:, :, s0:s0 + 127 * dil + 1:dil]
                ov = oacc[:, :HW].rearrange("p (h t) -> p h t", h=HA)
                lv = lacc[:, :HW].rearrange("p (h t) -> p h t", h=HA)
                if br == 0:
                    P.op("dve", lambda t, dst_o=dst_o, ov=ov: t.tensor_copy(out=dst_o, in_=ov), reads=[cx.dbb[2]], writes=[accb])
                    P.op("act", lambda t, dst_l=dst_l, lv=lv: t.activation(out=dst_l, in_=lv, func=AF.Copy), reads=[cx.dbb[3]], writes=[accb])
                else:
                    P.op("dve", lambda t, dst_o=dst_o, ov=ov: t.tensor_tensor(out=dst_o, in0=dst_o, in1=ov, op=ALU.add), reads=[cx.dbb[2], accb], writes=[accb])
                    P.op("dve", lambda t, dst_l=dst_l, lv=lv: t.tensor_tensor(out=dst_l, in0=dst_l, in1=lv, op=ALU.add), reads=[cx.dbb[3], accb], writes=[accb])
        for c in range(8):
            sl = slice(c * 256, (c + 1) * 256)
            P.op("dve", lambda t, sl=sl: t.reciprocal(out=laccT[:, :, sl], in_=laccT[:, :, sl]), reads=[accb], writes=[accb])
            oi = c % 2
            P.op("dve", lambda t, sl=sl, oi=oi: t.tensor_tensor(out=ob[oi][:], in0=oaccT[:, :, sl], in1=laccT[:, :, sl], op=ALU.mult), reads=[accb], writes=[obb[oi]])
            P.dma("sp", out_ma[:, :, u * 2048 + c * 256: u * 2048 + (c + 1) * 256], ob[oi][:], reads=[obb[oi]])


def build_L2(cfg):
    cx = CtxA(cfg)
    HA, HB, KT = cfg.HA, cfg.HB, 128 * cfg.CPB
    NQB = cfg.S // KT
    NKB = cfg.S // 128
    A = {"npf": cx.din("a_npf", [128, 1], F32), "tb": cx.din("a_tb", [3, 2, 128, HA * 128], F32), "vm": cx.din("a_vm", [3, 2, 128, HA * 128], F32),
         "q": cx.din("a_q", [3, 2, 16, 128, HA * 128], BF16), "k": cx.din("a_k", [3, 2, 16, 2, 128, HA * 128], BF16),
         "v": cx.din("a_v", [3, 2, 16, 2, 128, HA * 128], BF16)}
    Bd = {"ki2": cx.din("b_ki2", [128, cfg.S], BF16), "ident": cx.din("b_ident", [128, 128], BF16), "qrel": cx.din("b_qrel", [128, 1], F32),
          "iota": cx.din("b_iota", [128, KT], F32), "cb": cx.din("b_cb", [128, HB * 128], F32), "qb": cx.din("b_qb", [NQB, 128, HB * 128], BF16),
          "qi": cx.din("b_qi", [NQB, 128, cfg.NQI * 128], BF16), "wi": cx.din("b_wi", [NQB, 128, cfg.NIH], F32),
          "kb": cx.din("b_kb", [NKB, 128, HB * 128], BF16), "vb": cx.din("b_vb", [NKB, 128, HB * 128], BF16),
          "tb": cx.din("b_tb", [12 + cfg.CPB, 128, HB * 128], F32)}
    o_ma = cx.dout("o_ma", [128, HA, cfg.T], BF16)
    o_mb = cx.dout("o_mb", [128, HB, NQB * 128], BF16)
    with ExitStack() as es2:
        dil_phase(cx, es2, A, o_ma)
        cx.P.barrier()
    with ExitStack() as es3:
        dsa_phase(cx, es3, Bd, o_mb)
        cx.P.finish()
    return cx


def rel_bucket_np(dist):
    dist = np.asarray(dist, np.int64)
    df = np.maximum(dist, 1).astype(np.float32)
    large = 16 + (np.log(df / np.float32(16)) / np.float32(np.log(2048 / 16)) * np.float32(16)).astype(np.int32)
    large = np.minimum(large, 31)
    return np.where(dist < 16, np.maximum(dist, 0), large).astype(np.int64)


def core_tokens(cfg, c):
    r = c % cfg.CPB
    u0, u1 = r, cfg.NU - 1 - r
    return np.concatenate([np.arange(2048) + 2048 * u0, np.arange(2048) + 2048 * u1])


def gather_global(cfg, outs_per_core):
    G = []
    for b in range(cfg.B):
        g = {"qa": np.zeros((cfg.HA, 128, cfg.S), NPBF), "ka": np.zeros((cfg.HA, 128, cfg.S), NPBF), "va": np.zeros((cfg.S, cfg.WA), NPBF),
             "qb": np.zeros((cfg.HB, 128, cfg.S), NPBF), "kb": np.zeros((cfg.HB, 128, cfg.S), NPBF), "vb": np.zeros((cfg.S, cfg.WB), NPBF),
             "qi": np.zeros((cfg.NQI, 128, cfg.S), NPBF), "ki": np.zeros((64, cfg.S), NPBF), "wi": np.zeros((cfg.S, cfg.NIH), np.float32)}
        for r in range(cfg.CPB):
            c = b * cfg.CPB + r
            pos = core_tokens(cfg, c)
            o = outs_per_core[c]
            for k in ("qa", "ka", "qb", "kb", "qi", "ki"):
                g[k][..., pos] = o["o_" + k]
            for k in ("va", "vb", "wi"):
                g[k][pos] = o["o_" + k]
        G.append(g)
    return G


def l2_inputs(cfg, G, rel_bias, c):
    b, r = c // cfg.CPB, c % cfg.CPB
    g = G[b]
    HA, HB, CPB = cfg.HA, cfg.HB, cfg.CPB
    rel_bias = np.asarray(rel_bias, np.float32)
    ins = {}
    units = (r, cfg.NU - 1 - r)
    aq = np.zeros((3, 2, 16, 128, HA * 128), NPBF)
    ak = np.zeros((3, 2, 16, 2, 128, HA * 128), NPBF)
    av = np.zeros((3, 2, 16, 2, 128, HA * 128), NPBF)
    atb = np.zeros((3, 2, 128, HA, 128), np.float32)
    avm = np.zeros((3, 2, 128, HA, 128), np.float32)
    jj, ii = np.meshgrid(np.arange(128), np.arange(128), indexing="ij")
    for br, dil in enumerate((1, 4, 16)):
        span = 128 * dil
        for pc in range(2):
            step = ii - jj + (128 if pc == 0 else 0)
            valid = (step >= 0) & (step <= 128) & ((jj >= ii) if pc == 0 else (jj <= ii))
            bk = rel_bucket_np(np.clip(step, 0, 128) * dil)
            atb[br, pc] = np.where(valid[:, None, :], rel_bias[bk][:, :, :HA].transpose(0, 2, 1), 0.0)
            avm[br, pc] = valid[:, None, :].astype(np.float32)
        for u, gu in enumerate(units):
            for ti in range(16):
                np_, r_ = ti // dil, ti % dil
                p = 2048 * gu + np_ * span + r_ + dil * np.arange(128)
                aq[br, u, ti] = g["qa"][:, :, p].transpose(1, 0, 2).reshape(128, HA * 128)
                ak[br, u, ti, 1] = g["ka"][:, :, p].transpose(1, 0, 2).reshape(128, HA * 128)
                av[br, u, ti, 1] = g["va"][p]
                pp = p - span
                if pp[0] >= 0:
                    ak[br, u, ti, 0] = g["ka"][:, :, pp].transpose(1, 0, 2).reshape(128, HA * 128)
                    av[br, u, ti, 0] = g["va"][pp]
    ins.update({"a_q": aq, "a_k": ak, "a_v": av, "a_tb": atb.reshape(3, 2, 128, HA * 128), "a_vm": avm.reshape(3, 2, 128, HA * 128),
                "a_npf": np.full((128, 1), 0.0 if r == 0 else 1.0, np.float32)})
    KT = 128 * CPB
    NQB = cfg.S // KT
    NKB = cfg.S // 128
    gq = CPB * np.arange(NQB) + r
    qb = g["qb"].reshape(HB, 128, NKB, 128)[:, :, gq]
    ins["b_qb"] = np.ascontiguousarray(qb.transpose(2, 1, 0, 3)).reshape(NQB, 128, HB * 128)
    qi = g["qi"].reshape(cfg.NQI, 128, NKB, 128)[:, :, gq]
    ins["b_qi"] = np.ascontiguousarray(qi.transpose(2, 1, 0, 3)).reshape(NQB, 128, cfg.NQI * 128)
    ins["b_wi"] = np.ascontiguousarray(g["wi"].reshape(NKB, 128, cfg.NIH)[gq])
    ins["b_ki2"] = np.ascontiguousarray(np.concatenate([g["ki"], g["ki"]], 0))
    ins["b_kb"] = np.ascontiguousarray(g["kb"].reshape(HB, 128, NKB, 128).transpose(2, 1, 0, 3)).reshape(NKB, 128, HB * 128)
    ins["b_vb"] = np.ascontiguousarray(g["vb"].reshape(NKB, 128, HB * 128))
    ins["b_qrel"] = (r * 128 + np.arange(128, dtype=np.float32)).reshape(128, 1)
    ins["b_iota"] = np.ascontiguousarray(np.broadcast_to(np.arange(KT, dtype=np.float32), (128, KT)))
    ins["b_cb"] = np.ascontiguousarray(np.broadcast_to(rel_bias[31, HA:HA + HB][None, :, None], (128, HB, 128))).reshape(128, HB * 128)
    NN = 12 + CPB
    tb = np.zeros((NN, 128, HB, 128), np.float32)
    for d in range(NN):
        dist = 128 * (d - (CPB - 1 - r)) + ii - jj
        tb[d] = rel_bias[rel_bucket_np(np.maximum(dist, 0))][:, :, HA:HA + HB].transpose(0, 2, 1)
    ins["b_tb"] = tb.reshape(NN, 128, HB * 128)
    ins["b_ident"] = np.eye(128, dtype=np.float32).astype(NPBF)
    return ins


def scatter_mix(cfg, res_per_core):
    M = []
    KT = 128 * cfg.CPB
    NQB = cfg.S // KT
    for b in range(cfg.B):
        mix = np.zeros((cfg.S, cfg.WA + cfg.WB), NPBF)
        for r in range(cfg.CPB):
            c = b * cfg.CPB + r
            pos = core_tokens(cfg, c)
            ma = res_per_core[c]["o_ma"]
            mix[pos, :cfg.WA] = ma.transpose(2, 1, 0).reshape(cfg.T, cfg.WA)
            mb = res_per_core[c]["o_mb"].reshape(128, cfg.HB, NQB, 128)
            gq = cfg.CPB * np.arange(NQB) + r
            posb = (gq[:, None] * 128 + np.arange(128)[None, :]).reshape(-1)
            mix[posb, cfg.WA:] = mb.transpose(2, 3, 1, 0).reshape(NQB * 128, cfg.WB)
        M.append(mix)
    return M


def build_L3(cfg, with_next=True):
    cx = Ctx(cfg)
    common_consts(cx)
    NM = (cfg.WA + cfg.WB) // 128
    x_in = cx.din("xT", [cfg.D, cfg.T], F32)
    mixT = cx.din("mixT", [NM * 128, cfg.T], BF16)
    wot = cx.din("wot", [cfg.DC, 128, NM * 128], F32)
    f2 = declare_dense_inputs(cx, "f2_", with_proj=False)
    xa = cx.dint("xa", [cfg.D, cfg.T], F32)
    xb = cx.dout("xT_mid", [cfg.D, cfg.T], F32)
    if with_next:
        nx = declare_dense_inputs(cx, "nx_")
        xc = cx.dout("xT_out", [cfg.D, cfg.T], F32)
        outs = declare_proj_outs(cx)
    with ExitStack() as es2:
        W = alloc_dense(cx, es2)
        g2 = load_vec(cx, es2, "g2s", f2["g1"], cfg.DC)
        if with_next:
            g1 = load_vec(cx, es2, "g1s", nx["g1"], cfg.DC)
            gm = load_vec(cx, es2, "gms", nx["gm"], cfg.DC)
            hg = head_gains(cx, es2, nx["hg"])
        for g in range(cfg.T // cfg.G):
            tok0 = g * cfg.G
            load_actT(cx, W, mixT, NM, tok0)
            linres_phase(cx, W, wot, NM, "xin", x_in, "xa", xa, tok0, 1.0)
            norm_phase(cx, W, "xa", xa, g2[0], g2[1], tok0)
            inproj_phase(cx, W, f2["w1t"])
            linres_phase(cx, W, f2["w2t"], cfg.FC, "xa", xa, "xb", xb, tok0, 0.5)
            if not with_next:
                continue
            norm_phase(cx, W, "xb", xb, g1[0], g1[1], tok0)
            inproj_phase(cx, W, nx["w1t"])
            linres_phase(cx, W, nx["w2t"], cfg.FC, "xb", xb, "xc", xc, tok0, 0.5)
            norm_phase(cx, W, "xc", xc, gm[0], gm[1], tok0)
            proj_phase(cx, W, nx["wpt"], hg, outs, tok0)
        cx.P.finish()
    return cx


def _run(cx, in_maps, n):
    res = run_bass_kernel_spmd(cx.nc, in_maps, core_ids=list(range(n)))
    cx.es.close()
    return res.results


def layer_dense_inputs(cfg, inp, l, pre, with_ffn1=True):
    d = {}
    if with_ffn1:
        d[pre + "g1"] = vec_pc(inp["norm_ffn1"][l], cfg)
        d[pre + "w1t"] = tile_w1(inp["w_ffn1_in"][l], cfg)
        d[pre + "w2t"] = tile_w2(inp["w_ffn1_out"][l], cfg.FC, cfg)
    d[pre + "gm"] = vec_pc(inp["norm_mix"][l], cfg)
    d[pre + "wpt"] = tile_wp(inp["w_in"][l], cfg)
    d[pre + "hg"] = np.ascontiguousarray(np.stack([inp["q_norm_a"][l], inp["k_norm_a"][l], inp["q_norm_b"][l], inp["k_norm_b"][l]], 1))
    return d


def run_model(cfg, inp):
    inp = {k: np.asarray(v) for k, v in inp.items()}
    n = cfg.NCORES
    x = inp["x"]
    xT = [np.ascontiguousarray(x[c // cfg.CPB][core_tokens(cfg, c)].T) for c in range(n)]
    cx = build_L1(cfg)
    d0 = layer_dense_inputs(cfg, inp, 0, "")
    res = _run(cx, [dict(d0, xT=xT[c]) for c in range(n)], n)
    cxa = build_L2(cfg)
    cx3 = None
    for l in range(cfg.DEPTH):
        G = gather_global(cfg, res)
        resa = _run(cxa, [l2_inputs(cfg, G, inp["rel_bias"], c) for c in range(n)], n)
        if l + 1 < cfg.DEPTH:
            cxa = build_L2(cfg)
        mix = scatter_mix(cfg, resa)
        last = (l + 1 == cfg.DEPTH)
        cx3 = build_L3(cfg, with_next=not last)
        d3 = {} if last else layer_dense_inputs(cfg, inp, l + 1, "nx_")
        d3["wot"] = tile_w2(inp["w_out"][l], (cfg.WA + cfg.WB) // 128, cfg)
        d3["f2_g1"] = vec_pc(inp["norm_ffn2"][l], cfg)
        d3["f2_w1t"] = tile_w1(inp["w_ffn2_in"][l], cfg)
        d3["f2_w2t"] = tile_w2(inp["w_ffn2_out"][l], cfg.FC, cfg)
        ims = []
        for c in range(n):
            xin = res[c]["xT_out"]
            mt = np.ascontiguousarray(mix[c // cfg.CPB][core_tokens(cfg, c)].T)
            ims.append(dict(d3, xT=xin, mixT=mt))
        res = _run(cx3, ims, n)
    out = np.zeros((cfg.B, cfg.S, cfg.D), np.float32)
    for c in range(n):
        out[c // cfg.CPB, core_tokens(cfg, c)] = res[c]["xT_mid"].T
    return out


def kernel(**inputs):
    return run_model(Cfg(), inputs)
```

```python
import numpy as np
from contextlib import ExitStack
import ml_dtypes
import concourse.bass as bass
import concourse.mybir as mybir
from concourse.bass_utils import run_bass_kernel_spmd

F32 = mybir.dt.float32
F32R = mybir.dt.float32r
BF16 = mybir.dt.bfloat16
ALU = mybir.AluOpType
AF = mybir.ActivationFunctionType
NPBF = ml_dtypes.bfloat16
EPS = 1e-6


class Cfg:
    def __init__(s, D=2048, F=5632, HA=8, HB=8, NIH=16, B=2, S=16384, CPB=4, DEPTH=2):
        s.D, s.F, s.HA, s.HB, s.NIH, s.B, s.S, s.CPB, s.DEPTH = D, F, HA, HB, NIH, B, S, CPB, DEPTH
        s.DC, s.FC = D // 128, F // 128
        s.NCORES = B * CPB
        s.UNIT = 2048
        s.NU = S // s.UNIT
        assert s.NU == 2 * CPB
        s.T = 2 * s.UNIT
        s.G = 1024
        s.WA, s.WB, s.QI = HA * 128, HB * 128, NIH * 64
        s.PW = 3 * s.WA + 3 * s.WB + s.QI + 64 + NIH
        s.NCC = (s.PW + 127) // 128
        s.o_qa, s.o_ka, s.o_va = 0, s.WA, 2 * s.WA
        s.o_qb, s.o_kb, s.o_vb = 3 * s.WA, 3 * s.WA + s.WB, 3 * s.WA + 2 * s.WB
        s.o_qi = 3 * s.WA + 3 * s.WB
        s.o_ki = s.o_qi + s.QI
        s.o_wi = s.o_ki + 64
        assert s.o_ki % 128 == 0
        s.NQI = s.QI // 128


class Buf:
    __slots__ = ("lw", "rd")

    def __init__(self):
        self.lw = None
        self.rd = []


class EngS:
    def __init__(self, name):
        self.name = name
        self.ops = []
        self.count = 0
        self.waited = {}


class Prog:
    NDMASEM = 12

    def __init__(self, nc, es):
        self.nc = nc
        self.E = {n: EngS(n) for n in ("pe", "act", "dve", "pool", "sp")}
        self.sems = {}
        for n in self.E:
            self.sems[("e", n)] = es.enter_context(nc.semaphore("s_" + n))
        self.dq = {}
        for q in ("sp", "pool"):
            lst = []
            for i in range(self.NDMASEM):
                k = ("d", q, i)
                self.sems[k] = es.enter_context(nc.semaphore("d_%s_%d" % (q, i)))
                lst.append(k)
            self.dq[q] = {"keys": lst, "n": 0, "vals": [0] * self.NDMASEM}

    def _deps(self, reads, writes, extra=()):
        deps = list(extra)
        for b in reads:
            if b.lw is not None:
                deps.append(b.lw)
        for b in writes:
            if b.lw is not None:
                deps.append(b.lw)
            deps.extend(b.rd)
        return deps

    def _prune(self, e, deps):
        best = {}
        for (k, v) in deps:
            if e.name == "pe" and k == ("e", "pe"):
                continue
            if e.waited.get(k, 0) >= v:
                continue
            if best.get(k, 0) < v:
                best[k] = v
        for k, v in best.items():
            e.waited[k] = v
        return list(best.items())

    def _commit(self, tok, reads, writes):
        for b in reads:
            b.rd.append(tok)
        for b in writes:
            b.lw = tok
            b.rd = []

    def op(self, eng, fn, reads=(), writes=()):
        e = self.E[eng]
        waits = self._prune(e, self._deps(reads, writes))
        e.count += 1
        tok = (("e", eng), e.count)
        e.ops.append((waits, fn, (("e", eng), 1)))
        self._commit(tok, reads, writes)
        return tok

    def dma(self, q, out, in_, reads=(), writes=()):
        e = self.E[q]
        d = self.dq[q]
        i = d["n"] % self.NDMASEM
        d["n"] += 1
        key = d["keys"][i]
        prev = d["vals"][i]
        extra = [(key, prev)] if prev > 0 else []
        waits = self._prune(e, self._deps(reads, writes, extra))
        d["vals"][i] = prev + 16
        tok = (key, prev + 16)
        e.ops.append((waits, (lambda t, out=out, in_=in_: t.dma_start(out=out, in_=in_)), (key, 16)))
        self._commit(tok, reads, writes)
        return tok

    def all_tokens(self):
        toks = []
        for n, e in self.E.items():
            if e.count:
                toks.append((("e", n), e.count))
        for q, d in self.dq.items():
            for k, v in zip(d["keys"], d["vals"]):
                if v:
                    toks.append((k, v))
        return toks

    def barrier(self):
        toks = self.all_tokens()
        for n, e in self.E.items():
            waits = self._prune(e, toks)
            if waits:
                e.ops.append((waits, None, None))

    def finish(self):
        self.barrier()
        nc, sems, E = self.nc, self.sems, self.E

        def replay(engname, engobj):
            for (waits, fn, inc) in E[engname].ops:
                for (k, v) in waits:
                    engobj.wait_ge(sems[k], v)
                if fn is not None:
                    fn(engobj).then_inc(sems[inc[0]], inc[1])

        with nc.Block() as block:
            @block.tensor
            def _(t):
                replay("pe", t)

            @block.scalar
            def _(t):
                replay("act", t)

            @block.vector
            def _(t):
                replay("dve", t)

            @block.gpsimd
            def _(t):
                replay("pool", t)

            @block.sync
            def _(t):
                replay("sp", t)


class Ctx:
    def __init__(self, cfg):
        self.cfg = cfg
        nc = bass.Bass("TRN2", target_bir_lowering=False)
        nc.dge_precook = False
        self.nc = nc
        self.es = ExitStack()
        self.P = Prog(nc, self.es)
        self.banks = [self.es.enter_context(nc.psum_tensor("bank%d" % i, [128, 512], F32)) for i in range(8)]
        self.bankb = [Buf() for _ in range(8)]
        self.ones = self.es.enter_context(nc.sbuf_tensor("ones", [128, 128], BF16))
        self.onesb = Buf()
        self.P.op("dve", lambda t: t.memset(self.ones[:], 1.0), writes=[self.onesb])
        self.xbufs = {}
        self.n_in = {}

    def din(self, name, shape, dt):
        return self.nc.dram_tensor(name, list(shape), dt, kind="ExternalInput").ap()

    def dout(self, name, shape, dt):
        return self.nc.dram_tensor(name, list(shape), dt, kind="ExternalOutput").ap()

    def dint(self, name, shape, dt):
        return self.nc.dram_tensor(name, list(shape), dt, kind="Internal").ap()

    def sb(self, es, name, shape, dt):
        return es.enter_context(self.nc.sbuf_tensor(name, list(shape), dt))

    def xb(self, name, dc, tt):
        k = (name, dc, tt)
        if k not in self.xbufs:
            self.xbufs[k] = Buf()
        return self.xbufs[k]


def alloc_dense(cx, es):
    cfg = cx.cfg
    W = {}
    NCH = max(cfg.FC, cfg.DC)
    W["hT"] = cx.sb(es, "hT", [128, cfg.DC, cfg.G], BF16)
    W["hTb"] = [Buf() for _ in range(cfg.G // 256)]
    W["actT"] = cx.sb(es, "actT", [128, NCH, cfg.G], BF16)
    W["actTb"] = [Buf() for _ in range(NCH)]
    W["w1"] = [cx.sb(es, "w1_%d" % i, [128, cfg.DC * 256], BF16) for i in range(2)]
    W["w1b"] = [Buf(), Buf()]
    wbsz = max(NCH * 128, 4 * cfg.DC * 128)
    W["wb"] = [cx.sb(es, "wb_%d" % i, [128, wbsz], BF16) for i in range(2)]
    W["wbb"] = [Buf(), Buf()]
    W["xn"] = cx.sb(es, "xn", [128, cfg.DC, 256], F32)
    W["xnb"] = Buf()
    W["sq"] = [cx.sb(es, "sq%d" % i, [128, 512], BF16) for i in range(2)]
    W["sqb"] = [Buf(), Buf()]
    W["rstd"] = cx.sb(es, "rstd", [128, 512], F32)
    W["rstdb"] = Buf()
    W["sg"] = [cx.sb(es, "sg%d" % i, [128, 512], F32) for i in range(2)]
    W["sgb"] = [Buf(), Buf()]
    W["xe"] = [cx.sb(es, "xe%d" % i, [128, 512], F32) for i in range(2)]
    W["xeb"] = [Buf(), Buf()]
    W["xo"] = [cx.sb(es, "xo%d" % i, [128, 512], F32) for i in range(2)]
    W["xob"] = [Buf(), Buf()]
    W["ot"] = [cx.sb(es, "ot%d" % i, [128, 512], BF16) for i in range(2)]
    W["otb"] = [Buf(), Buf()]
    W["wis"] = [cx.sb(es, "wis%d" % i, [128, 16], F32) for i in range(2)]
    W["wisb"] = [Buf(), Buf()]
    W["cnt"] = {"g": 0, "y": 0, "w1": 0, "wb": 0, "sq": 0, "sg": 0, "xe": 0, "ot": 0, "wis": 0}
    return W


def load_vec(cx, es, name, dram_ap, ncol):
    t = cx.sb(es, name, [128, ncol], F32)
    b = Buf()
    cx.P.dma("sp", t[:], dram_ap, writes=[b])
    return t, b


def rstd_from_ssq(cx, W, ssq_ap, ssq_buf, n, width):
    P = cx.P
    rs = W["rstd"]
    P.op("act", lambda t: t.activation(out=rs[:, :width], in_=ssq_ap, func=AF.Sqrt, scale=1.0 / n, bias=cx.epsb[:, 0:1]),
         reads=[ssq_buf, cx.epsbb], writes=[W["rstdb"]])
    P.op("dve", lambda t: t.reciprocal(out=rs[:, :width], in_=rs[:, :width]), reads=[W["rstdb"]], writes=[W["rstdb"]])


def norm_phase(cx, W, xname, xap, gain, gainb, tok0):
    cfg, P = cx.cfg, cx.P
    xv = xap.rearrange("(c p) t -> p c t", p=128)
    for q in range(cfg.G // 256):
        t0 = tok0 + q * 256
        tt = t0 // 512
        xn = W["xn"]
        P.dma("sp", xn[:], xv[:, :, t0:t0 + 256], reads=[cx.xb(xname, dc, tt) for dc in range(cfg.DC)], writes=[W["xnb"]])
        ssq = cx.banks[6]
        for c in range(cfg.DC):
            i = W["cnt"]["sq"] % 2
            W["cnt"]["sq"] += 1
            sq = W["sq"][i]
            P.op("act", lambda t, sq=sq, c=c: t.activation(out=sq[:, :256], in_=xn[:, c, :], func=AF.Square),
                 reads=[W["xnb"]], writes=[W["sqb"][i]])
            P.op("pe", lambda t, sq=sq, c=c: t.matmul(ssq[:, :256], lhsT=cx.ones[:], rhs=sq[:, :256], start=(c == 0), stop=(c == cfg.DC - 1)),
                 reads=[W["sqb"][i], cx.onesb], writes=[cx.bankb[6]])
        rstd_from_ssq(cx, W, ssq[:, :256], cx.bankb[6], cfg.D, 256)
        for c in range(cfg.DC):
            P.op("dve", lambda t, c=c, q=q: t.scalar_tensor_tensor(out=W["hT"][:, c, q * 256:(q + 1) * 256], in0=xn[:, c, :], scalar=gain[:, c:c + 1],
                                                                 in1=W["rstd"][:, :256], op0=ALU.mult, op1=ALU.mult),
                 reads=[W["xnb"], W["rstdb"], gainb], writes=[W["hTb"][q]])


def inproj_phase(cx, W, w1t):
    cfg, P = cx.cfg, cx.P
    ntt = cfg.G // 512
    for f in range(cfg.FC):
        wi = W["cnt"]["w1"] % 2
        W["cnt"]["w1"] += 1
        w1 = W["w1"][wi]
        P.dma("pool", w1[:], w1t[f], writes=[W["w1b"][wi]])
        for tt in range(ntt):
            gi = W["cnt"]["g"] % 2
            W["cnt"]["g"] += 1
            pg, pu = cx.banks[gi], cx.banks[2 + gi]
            hb = [W["hTb"][2 * tt], W["hTb"][2 * tt + 1]]
            for gu, pb, bi in ((0, pg, gi), (1, pu, 2 + gi)):
                for kc in range(cfg.DC):
                    P.op("pe", lambda t, pb=pb, w1=w1, kc=kc, gu=gu, tt=tt: t.matmul(
                        pb[:], lhsT=w1[:, kc * 256 + gu * 128: kc * 256 + gu * 128 + 128], rhs=W["hT"][:, kc, tt * 512:(tt + 1) * 512],
                        start=(kc == 0), stop=(kc == cfg.DC - 1)), reads=[W["w1b"][wi]] + hb, writes=[cx.bankb[bi]])
            si = W["cnt"]["sg"] % 2
            W["cnt"]["sg"] += 1
            sg = W["sg"][si]
            P.op("act", lambda t, sg=sg, pg=pg: t.activation(out=sg[:], in_=pg[:], func=AF.Silu), reads=[cx.bankb[gi]], writes=[W["sgb"][si]])
            P.op("dve", lambda t, sg=sg, pu=pu, f=f, tt=tt: t.tensor_tensor(out=W["actT"][:, f, tt * 512:(tt + 1) * 512], in0=sg[:], in1=pu[:], op=ALU.mult),
                 reads=[W["sgb"][si], cx.bankb[2 + gi]], writes=[W["actTb"][f]])


def linres_phase(cx, W, w2t, nch, xsname, xs, xdname, xd, tok0, scale):
    cfg, P = cx.cfg, cx.P
    ntt = cfg.G // 512
    for dc in range(cfg.DC):
        wi = W["cnt"]["wb"] % 2
        W["cnt"]["wb"] += 1
        wb = W["wb"][wi]
        P.dma("pool", wb[:, :nch * 128], w2t[dc], writes=[W["wbb"][wi]])
        for tt in range(ntt):
            yi = W["cnt"]["y"] % 2
            W["cnt"]["y"] += 1
            py = cx.banks[4 + yi]
            gt = (tok0 + tt * 512) // 512
            ei = W["cnt"]["xe"] % 2
            W["cnt"]["xe"] += 1
            xe, xo = W["xe"][ei], W["xo"][ei]
            P.dma("sp", xe[:], xs[dc * 128:(dc + 1) * 128, tok0 + tt * 512: tok0 + (tt + 1) * 512],
                  reads=[cx.xb(xsname, dc, gt)], writes=[W["xeb"][ei]])
            for f in range(nch):
                P.op("pe", lambda t, py=py, wb=wb, f=f, tt=tt: t.matmul(py[:], lhsT=wb[:, f * 128:(f + 1) * 128], rhs=W["actT"][:, f, tt * 512:(tt + 1) * 512],
                                                                      start=(f == 0), stop=(f == nch - 1)),
                     reads=[W["wbb"][wi], W["actTb"][f]], writes=[cx.bankb[4 + yi]])
            P.op("dve", lambda t, py=py, xe=xe, xo=xo: t.scalar_tensor_tensor(out=xo[:], in0=py[:], scalar=float(scale), in1=xe[:], op0=ALU.mult, op1=ALU.add),
                 reads=[cx.bankb[4 + yi], W["xeb"][ei]], writes=[W["xob"][ei]])
            P.dma("sp", xd[dc * 128:(dc + 1) * 128, tok0 + tt * 512: tok0 + (tt + 1) * 512], xo[:],
                  reads=[W["xob"][ei]], writes=[cx.xb(xdname, dc, gt)])


def load_actT(cx, W, src, nch, tok0):
    cfg, P = cx.cfg, cx.P
    sv = src.rearrange("(c p) t -> p c t", p=128)
    for c in range(nch):
        P.dma("sp", W["actT"][:, c, :], sv[:, c, tok0:tok0 + cfg.G], writes=[W["actTb"][c]])


def proj_phase(cx, W, wpt, gains, outs, tok0):
    cfg, P = cx.cfg, cx.P
    ntt = cfg.G // 512
    nsub = cfg.G // 128
    allh = W["hTb"]
    kinds = {}
    for h in range(cfg.HA):
        kinds[cfg.o_qa // 128 + h] = ("n", "qa", h, 0)
        kinds[cfg.o_ka // 128 + h] = ("n", "ka", h, 1)
    for h in range(cfg.HB):
        kinds[cfg.o_qb // 128 + h] = ("n", "qb", h, 2)
        kinds[cfg.o_kb // 128 + h] = ("n", "kb", h, 3)
    for j in range(cfg.NQI):
        kinds[cfg.o_qi // 128 + j] = ("p", "qi", j, 128)
    kinds[cfg.o_ki // 128] = ("p", "ki", 0, 64)
    for v0, nm, wdt in ((cfg.o_va // 128, "va", cfg.WA), (cfg.o_vb // 128, "vb", cfg.WB)):
        for j in range(wdt // 128):
            kinds[v0 + j] = ("v", nm, j, 0)
    gain_t, gain_b = gains
    for cc0 in range(0, cfg.NCC, 4):
        ncl = min(4, cfg.NCC - cc0)
        wi = W["cnt"]["wb"] % 2
        W["cnt"]["wb"] += 1
        wb = W["wb"][wi]
        wbv = wb[:, :4 * cfg.DC * 128].rearrange("p (j k c) -> p j k c", j=4, k=cfg.DC)
        P.dma("pool", wbv[:, :ncl], wpt[cc0:cc0 + ncl].rearrange("j p (k c) -> p j k c", k=cfg.DC), writes=[W["wbb"][wi]])
        j = 0
        while j < ncl:
            cc = cc0 + j
            kind = kinds[cc]
            if kind[0] == "v":
                j1 = j
                while j1 < ncl and kinds[cc0 + j1][0] == "v" and kinds[cc0 + j1][1] == kind[1]:
                    j1 += 1
                nv = j1 - j
                for sub in range(nsub):
                    gi = W["cnt"]["g"] % 2
                    W["cnt"]["g"] += 1
                    pb = cx.banks[gi]
                    for kc in range(cfg.DC):
                        P.op("pe", lambda t, pb=pb, kc=kc, sub=sub, j=j, nv=nv, wbv=wbv: t.matmul(
                            pb[:, :nv * 128], lhsT=W["hT"][:, kc, sub * 128:(sub + 1) * 128], rhs=wbv[:, j:j + nv, kc, :],
                            start=(kc == 0), stop=(kc == cfg.DC - 1)), reads=[W["wbb"][wi], allh[sub // 2]], writes=[cx.bankb[gi]])
                    oi = W["cnt"]["ot"] % 2
                    W["cnt"]["ot"] += 1
                    ot = W["ot"][oi]
                    P.op("act", lambda t, ot=ot, pb=pb, nv=nv: t.activation(out=ot[:, :nv * 128], in_=pb[:, :nv * 128], func=AF.Copy),
                         reads=[cx.bankb[gi]], writes=[W["otb"][oi]])
                    c0 = kind[2] * 128
                    P.dma("sp", outs[kind[1]][tok0 + sub * 128: tok0 + (sub + 1) * 128, c0:c0 + nv * 128], ot[:, :nv * 128], reads=[W["otb"][oi]])
                j = j1
                continue
            M = 128 if kind[0] == "n" else kind[3]
            for tt in range(ntt):
                gi = W["cnt"]["g"] % 2
                W["cnt"]["g"] += 1
                pb = cx.banks[gi]
                hb = [allh[2 * tt], allh[2 * tt + 1]]
                for kc in range(cfg.DC):
                    P.op("pe", lambda t, pb=pb, kc=kc, tt=tt, j=j, M=M, wbv=wbv: t.matmul(
                        pb[:M, :], lhsT=wbv[:, j, kc, :M], rhs=W["hT"][:, kc, tt * 512:(tt + 1) * 512],
                        start=(kc == 0), stop=(kc == cfg.DC - 1)), reads=[W["wbb"][wi]] + hb, writes=[cx.bankb[gi]])
                oi = W["cnt"]["ot"] % 2
                W["cnt"]["ot"] += 1
                ot = W["ot"][oi]
                tsl = slice(tok0 + tt * 512, tok0 + (tt + 1) * 512)
                if kind[0] == "n":
                    si = W["cnt"]["sq"] % 2
                    W["cnt"]["sq"] += 1
                    sq = W["sq"][si]
                    P.op("act", lambda t, sq=sq, pb=pb: t.activation(out=sq[:], in_=pb[:], func=AF.Square), reads=[cx.bankb[gi]], writes=[W["sqb"][si]])
                    ssq = cx.banks[6]
                    P.op("pe", lambda t, sq=sq: t.matmul(ssq[:], lhsT=cx.ones[:], rhs=sq[:], start=True, stop=True),
                         reads=[W["sqb"][si], cx.onesb], writes=[cx.bankb[6]])
                    rstd_from_ssq(cx, W, ssq[:], cx.bankb[6], 128, 512)
                    gcol = kind[3]
                    P.op("dve", lambda t, ot=ot, pb=pb, gcol=gcol: t.scalar_tensor_tensor(out=ot[:], in0=pb[:], scalar=gain_t[:, gcol:gcol + 1], in1=W["rstd"][:],
                                                                                        op0=ALU.mult, op1=ALU.mult),
                         reads=[cx.bankb[gi], W["rstdb"], gain_b], writes=[W["otb"][oi]])
                    P.dma("sp", outs[kind[1]][kind[2], :, tsl], ot[:], reads=[W["otb"][oi]])
                else:
                    P.op("act", lambda t, ot=ot, pb=pb, M=M: t.activation(out=ot[:M, :], in_=pb[:M, :], func=AF.Copy), reads=[cx.bankb[gi]], writes=[W["otb"][oi]])
                    if kind[1] == "qi":
                        P.dma("sp", outs["qi"][kind[2], :, tsl], ot[:], reads=[W["otb"][oi]])
                    else:
                        P.dma("sp", outs["ki"][:, tsl], ot[:64, :], reads=[W["otb"][oi]])
            if kind[1] == "ki":
                for sub in range(nsub):
                    gi = W["cnt"]["g"] % 2
                    W["cnt"]["g"] += 1
                    pb = cx.banks[gi]
                    for kc in range(cfg.DC):
                        P.op("pe", lambda t, pb=pb, kc=kc, sub=sub, j=j, wbv=wbv: t.matmul(
                            pb[:, :cfg.NIH], lhsT=W["hT"][:, kc, sub * 128:(sub + 1) * 128], rhs=wbv[:, j, kc, 64:64 + cfg.NIH],
                            start=(kc == 0), stop=(kc == cfg.DC - 1)), reads=[W["wbb"][wi], allh[sub // 2]], writes=[cx.bankb[gi]])
                    oi = W["cnt"]["wis"] % 2
                    W["cnt"]["wis"] += 1
                    ws = W["wis"][oi]
                    P.op("dve", lambda t, ws=ws, pb=pb: t.tensor_copy(out=ws[:, :cfg.NIH], in_=pb[:, :cfg.NIH]), reads=[cx.bankb[gi]], writes=[W["wisb"][oi]])
                    P.dma("sp", outs["wi"][tok0 + sub * 128: tok0 + (sub + 1) * 128, :], ws[:, :cfg.NIH], reads=[W["wisb"][oi]])
            j += 1


def common_consts(cx):
    es = cx.es
    cx.epsb = cx.sb(es, "epsb", [128, 1], F32)
    cx.epsbb = Buf()
    cx.P.op("dve", lambda t: t.memset(cx.epsb[:], EPS), writes=[cx.epsbb])


def declare_dense_inputs(cx, pre, with_ffn=True, with_proj=True):
    cfg = cx.cfg
    d = {}
    if with_ffn:
        d["g1"] = cx.din(pre + "g1", [128, cfg.DC], F32)
        d["w1t"] = cx.din(pre + "w1t", [cfg.FC, 128, cfg.DC * 256], F32)
        d["w2t"] = cx.din(pre + "w2t", [cfg.DC, 128, cfg.FC * 128], F32)
    if with_proj:
        d["gm"] = cx.din(pre + "gm", [128, cfg.DC], F32)
        d["wpt"] = cx.din(pre + "wpt", [cfg.NCC, 128, cfg.DC * 128], F32)
        d["hg"] = cx.din(pre + "hg", [128, 4], F32)
    return d


def declare_proj_outs(cx):
    cfg = cx.cfg
    o = {}
    o["qa"] = cx.dout("o_qa", [cfg.HA, 128, cfg.T], BF16)
    o["ka"] = cx.dout("o_ka", [cfg.HA, 128, cfg.T], BF16)
    o["va"] = cx.dout("o_va", [cfg.T, cfg.WA], BF16)
    o["qb"] = cx.dout("o_qb", [cfg.HB, 128, cfg.T], BF16)
    o["kb"] = cx.dout("o_kb", [cfg.HB, 128, cfg.T], BF16)
    o["vb"] = cx.dout("o_vb", [cfg.T, cfg.WB], BF16)
    o["qi"] = cx.dout("o_qi", [cfg.NQI, 128, cfg.T], BF16)
    o["ki"] = cx.dout("o_ki", [64, cfg.T], BF16)
    o["wi"] = cx.dout("o_wi", [cfg.T, cfg.NIH], F32)
    return o


def head_gains(cx, es, hg_ap):
    t, b = load_vec(cx, es, "hgs", hg_ap, 4)
    sc = 128.0 ** -0.5
    for col in (0, 2):
        cx.P.op("dve", lambda tt, col=col: tt.tensor_scalar(out=t[:, col:col + 1], in0=t[:, col:col + 1], scalar1=sc, scalar2=None, op0=ALU.mult),
                reads=[b], writes=[b])
    return t, b


def build_L1(cfg):
    cx = Ctx(cfg)
    common_consts(cx)
    x_in = cx.din("xT", [cfg.D, cfg.T], F32)
    di = declare_dense_inputs(cx, "")
    x_out = cx.dout("xT_out", [cfg.D, cfg.T], F32)
    outs = declare_proj_outs(cx)
    with ExitStack() as es2:
        W = alloc_dense(cx, es2)
        g1 = load_vec(cx, es2, "g1s", di["g1"], cfg.DC)
        gm = load_vec(cx, es2, "gms", di["gm"], cfg.DC)
        hg = head_gains(cx, es2, di["hg"])
        for g in range(cfg.T // cfg.G):
            tok0 = g * cfg.G
            norm_phase(cx, W, "xin", x_in, g1[0], g1[1], tok0)
            inproj_phase(cx, W, di["w1t"])
            linres_phase(cx, W, di["w2t"], cfg.FC, "xin", x_in, "xout", x_out, tok0, 0.5)
            norm_phase(cx, W, "xout", x_out, gm[0], gm[1], tok0)
            proj_phase(cx, W, di["wpt"], hg, outs, tok0)
        cx.P.finish()
    return cx


def tile_w1(w, cfg):
    a = w.reshape(cfg.DC, 128, 2, cfg.FC, 128).transpose(3, 1, 0, 2, 4)
    return np.ascontiguousarray(a).reshape(cfg.FC, 128, cfg.DC * 256)


def tile_w2(w, nch, cfg):
    a = w.reshape(nch, 128, cfg.DC, 128).transpose(2, 1, 0, 3)
    return np.ascontiguousarray(a).reshape(cfg.DC, 128, nch * 128)


def tile_wp(w, cfg):
    wpad = np.zeros((cfg.D, cfg.NCC * 128), np.float32)
    wpad[:, :cfg.PW] = w
    a = wpad.reshape(cfg.DC, 128, cfg.NCC, 128).transpose(2, 1, 0, 3)
    return np.ascontiguousarray(a).reshape(cfg.NCC, 128, cfg.DC * 128)


def vec_pc(v, cfg):
    return np.ascontiguousarray(v.reshape(cfg.DC, 128).T)


NBIS = 20
PEN = -30000.0


class DB:
    pass


class CtxA(Ctx):
    def __init__(self, cfg):
        self.cfg = cfg
        nc = bass.Bass("TRN2", target_bir_lowering=False)
        nc.dge_precook = False
        self.nc = nc
        self.es = ExitStack()
        self.P = Prog(nc, self.es)
        self.dbank = [self.es.enter_context(nc.psum_tensor("dbank%d" % i, [128, 1024], F32)) for i in range(4)]
        self.dbb = [Buf() for _ in range(4)]
        self.hb = [[Buf(), Buf()] for _ in range(4)]
        self.ones = self.es.enter_context(nc.sbuf_tensor("ones", [128, 128], BF16))
        self.onesb = Buf()
        self.P.op("dve", lambda t: t.memset(self.ones[:], 1.0), writes=[self.onesb])
        self.xbufs = {}


def dsa_phase(cx, es, I, out_mb):
    cfg, P = cx.cfg, cx.P
    HB, NIH, CPB = cfg.HB, cfg.NIH, cfg.CPB
    KT = 128 * CPB
    NQB = cfg.S // KT
    NN = 12 + CPB
    HW = HB * 128
    sb = lambda n, s, d: cx.sb(es, n, s, d)
    scores = sb("scores", [128, cfg.S], F32)
    scb = Buf()
    maskb = sb("maskb", [128, cfg.S], BF16)
    mkb = Buf()
    ki2 = sb("ki2", [128, cfg.S], BF16)
    ki2b = Buf()
    P.dma("sp", ki2[:], I["ki2"], writes=[ki2b])
    ident = sb("ident", [128, 128], BF16)
    identb = Buf()
    P.dma("sp", ident[:], I["ident"], writes=[identb])
    pen = sb("pen", [128, KT], F32)
    penb = Buf()
    qrel = sb("qrel", [128, 1], F32)
    P.dma("sp", qrel[:], I["qrel"], writes=[penb])
    P.dma("sp", pen[:], I["iota"], writes=[penb])
    P.op("dve", lambda t: t.tensor_scalar(out=pen[:], in0=pen[:], scalar1=qrel[:, 0:1], scalar2=PEN, op0=ALU.is_gt, op1=ALU.mult), reads=[penb], writes=[penb])
    cbt = sb("cbt", [128, HW], F32)
    cbb = Buf()
    P.dma("sp", cbt[:], I["cb"], writes=[cbb])
    qbt = [sb("qbt%d" % i, [128, HW], BF16) for i in range(2)]
    qbtb = [Buf(), Buf()]
    qit = [sb("qit%d" % i, [128, cfg.NQI * 128], BF16) for i in range(2)]
    qitb = [Buf(), Buf()]
    wit = [sb("wit%d" % i, [128, 3 * NIH], F32) for i in range(2)]
    witb = [Buf(), Buf()]
    R = [sb("R%d" % i, [128, KT], F32R) for i in range(4)]
    Rb = [Buf() for _ in range(4)]
    sm = sb("sm", [128, 8], F32)
    smb = Buf()
    sma = sb("sma", [128, 2], F32)
    smab = Buf()
    midb = Buf()
    mkb2 = Buf()
    Dg = [sb("Dg%d" % i, [128, NIH * 128], F32R) for i in range(2)]
    Dgb = [Buf(), Buf()]
    identf = sb("identf", [128, 128], F32)
    identfb = Buf()
    P.dma("sp", identf[:], I["identf"], writes=[identfb])
    kt_ = [sb("kt%d" % i, [128, HW], BF16) for i in range(2)]
    ktb = [Buf(), Buf()]
    vt_ = [sb("vt%d" % i, [128, HW], BF16) for i in range(2)]
    vtb = [Buf(), Buf()]
    E = [sb("E%d" % i, [128, HW], F32) for i in range(2)]
    Eb = [Buf(), Buf()]
    PT = [sb("PT%d" % i, [128, HW], BF16) for i in range(2)]
    PTb = [Buf(), Buf()]
    Ehb = [[Buf(), Buf()] for _ in range(2)]
    PThb = [[Buf(), Buf()] for _ in range(2)]
    tbt = [sb("tbt%d" % i, [128, HW], F32) for i in range(2)]
    tbb = [Buf(), Buf()]
    ob = sb("ob", [128, HW], BF16)
    obb = Buf()
    rl = sb("rl", [128, HW], F32)
    rlb = Buf()
    cnt = {"R": 0, "kv": 0, "E": 0, "tb": 0, "ps": 0, "pt": 0}
    for m in range(NQB):
        qi_ = m % 2
        P.dma("sp", qbt[qi_][:], I["qb"][m], writes=[qbtb[qi_]])
        P.dma("sp", qit[qi_][:], I["qi"][m], writes=[qitb[qi_]])
        wt = wit[qi_]
        P.dma("sp", wt[:, :NIH], I["wi"][m], writes=[witb[qi_]])
        P.op("dve", lambda t, wt=wt: t.scalar_tensor_tensor(out=wt[:, NIH:2 * NIH], in0=wt[:, :NIH], scalar=-1.0, in1=wt[:, :NIH], op0=ALU.mult, op1=ALU.max), reads=[witb[qi_]], writes=[witb[qi_]])
        P.op("dve", lambda t, wt=wt: t.tensor_scalar(out=wt[:, 2 * NIH:3 * NIH], in0=wt[:, :NIH], scalar1=0.0, scalar2=2.0, op0=ALU.is_ge, op1=ALU.mult), reads=[witb[qi_]], writes=[witb[qi_]])
        P.op("dve", lambda t, wt=wt: t.tensor_scalar(out=wt[:, 2 * NIH:3 * NIH], in0=wt[:, 2 * NIH:3 * NIH], scalar1=-1.0, scalar2=None, op0=ALU.add), reads=[witb[qi_]], writes=[witb[qi_]])
        L = KT * (m + 1)
        Dq = Dg[qi_]
        for h in range(NIH):
            P.op("dve", lambda t, Dq=Dq, wt=wt, h=h: t.tensor_scalar(out=Dq[:, h * 128:(h + 1) * 128], in0=identf[:], scalar1=wt[:, h:h + 1], scalar2=None, op0=ALU.mult),
                 reads=[identfb, witb[qi_]], writes=[Dgb[qi_]])
        for kt in range(m + 1):
            k0 = kt * KT
            accp = cx.dbank[1][:, 512:512 + KT]
            accb = cx.hb[1][1]
            for h in range(NIH):
                bi = cnt["ps"] % 3
                cnt["ps"] += 1
                pb = cx.dbank[bi // 2][:, (bi % 2) * 512:(bi % 2) * 512 + KT]
                pbb = cx.hb[bi // 2][bi % 2]
                p0 = (h % 2) * 64
                P.op("pe", lambda t, pb=pb, h=h, p0=p0, k0=k0, qi_=qi_: t.matmul(pb, lhsT=qit[qi_][p0:p0 + 64, (h // 2) * 128:(h // 2) * 128 + 128], rhs=ki2[p0:p0 + 64, k0:k0 + KT],
                                                                              start=True, stop=True), reads=[qitb[qi_], ki2b], writes=[pbb, cx.dbb[bi // 2]])
                ri = cnt["R"] % 4
                cnt["R"] += 1
                Rt = R[ri]
                P.op("act", lambda t, Rt=Rt, pb=pb: t.activation(out=Rt[:], in_=pb, func=AF.Relu), reads=[pbb], writes=[Rb[ri]])
                P.op("pe", lambda t, accp=accp, Dq=Dq, Rt=Rt, h=h: t.matmul(accp, lhsT=Dq[:, h * 128:(h + 1) * 128], rhs=Rt[:], start=(h == 0), stop=(h == NIH - 1)),
                     reads=[Dgb[qi_], Rb[ri]], writes=[accb, cx.dbb[1]])
            P.op("dve", lambda t, accp=accp, k0=k0: t.tensor_copy(out=scores[:, k0:k0 + KT], in_=accp), reads=[accb], writes=[scb])
        P.op("dve", lambda t, L=L: t.tensor_reduce(out=sm[:, 0:1], in_=scores[:, :L], axis=mybir.AxisListType.X, op=ALU.min), reads=[scb], writes=[smb])
        P.op("dve", lambda t, L=L: t.tensor_tensor(out=scores[:, L - KT:L], in0=scores[:, L - KT:L], in1=pen[:], op=ALU.add), reads=[scb, penb], writes=[scb])
        P.op("dve", lambda t, L=L: t.tensor_reduce(out=sm[:, 5:6], in_=scores[:, :L], axis=mybir.AxisListType.X, op=ALU.max), reads=[scb, smb], writes=[smb])
        P.op("dve", lambda t: t.scalar_tensor_tensor(out=sm[:, 1:2], in0=sm[:, 5:6], scalar=1e-6, in1=sm[:, 0:1], op0=ALU.add, op1=ALU.subtract), reads=[smb], writes=[smb])
        La = max(64, (int(L * 0.42) // 64) * 64)
        nact = L - La
        for it in range(NBIS):
            f = 2.0 ** -(it + 1)
            P.op("dve", lambda t, f=f: t.scalar_tensor_tensor(out=sm[:, 2:3], in0=sm[:, 1:2], scalar=f, in1=sm[:, 0:1], op0=ALU.mult, op1=ALU.add), reads=[smb], writes=[smb, midb])
            P.op("act", lambda t, L=L, La=La: t.activation(out=maskb[:, La:L], in_=scores[:, La:L], func=AF.Sign, scale=-1.0, bias=sm[:, 2:3], accum_out=sma[:, 0:1]),
                 reads=[scb, midb], writes=[mkb2, smab])
            P.op("dve", lambda t, La=La: t.tensor_scalar(out=maskb[:, :La], in0=scores[:, :La], scalar1=sm[:, 2:3], scalar2=None, op0=ALU.is_ge, op1=ALU.add, accum_out=sm[:, 3:4]),
                 reads=[scb, smb], writes=[mkb, smb])
            P.op("dve", lambda t: t.scalar_tensor_tensor(out=sm[:, 3:4], in0=sma[:, 0:1], scalar=-0.5, in1=sm[:, 3:4], op0=ALU.mult, op1=ALU.add), reads=[smb, smab], writes=[smb])
            P.op("dve", lambda t, f=f, nact=nact: t.tensor_scalar(out=sm[:, 4:5], in0=sm[:, 3:4], scalar1=255.5 - 0.5 * nact, scalar2=f, op0=ALU.is_ge, op1=ALU.mult), reads=[smb], writes=[smb])
            P.op("dve", lambda t: t.scalar_tensor_tensor(out=sm[:, 0:1], in0=sm[:, 4:5], scalar=sm[:, 1:2], in1=sm[:, 0:1], op0=ALU.mult, op1=ALU.add), reads=[smb], writes=[smb])
        P.op("dve", lambda t, L=L: t.tensor_scalar(out=maskb[:, :L], in0=scores[:, :L], scalar1=sm[:, 0:1], scalar2=None, op0=ALU.is_ge), reads=[scb, smb], writes=[mkb, mkb2])
        nkb = CPB * (m + 1)
        oacc, lacc, sbk, mtb = cx.dbank[2], cx.dbank[3], cx.dbank[0], cx.dbank[1]
        mt16 = mtb[:].bitcast(BF16)
        for kb in range(nkb):
            ki_ = cnt["kv"] % 2
            cnt["kv"] += 1
            P.dma("sp", kt_[ki_][:], I["kb"][kb], writes=[ktb[ki_]])
            P.dma("sp", vt_[ki_][:], I["vb"][kb], writes=[vtb[ki_]])
            mi = kb % 2
            mts = mt16[:, mi * 1024:mi * 1024 + 128]
            P.op("pe", lambda t, mts=mts, kb=kb: t.transpose(out=mts, in_=maskb[:, kb * 128:(kb + 1) * 128], identity=ident[:]),
                 reads=[mkb, mkb2, identb], writes=[cx.hb[1][mi]])
            ei = cnt["E"] % 2
            cnt["E"] += 1
            Et, PTt = E[ei], PT[ei]
            delta = (nkb - 1) - kb
            tb = None
            if delta < NN:
                ti = cnt["tb"] % 2
                cnt["tb"] += 1
                tb = tbt[ti]
                P.dma("sp", tb[:], I["tb"][delta], writes=[tbb[ti]])
                P.op("dve", lambda t, tb=tb: t.tensor_tensor(out=tb[:], in0=tb[:], in1=cbt[:], op=ALU.subtract), reads=[tbb[ti], cbb], writes=[tbb[ti]])
                P.op("act", lambda t, tb=tb: t.activation(out=tb[:], in_=tb[:], func=AF.Exp), reads=[tbb[ti]], writes=[tbb[ti]])
            for hh in range((HB + 3) // 4):
                nh = min(4, HB - 4 * hh)
                c0, c1 = hh * 512, hh * 512 + nh * 128
                for h in range(4 * hh, 4 * hh + nh):
                    P.op("pe", lambda t, h=h, ki_=ki_, qi_=qi_: t.matmul(sbk[:, h * 128:(h + 1) * 128], lhsT=kt_[ki_][:, h * 128:(h + 1) * 128], rhs=qbt[qi_][:, h * 128:(h + 1) * 128],
                                                                      start=True, stop=True), reads=[ktb[ki_], qbtb[qi_]], writes=[cx.hb[0][hh]])
                P.op("act", lambda t, Et=Et, c0=c0, c1=c1: t.activation(out=Et[:, c0:c1], in_=sbk[:, c0:c1], func=AF.Exp), reads=[cx.hb[0][hh]], writes=[Ehb[ei][hh]])
                if tb is not None:
                    P.op("dve", lambda t, tb=tb, Et=Et, c0=c0, c1=c1: t.tensor_tensor(out=Et[:, c0:c1], in0=Et[:, c0:c1], in1=tb[:, c0:c1], op=ALU.mult),
                         reads=[tbb[ti], Ehb[ei][hh]], writes=[Ehb[ei][hh]])
                mtbc = mts.unsqueeze(1).to_broadcast([128, nh, 128])
                P.op("dve", lambda t, Et=Et, PTt=PTt, mtbc=mtbc, c0=c0, c1=c1, nh=nh: t.tensor_tensor(out=PTt[:, c0:c1].rearrange("p (h t) -> p h t", h=nh), in0=Et[:, c0:c1].rearrange("p (h t) -> p h t", h=nh), in1=mtbc, op=ALU.mult),
                     reads=[Ehb[ei][hh], cx.hb[1][mi]], writes=[PThb[ei][hh]])
                for h in range(4 * hh, 4 * hh + nh):
                    P.op("pe", lambda t, h=h, ki_=ki_, PTt=PTt, kb=kb, nkb=nkb: t.matmul(oacc[:, h * 128:(h + 1) * 128], lhsT=vt_[ki_][:, h * 128:(h + 1) * 128], rhs=PTt[:, h * 128:(h + 1) * 128],
                                                                                      start=(kb == 0 and h % 4 == 0), stop=(kb == nkb - 1), skip_group_check=True), reads=[vtb[ki_], PThb[ei][hh]], writes=[cx.dbb[2]])
                P.op("pe", lambda t, PTt=PTt, kb=kb, c0=c0, c1=c1, nkb=nkb: t.matmul(lacc[:, c0:c1], lhsT=cx.ones[:], rhs=PTt[:, c0:c1], start=(kb == 0), stop=(kb == nkb - 1)),
                     reads=[cx.onesb, PThb[ei][hh]], writes=[cx.dbb[3]])
        P.op("dve", lambda t: t.reciprocal(out=rl[:], in_=lacc[:, :HW]), reads=[cx.dbb[3]], writes=[rlb])
        P.op("dve", lambda t: t.tensor_tensor(out=ob[:], in0=oacc[:, :HW], in1=rl[:], op=ALU.mult), reads=[cx.dbb[2], rlb], writes=[obb])
        P.dma("sp", out_mb[:, :, m * 128:(m + 1) * 128], ob[:].rearrange("p (h t) -> p h t", h=HB), reads=[obb])


def dil_phase(cx, es, I, out_ma):
    cfg, P = cx.cfg, cx.P
    HA = cfg.HA
    HW = HA * 128
    sb = lambda n, s, d: cx.sb(es, n, s, d)
    npf = sb("npf", [128, 1], F32)
    npfb = Buf()
    P.dma("sp", npf[:], I["npf"], writes=[npfb])
    EBA = [[sb("eba%d_%d" % (br, v), [128, HW], F32) for v in range(3)] for br in range(3)]
    ebab = Buf()
    tmpv = sb("aE0", [128, HW], F32)
    for br in range(3):
        for pc in range(2):
            e_ = EBA[br][pc]
            P.dma("sp", e_[:], I["tb"][br, pc], writes=[ebab])
            P.dma("sp", tmpv[:], I["vm"][br, pc], reads=[ebab], writes=[ebab])
            P.op("act", lambda t, e_=e_: t.activation(out=e_[:], in_=e_[:], func=AF.Exp), reads=[ebab], writes=[ebab])
            P.op("dve", lambda t, e_=e_: t.tensor_tensor(out=e_[:], in0=e_[:], in1=tmpv[:], op=ALU.mult), reads=[ebab], writes=[ebab])
        P.op("dve", lambda t, br=br: t.tensor_scalar(out=EBA[br][2][:], in0=EBA[br][0][:], scalar1=npf[:, 0:1], scalar2=None, op0=ALU.mult), reads=[ebab, npfb], writes=[ebab])
    oaccT = sb("oaccT", [128, HA, 2048], F32)
    laccT = sb("laccT", [128, HA, 2048], F32)
    accb = Buf()
    qt = [sb("aq%d" % i, [128, HW], BF16) for i in range(2)]
    kt = [sb("ak%d" % i, [128, 2, HW], BF16) for i in range(2)]
    vt = [sb("av%d" % i, [128, 2, HW], BF16) for i in range(2)]
    qkvb = [Buf(), Buf()]
    E0 = tmpv
    E = [E0, E0]
    Eb0 = ebab
    Eb = [Eb0, Eb0]
    PT = [sb("aPT%d" % i, [128, HW], BF16) for i in range(2)]
    PTb = [Buf(), Buf()]
    ob = [sb("aob%d" % i, [128, HA, 256], BF16) for i in range(2)]
    obb = [Buf(), Buf()]
    n = 0
    ne = 0
    for u in range(2):
        for br, dil in enumerate((1, 4, 16)):
            span = 128 * dil
            for ti in range(16):
                np_, r = ti // dil, ti % dil
                bi = n % 2
                n += 1
                P.dma("sp", qt[bi][:], I["q"][br, u, ti], writes=[qkvb[bi]])
                P.dma("sp", kt[bi][:], I["k"][br, u, ti].rearrange("c p x -> p c x"), writes=[qkvb[bi]])
                P.dma("sp", vt[bi][:], I["v"][br, u, ti].rearrange("c p x -> p c x"), writes=[qkvb[bi]])
                sbk = cx.dbank[bi]
                oacc, lacc = cx.dbank[2], cx.dbank[3]
                for pc in range(2):
                    for h in range(HA):
                        P.op("pe", lambda t, h=h, bi=bi, pc=pc, sbk=sbk: t.matmul(sbk[:, h * 128:(h + 1) * 128], lhsT=kt[bi][:, pc, h * 128:(h + 1) * 128], rhs=qt[bi][:, h * 128:(h + 1) * 128],
                                                                              start=True, stop=True), reads=[qkvb[bi]], writes=[cx.dbb[bi]])
                    ei = ne % 2
                    ne += 1
                    Et, PTt = E[ei], PT[ei]
                    P.op("act", lambda t, Et=Et, sbk=sbk: t.activation(out=Et[:], in_=sbk[:, :HW], func=AF.Exp), reads=[cx.dbb[bi]], writes=[Eb[ei]])
                    ev = EBA[br][2] if (pc == 0 and u == 0 and np_ == 0) else EBA[br][pc]
                    P.op("dve", lambda t, Et=Et, PTt=PTt, ev=ev: t.tensor_tensor(out=PTt[:], in0=Et[:], in1=ev[:], op=ALU.mult), reads=[Eb[ei], ebab], writes=[PTb[ei]])
                    for h in range(HA):
                        P.op("pe", lambda t, h=h, bi=bi, pc=pc, PTt=PTt: t.matmul(oacc[:, h * 128:(h + 1) * 128], lhsT=vt[bi][:, pc, h * 128:(h + 1) * 128], rhs=PTt[:, h * 128:(h + 1) * 128],
                                                                              start=(pc == 0 and h % 4 == 0), stop=(pc == 1), skip_group_check=True), reads=[qkvb[bi], PTb[ei]], writes=[cx.dbb[2]])
                    for c0 in range(0, HW, 512):
                        c1 = min(HW, c0 + 512)
                        P.op("pe", lambda t, PTt=PTt, pc=pc, c0=c0, c1=c1: t.matmul(lacc[:, c0:c1], lhsT=cx.ones[:], rhs=PTt[:, c0:c1], start=(pc == 0), stop=(pc == 1)),
                             reads=[cx.onesb, PTb[ei]], writes=[cx.dbb[3]])
                s0 = np_ * span + r
                dst_o = oaccT[:, :, s0:s0 + 127 * dil + 1:dil]
                dst_l = laccT[:, :, s0:s0 + 127 * dil + 1:dil]
                ov = oacc[:, :HW].rearrange("p (h t) -> p h t", h=HA)
                lv = lacc[:, :HW].rearrange("p (h t) -> p h t", h=HA)
                if br == 0:
                    P.op("dve", lambda t, dst_o=dst_o, ov=ov: t.tensor_copy(out=dst_o, in_=ov), reads=[cx.dbb[2]], writes=[accb])
                    P.op("act", lambda t, dst_l=dst_l, lv=lv: t.activation(out=dst_l, in_=lv, func=AF.Copy), reads=[cx.dbb[3]], writes=[accb])
                else:
                    P.op("dve", lambda t, dst_o=dst_o, ov=ov: t.tensor_tensor(out=dst_o, in0=dst_o, in1=ov, op=ALU.add), reads=[cx.dbb[2], accb], writes=[accb])
                    P.op("dve", lambda t, dst_l=dst_l, lv=lv: t.tensor_tensor(out=dst_l, in0=dst_l, in1=lv, op=ALU.add), reads=[cx.dbb[3], accb], writes=[accb])
        for c in range(8):
            sl = slice(c * 256, (c + 1) * 256)
            P.op("dve", lambda t, sl=sl: t.reciprocal(out=laccT[:, :, sl], in_=laccT[:, :, sl]), reads=[accb], writes=[accb])
            oi = c % 2
            P.op("dve", lambda t, sl=sl, oi=oi: t.tensor_tensor(out=ob[oi][:], in0=oaccT[:, :, sl], in1=laccT[:, :, sl], op=ALU.mult), reads=[accb], writes=[obb[oi]])
            P.dma("sp", out_ma[:, :, u * 2048 + c * 256: u * 2048 + (c + 1) * 256], ob[oi][:], reads=[obb[oi]])


def build_L2(cfg):
    cx = CtxA(cfg)
    HA, HB, KT = cfg.HA, cfg.HB, 128 * cfg.CPB
    NQB = cfg.S // KT
    NKB = cfg.S // 128
    A = {"npf": cx.din("a_npf", [128, 1], F32), "tb": cx.din("a_tb", [3, 2, 128, HA * 128], F32), "vm": cx.din("a_vm", [3, 2, 128, HA * 128], F32),
         "q": cx.din("a_q", [3, 2, 16, 128, HA * 128], BF16), "k": cx.din("a_k", [3, 2, 16, 2, 128, HA * 128], BF16),
         "v": cx.din("a_v", [3, 2, 16, 2, 128, HA * 128], BF16)}
    Bd = {"ki2": cx.din("b_ki2", [128, cfg.S], BF16), "ident": cx.din("b_ident", [128, 128], BF16), "identf": cx.din("b_identf", [128, 128], F32), "qrel": cx.din("b_qrel", [128, 1], F32),
          "iota": cx.din("b_iota", [128, KT], F32), "cb": cx.din("b_cb", [128, HB * 128], F32), "qb": cx.din("b_qb", [NQB, 128, HB * 128], BF16),
          "qi": cx.din("b_qi", [NQB, 128, cfg.NQI * 128], BF16), "wi": cx.din("b_wi", [NQB, 128, cfg.NIH], F32),
          "kb": cx.din("b_kb", [NKB, 128, HB * 128], BF16), "vb": cx.din("b_vb", [NKB, 128, HB * 128], BF16),
          "tb": cx.din("b_tb", [12 + cfg.CPB, 128, HB * 128], F32)}
    o_ma = cx.dout("o_ma", [128, HA, cfg.T], BF16)
    o_mb = cx.dout("o_mb", [128, HB, NQB * 128], BF16)
    with ExitStack() as es2:
        dil_phase(cx, es2, A, o_ma)
        cx.P.barrier()
    with ExitStack() as es3:
        dsa_phase(cx, es3, Bd, o_mb)
        cx.P.finish()
    return cx


def rel_bucket_np(dist):
    dist = np.asarray(dist, np.int64)
    df = np.maximum(dist, 1).astype(np.float32)
    large = 16 + (np.log(df / np.float32(16)) / np.float32(np.log(2048 / 16)) * np.float32(16)).astype(np.int32)
    large = np.minimum(large, 31)
    return np.where(dist < 16, np.maximum(dist, 0), large).astype(np.int64)


def core_tokens(cfg, c):
    r = c % cfg.CPB
    u0, u1 = r, cfg.NU - 1 - r
    return np.concatenate([np.arange(2048) + 2048 * u0, np.arange(2048) + 2048 * u1])


def gather_global(cfg, outs_per_core):
    G = []
    for b in range(cfg.B):
        g = {"qa": np.zeros((cfg.HA, 128, cfg.S), NPBF), "ka": np.zeros((cfg.HA, 128, cfg.S), NPBF), "va": np.zeros((cfg.S, cfg.WA), NPBF),
             "qb": np.zeros((cfg.HB, 128, cfg.S), NPBF), "kb": np.zeros((cfg.HB, 128, cfg.S), NPBF), "vb": np.zeros((cfg.S, cfg.WB), NPBF),
             "qi": np.zeros((cfg.NQI, 128, cfg.S), NPBF), "ki": np.zeros((64, cfg.S), NPBF), "wi": np.zeros((cfg.S, cfg.NIH), np.float32)}
        for r in range(cfg.CPB):
            c = b * cfg.CPB + r
            pos = core_tokens(cfg, c)
            o = outs_per_core[c]
            for k in ("qa", "ka", "qb", "kb", "qi", "ki"):
                g[k][..., pos] = o["o_" + k]
            for k in ("va", "vb", "wi"):
                g[k][pos] = o["o_" + k]
        G.append(g)
    return G


def l2_inputs(cfg, G, rel_bias, c):
    b, r = c // cfg.CPB, c % cfg.CPB
    g = G[b]
    HA, HB, CPB = cfg.HA, cfg.HB, cfg.CPB
    rel_bias = np.asarray(rel_bias, np.float32)
    ins = {}
    units = (r, cfg.NU - 1 - r)
    aq = np.zeros((3, 2, 16, 128, HA * 128), NPBF)
    ak = np.zeros((3, 2, 16, 2, 128, HA * 128), NPBF)
    av = np.zeros((3, 2, 16, 2, 128, HA * 128), NPBF)
    atb = np.zeros((3, 2, 128, HA, 128), np.float32)
    avm = np.zeros((3, 2, 128, HA, 128), np.float32)
    jj, ii = np.meshgrid(np.arange(128), np.arange(128), indexing="ij")
    for br, dil in enumerate((1, 4, 16)):
        span = 128 * dil
        for pc in range(2):
            step = ii - jj + (128 if pc == 0 else 0)
            valid = (step >= 0) & (step <= 128) & ((jj >= ii) if pc == 0 else (jj <= ii))
            bk = rel_bucket_np(np.clip(step, 0, 128) * dil)
            atb[br, pc] = np.where(valid[:, None, :], rel_bias[bk][:, :, :HA].transpose(0, 2, 1), 0.0)
            avm[br, pc] = valid[:, None, :].astype(np.float32)
        for u, gu in enumerate(units):
            for ti in range(16):
                np_, r_ = ti // dil, ti % dil
                p = 2048 * gu + np_ * span + r_ + dil * np.arange(128)
                aq[br, u, ti] = g["qa"][:, :, p].transpose(1, 0, 2).reshape(128, HA * 128)
                ak[br, u, ti, 1] = g["ka"][:, :, p].transpose(1, 0, 2).reshape(128, HA * 128)
                av[br, u, ti, 1] = g["va"][p]
                pp = p - span
                if pp[0] >= 0:
                    ak[br, u, ti, 0] = g["ka"][:, :, pp].transpose(1, 0, 2).reshape(128, HA * 128)
                    av[br, u, ti, 0] = g["va"][pp]
    ins.update({"a_q": aq, "a_k": ak, "a_v": av, "a_tb": atb.reshape(3, 2, 128, HA * 128), "a_vm": avm.reshape(3, 2, 128, HA * 128),
                "a_npf": np.full((128, 1), 0.0 if r == 0 else 1.0, np.float32)})
    KT = 128 * CPB
    NQB = cfg.S // KT
    NKB = cfg.S // 128
    gq = CPB * np.arange(NQB) + r
    qb = g["qb"].reshape(HB, 128, NKB, 128)[:, :, gq]
    ins["b_qb"] = np.ascontiguousarray(qb.transpose(2, 1, 0, 3)).reshape(NQB, 128, HB * 128)
    qi = g["qi"].reshape(cfg.NQI, 128, NKB, 128)[:, :, gq]
    ins["b_qi"] = np.ascontiguousarray(qi.transpose(2, 1, 0, 3)).reshape(NQB, 128, cfg.NQI * 128)
    ins["b_wi"] = np.ascontiguousarray(g["wi"].reshape(NKB, 128, cfg.NIH)[gq])
    ins["b_ki2"] = np.ascontiguousarray(np.concatenate([g["ki"], g["ki"]], 0))
    ins["b_kb"] = np.ascontiguousarray(g["kb"].reshape(HB, 128, NKB, 128).transpose(2, 1, 0, 3)).reshape(NKB, 128, HB * 128)
    ins["b_vb"] = np.ascontiguousarray(g["vb"].reshape(NKB, 128, HB * 128))
    ins["b_qrel"] = (r * 128 + np.arange(128, dtype=np.float32)).reshape(128, 1)
    ins["b_iota"] = np.ascontiguousarray(np.broadcast_to(np.arange(KT, dtype=np.float32), (128, KT)))
    ins["b_cb"] = np.ascontiguousarray(np.broadcast_to(rel_bias[31, HA:HA + HB][None, :, None], (128, HB, 128))).reshape(128, HB * 128)
    NN = 12 + CPB
    tb = np.zeros((NN, 128, HB, 128), np.float32)
    for d in range(NN):
        dist = 128 * (d - (CPB - 1 - r)) + ii - jj
        tb[d] = rel_bias[rel_bucket_np(np.maximum(dist, 0))][:, :, HA:HA + HB].transpose(0, 2, 1)
    ins["b_tb"] = tb.reshape(NN, 128, HB * 128)
    ins["b_ident"] = np.eye(128, dtype=np.float32).astype(NPBF)
    ins["b_identf"] = np.eye(128, dtype=np.float32)
    return ins


def scatter_mix(cfg, res_per_core):
    M = []
    KT = 128 * cfg.CPB
    NQB = cfg.S // KT
    for b in range(cfg.B):
        mix = np.zeros((cfg.S, cfg.WA + cfg.WB), NPBF)
        for r in range(cfg.CPB):
            c = b * cfg.CPB + r
            pos = core_tokens(cfg, c)
            ma = res_per_core[c]["o_ma"]
            mix[pos, :cfg.WA] = ma.transpose(2, 1, 0).reshape(cfg.T, cfg.WA)
            mb = res_per_core[c]["o_mb"].reshape(128, cfg.HB, NQB, 128)
            gq = cfg.CPB * np.arange(NQB) + r
            posb = (gq[:, None] * 128 + np.arange(128)[None, :]).reshape(-1)
            mix[posb, cfg.WA:] = mb.transpose(2, 3, 1, 0).reshape(NQB * 128, cfg.WB)
        M.append(mix)
    return M


def build_L3(cfg, with_next=True):
    cx = Ctx(cfg)
    common_consts(cx)
    NM = (cfg.WA + cfg.WB) // 128
    x_in = cx.din("xT", [cfg.D, cfg.T], F32)
    mixT = cx.din("mixT", [NM * 128, cfg.T], BF16)
    wot = cx.din("wot", [cfg.DC, 128, NM * 128], F32)
    f2 = declare_dense_inputs(cx, "f2_", with_proj=False)
    xa = cx.dint("xa", [cfg.D, cfg.T], F32)
    xb = cx.dout("xT_mid", [cfg.D, cfg.T], F32)
    if with_next:
        nx = declare_dense_inputs(cx, "nx_")
        xc = cx.dout("xT_out", [cfg.D, cfg.T], F32)
        outs = declare_proj_outs(cx)
    with ExitStack() as es2:
        W = alloc_dense(cx, es2)
        g2 = load_vec(cx, es2, "g2s", f2["g1"], cfg.DC)
        if with_next:
            g1 = load_vec(cx, es2, "g1s", nx["g1"], cfg.DC)
            gm = load_vec(cx, es2, "gms", nx["gm"], cfg.DC)
            hg = head_gains(cx, es2, nx["hg"])
        for g in range(cfg.T // cfg.G):
            tok0 = g * cfg.G
            load_actT(cx, W, mixT, NM, tok0)
            linres_phase(cx, W, wot, NM, "xin", x_in, "xa", xa, tok0, 1.0)
            norm_phase(cx, W, "xa", xa, g2[0], g2[1], tok0)
            inproj_phase(cx, W, f2["w1t"])
            linres_phase(cx, W, f2["w2t"], cfg.FC, "xa", xa, "xb", xb, tok0, 0.5)
            if not with_next:
                continue
            norm_phase(cx, W, "xb", xb, g1[0], g1[1], tok0)
            inproj_phase(cx, W, nx["w1t"])
            linres_phase(cx, W, nx["w2t"], cfg.FC, "xb", xb, "xc", xc, tok0, 0.5)
            norm_phase(cx, W, "xc", xc, gm[0], gm[1], tok0)
            proj_phase(cx, W, nx["wpt"], hg, outs, tok0)
        cx.P.finish()
    return cx


def _run(cx, in_maps, n):
    res = run_bass_kernel_spmd(cx.nc, in_maps, core_ids=list(range(n)))
    cx.es.close()
    return res.results


def layer_dense_inputs(cfg, inp, l, pre, with_ffn1=True):
    d = {}
    if with_ffn1:
        d[pre + "g1"] = vec_pc(inp["norm_ffn1"][l], cfg)
        d[pre + "w1t"] = tile_w1(inp["w_ffn1_in"][l], cfg)
        d[pre + "w2t"] = tile_w2(inp["w_ffn1_out"][l], cfg.FC, cfg)
    d[pre + "gm"] = vec_pc(inp["norm_mix"][l], cfg)
    d[pre + "wpt"] = tile_wp(inp["w_in"][l], cfg)
    d[pre + "hg"] = np.ascontiguousarray(np.stack([inp["q_norm_a"][l], inp["k_norm_a"][l], inp["q_norm_b"][l], inp["k_norm_b"][l]], 1))
    return d


def run_model(cfg, inp):
    inp = {k: np.asarray(v) for k, v in inp.items()}
    n = cfg.NCORES
    x = inp["x"]
    xT = [np.ascontiguousarray(x[c // cfg.CPB][core_tokens(cfg, c)].T) for c in range(n)]
    cx = build_L1(cfg)
    d0 = layer_dense_inputs(cfg, inp, 0, "")
    res = _run(cx, [dict(d0, xT=xT[c]) for c in range(n)], n)
    cxa = build_L2(cfg)
    cx3 = None
    for l in range(cfg.DEPTH):
        G = gather_global(cfg, res)
        resa = _run(cxa, [l2_inputs(cfg, G, inp["rel_bias"], c) for c in range(n)], n)
        if l + 1 < cfg.DEPTH:
            cxa = build_L2(cfg)
        mix = scatter_mix(cfg, resa)
        last = (l + 1 == cfg.DEPTH)
        cx3 = build_L3(cfg, with_next=not last)
        d3 = {} if last else layer_dense_inputs(cfg, inp, l + 1, "nx_")
        d3["wot"] = tile_w2(inp["w_out"][l], (cfg.WA + cfg.WB) // 128, cfg)
        d3["f2_g1"] = vec_pc(inp["norm_ffn2"][l], cfg)
        d3["f2_w1t"] = tile_w1(inp["w_ffn2_in"][l], cfg)
        d3["f2_w2t"] = tile_w2(inp["w_ffn2_out"][l], cfg.FC, cfg)
        ims = []
        for c in range(n):
            xin = res[c]["xT_out"]
            mt = np.ascontiguousarray(mix[c // cfg.CPB][core_tokens(cfg, c)].T)
            ims.append(dict(d3, xT=xin, mixT=mt))
        res = _run(cx3, ims, n)
    out = np.zeros((cfg.B, cfg.S, cfg.D), np.float32)
    for c in range(n):
        out[c // cfg.CPB, core_tokens(cfg, c)] = res[c]["xT_mid"].T
    return out


def kernel(**inputs):
    return run_model(Cfg(), inputs)
```

```python
import numpy as np
from contextlib import ExitStack
import ml_dtypes
import concourse.bass as bass
import concourse.mybir as mybir
from concourse.bass_utils import run_bass_kernel_spmd

F32 = mybir.dt.float32
F32R = mybir.dt.float32r
BF16 = mybir.dt.bfloat16
ALU = mybir.AluOpType
AF = mybir.ActivationFunctionType
NPBF = ml_dtypes.bfloat16
EPS = 1e-6


class Cfg:
    def __init__(s, D=2048, F=5632, HA=8, HB=8, NIH=16, B=2, S=16384, CPB=4, DEPTH=2):
        s.D, s.F, s.HA, s.HB, s.NIH, s.B, s.S, s.CPB, s.DEPTH = D, F, HA, HB, NIH, B, S, CPB, DEPTH
        s.DC, s.FC = D // 128, F // 128
        s.NCORES = B * CPB
        s.UNIT = 2048
        s.NU = S // s.UNIT
        assert s.NU == 2 * CPB
        s.T = 2 * s.UNIT
        s.G = 1024
        s.WA, s.WB, s.QI = HA * 128, HB * 128, NIH * 64
        s.PW = 3 * s.WA + 3 * s.WB + s.QI + 64 + NIH
        s.NCC = (s.PW + 127) // 128
        s.o_qa, s.o_ka, s.o_va = 0, s.WA, 2 * s.WA
        s.o_qb, s.o_kb, s.o_vb = 3 * s.WA, 3 * s.WA + s.WB, 3 * s.WA + 2 * s.WB
        s.o_qi = 3 * s.WA + 3 * s.WB
        s.o_ki = s.o_qi + s.QI
        s.o_wi = s.o_ki + 64
        assert s.o_ki % 128 == 0
        s.NQI = s.QI // 128


class Buf:
    __slots__ = ("lw", "rd")

    def __init__(self):
        self.lw = None
        self.rd = []


class EngS:
    def __init__(self, name):
        self.name = name
        self.ops = []
        self.count = 0
        self.waited = {}


class Prog:
    NDMASEM = 12

    def __init__(self, nc, es):
        self.nc = nc
        self.E = {n: EngS(n) for n in ("pe", "act", "dve", "pool", "sp")}
        self.sems = {}
        for n in self.E:
            self.sems[("e", n)] = es.enter_context(nc.semaphore("s_" + n))
        self.dq = {}
        for q in ("sp", "pool"):
            lst = []
            for i in range(self.NDMASEM):
                k = ("d", q, i)
                self.sems[k] = es.enter_context(nc.semaphore("d_%s_%d" % (q, i)))
                lst.append(k)
            self.dq[q] = {"keys": lst, "n": 0, "vals": [0] * self.NDMASEM}

    def _deps(self, reads, writes, extra=()):
        deps = list(extra)
        for b in reads:
            if b.lw is not None:
                deps.append(b.lw)
        for b in writes:
            if b.lw is not None:
                deps.append(b.lw)
            deps.extend(b.rd)
        return deps

    def _prune(self, e, deps):
        best = {}
        for (k, v) in deps:
            if e.name == "pe" and k == ("e", "pe"):
                continue
            if e.waited.get(k, 0) >= v:
                continue
            if best.get(k, 0) < v:
                best[k] = v
        for k, v in best.items():
            e.waited[k] = v
        return list(best.items())

    def _commit(self, tok, reads, writes):
        for b in reads:
            b.rd.append(tok)
        for b in writes:
            b.lw = tok
            b.rd = []

    def op(self, eng, fn, reads=(), writes=()):
        e = self.E[eng]
        waits = self._prune(e, self._deps(reads, writes))
        e.count += 1
        tok = (("e", eng), e.count)
        e.ops.append((waits, fn, (("e", eng), 1)))
        self._commit(tok, reads, writes)
        return tok

    def dma(self, q, out, in_, reads=(), writes=()):
        e = self.E[q]
        d = self.dq[q]
        i = d["n"] % self.NDMASEM
        d["n"] += 1
        key = d["keys"][i]
        prev = d["vals"][i]
        extra = [(key, prev)] if prev > 0 else []
        waits = self._prune(e, self._deps(reads, writes, extra))
        d["vals"][i] = prev + 16
        tok = (key, prev + 16)
        e.ops.append((waits, (lambda t, out=out, in_=in_: t.dma_start(out=out, in_=in_)), (key, 16)))
        self._commit(tok, reads, writes)
        return tok

    def all_tokens(self):
        toks = []
        for n, e in self.E.items():
            if e.count:
                toks.append((("e", n), e.count))
        for q, d in self.dq.items():
            for k, v in zip(d["keys"], d["vals"]):
                if v:
                    toks.append((k, v))
        return toks

    def barrier(self):
        toks = self.all_tokens()
        for n, e in self.E.items():
            waits = self._prune(e, toks)
            if waits:
                e.ops.append((waits, None, None))

    def finish(self):
        self.barrier()
        nc, sems, E = self.nc, self.sems, self.E

        def replay(engname, engobj):
            for (waits, fn, inc) in E[engname].ops:
                for (k, v) in waits:
                    engobj.wait_ge(sems[k], v)
                if fn is not None:
                    fn(engobj).then_inc(sems[inc[0]], inc[1])

        with nc.Block() as block:
            @block.tensor
            def _(t):
                replay("pe", t)

            @block.scalar
            def _(t):
                replay("act", t)

            @block.vector
            def _(t):
                replay("dve", t)

            @block.gpsimd
            def _(t):
                replay("pool", t)

            @block.sync
            def _(t):
                replay("sp", t)


class Ctx:
    def __init__(self, cfg):
        self.cfg = cfg
        nc = bass.Bass("TRN2", target_bir_lowering=False)
        nc.dge_precook = False
        self.nc = nc
        self.es = ExitStack()
        self.P = Prog(nc, self.es)
        self.banks = [self.es.enter_context(nc.psum_tensor("bank%d" % i, [128, 512], F32)) for i in range(8)]
        self.bankb = [Buf() for _ in range(8)]
        self.ones = self.es.enter_context(nc.sbuf_tensor("ones", [128, 128], BF16))
        self.onesb = Buf()
        self.P.op("dve", lambda t: t.memset(self.ones[:], 1.0), writes=[self.onesb])
        self.xbufs = {}
        self.n_in = {}

    def din(self, name, shape, dt):
        return self.nc.dram_tensor(name, list(shape), dt, kind="ExternalInput").ap()

    def dout(self, name, shape, dt):
        return self.nc.dram_tensor(name, list(shape), dt, kind="ExternalOutput").ap()

    def dint(self, name, shape, dt):
        return self.nc.dram_tensor(name, list(shape), dt, kind="Internal").ap()

    def sb(self, es, name, shape, dt):
        return es.enter_context(self.nc.sbuf_tensor(name, list(shape), dt))

    def xb(self, name, dc, tt):
        k = (name, dc, tt)
        if k not in self.xbufs:
            self.xbufs[k] = Buf()
        return self.xbufs[k]


def alloc_dense(cx, es):
    cfg = cx.cfg
    W = {}
    NCH = max(cfg.FC, cfg.DC)
    W["hT"] = cx.sb(es, "hT", [128, cfg.DC, cfg.G], BF16)
    W["hTb"] = [Buf() for _ in range(cfg.G // 256)]
    W["actT"] = cx.sb(es, "actT", [128, NCH, cfg.G], BF16)
    W["actTb"] = [Buf() for _ in range(NCH)]
    W["w1"] = [cx.sb(es, "w1_%d" % i, [128, cfg.DC * 256], BF16) for i in range(2)]
    W["w1b"] = [Buf(), Buf()]
    wbsz = max(NCH * 128, 4 * cfg.DC * 128)
    W["wb"] = [cx.sb(es, "wb_%d" % i, [128, wbsz], BF16) for i in range(2)]
    W["wbb"] = [Buf(), Buf()]
    W["xn"] = cx.sb(es, "xn", [128, cfg.DC, 256], F32)
    W["xnb"] = Buf()
    W["sq"] = [cx.sb(es, "sq%d" % i, [128, 512], BF16) for i in range(2)]
    W["sqb"] = [Buf(), Buf()]
    W["rstd"] = cx.sb(es, "rstd", [128, 512], F32)
    W["rstdb"] = Buf()
    W["sg"] = [cx.sb(es, "sg%d" % i, [128, 512], F32) for i in range(2)]
    W["sgb"] = [Buf(), Buf()]
    W["xe"] = [cx.sb(es, "xe%d" % i, [128, 512], F32) for i in range(2)]
    W["xeb"] = [Buf(), Buf()]
    W["xo"] = [cx.sb(es, "xo%d" % i, [128, 512], F32) for i in range(2)]
    W["xob"] = [Buf(), Buf()]
    W["ot"] = [cx.sb(es, "ot%d" % i, [128, 512], BF16) for i in range(2)]
    W["otb"] = [Buf(), Buf()]
    W["wis"] = [cx.sb(es, "wis%d" % i, [128, 16], F32) for i in range(2)]
    W["wisb"] = [Buf(), Buf()]
    W["cnt"] = {"g": 0, "y": 0, "w1": 0, "wb": 0, "sq": 0, "sg": 0, "xe": 0, "ot": 0, "wis": 0}
    return W


def load_vec(cx, es, name, dram_ap, ncol):
    t = cx.sb(es, name, [128, ncol], F32)
    b = Buf()
    cx.P.dma("sp", t[:], dram_ap, writes=[b])
    return t, b


def rstd_from_ssq(cx, W, ssq_ap, ssq_buf, n, width):
    P = cx.P
    rs = W["rstd"]
    P.op("act", lambda t: t.activation(out=rs[:, :width], in_=ssq_ap, func=AF.Sqrt, scale=1.0 / n, bias=cx.epsb[:, 0:1]),
         reads=[ssq_buf, cx.epsbb], writes=[W["rstdb"]])
    P.op("dve", lambda t: t.reciprocal(out=rs[:, :width], in_=rs[:, :width]), reads=[W["rstdb"]], writes=[W["rstdb"]])


def norm_phase(cx, W, xname, xap, gain, gainb, tok0):
    cfg, P = cx.cfg, cx.P
    xv = xap.rearrange("(c p) t -> p c t", p=128)
    for q in range(cfg.G // 256):
        t0 = tok0 + q * 256
        tt = t0 // 512
        xn = W["xn"]
        P.dma("sp", xn[:], xv[:, :, t0:t0 + 256], reads=[cx.xb(xname, dc, tt) for dc in range(cfg.DC)], writes=[W["xnb"]])
        ssq = cx.banks[6]
        for c in range(cfg.DC):
            i = W["cnt"]["sq"] % 2
            W["cnt"]["sq"] += 1
            sq = W["sq"][i]
            P.op("act", lambda t, sq=sq, c=c: t.activation(out=sq[:, :256], in_=xn[:, c, :], func=AF.Square),
                 reads=[W["xnb"]], writes=[W["sqb"][i]])
            P.op("pe", lambda t, sq=sq, c=c: t.matmul(ssq[:, :256], lhsT=cx.ones[:], rhs=sq[:, :256], start=(c == 0), stop=(c == cfg.DC - 1)),
                 reads=[W["sqb"][i], cx.onesb], writes=[cx.bankb[6]])
        rstd_from_ssq(cx, W, ssq[:, :256], cx.bankb[6], cfg.D, 256)
        for c in range(cfg.DC):
            P.op("dve", lambda t, c=c, q=q: t.scalar_tensor_tensor(out=W["hT"][:, c, q * 256:(q + 1) * 256], in0=xn[:, c, :], scalar=gain[:, c:c + 1],
                                                                 in1=W["rstd"][:, :256], op0=ALU.mult, op1=ALU.mult),
                 reads=[W["xnb"], W["rstdb"], gainb], writes=[W["hTb"][q]])


def inproj_phase(cx, W, w1t):
    cfg, P = cx.cfg, cx.P
    ntt = cfg.G // 512
    for f in range(cfg.FC):
        wi = W["cnt"]["w1"] % 2
        W["cnt"]["w1"] += 1
        w1 = W["w1"][wi]
        P.dma("pool", w1[:], w1t[f], writes=[W["w1b"][wi]])
        for tt in range(ntt):
            gi = W["cnt"]["g"] % 2
            W["cnt"]["g"] += 1
            pg, pu = cx.banks[gi], cx.banks[2 + gi]
            hb = [W["hTb"][2 * tt], W["hTb"][2 * tt + 1]]
            for gu, pb, bi in ((0, pg, gi), (1, pu, 2 + gi)):
                for kc in range(cfg.DC):
                    P.op("pe", lambda t, pb=pb, w1=w1, kc=kc, gu=gu, tt=tt: t.matmul(
                        pb[:], lhsT=w1[:, kc * 256 + gu * 128: kc * 256 + gu * 128 + 128], rhs=W["hT"][:, kc, tt * 512:(tt + 1) * 512],
                        start=(kc == 0), stop=(kc == cfg.DC - 1)), reads=[W["w1b"][wi]] + hb, writes=[cx.bankb[bi]])
            si = W["cnt"]["sg"] % 2
            W["cnt"]["sg"] += 1
            sg = W["sg"][si]
            P.op("act", lambda t, sg=sg, pg=pg: t.activation(out=sg[:], in_=pg[:], func=AF.Silu), reads=[cx.bankb[gi]], writes=[W["sgb"][si]])
            P.op("dve", lambda t, sg=sg, pu=pu, f=f, tt=tt: t.tensor_tensor(out=W["actT"][:, f, tt * 512:(tt + 1) * 512], in0=sg[:], in1=pu[:], op=ALU.mult),
                 reads=[W["sgb"][si], cx.bankb[2 + gi]], writes=[W["actTb"][f]])


def linres_phase(cx, W, w2t, nch, xsname, xs, xdname, xd, tok0, scale):
    cfg, P = cx.cfg, cx.P
    ntt = cfg.G // 512
    for dc in range(cfg.DC):
        wi = W["cnt"]["wb"] % 2
        W["cnt"]["wb"] += 1
        wb = W["wb"][wi]
        P.dma("pool", wb[:, :nch * 128], w2t[dc], writes=[W["wbb"][wi]])
        for tt in range(ntt):
            yi = W["cnt"]["y"] % 2
            W["cnt"]["y"] += 1
            py = cx.banks[4 + yi]
            gt = (tok0 + tt * 512) // 512
            ei = W["cnt"]["xe"] % 2
            W["cnt"]["xe"] += 1
            xe, xo = W["xe"][ei], W["xo"][ei]
            P.dma("sp", xe[:], xs[dc * 128:(dc + 1) * 128, tok0 + tt * 512: tok0 + (tt + 1) * 512],
                  reads=[cx.xb(xsname, dc, gt)], writes=[W["xeb"][ei]])
            for f in range(nch):
                P.op("pe", lambda t, py=py, wb=wb, f=f, tt=tt: t.matmul(py[:], lhsT=wb[:, f * 128:(f + 1) * 128], rhs=W["actT"][:, f, tt * 512:(tt + 1) * 512],
                                                                      start=(f == 0), stop=(f == nch - 1)),
                     reads=[W["wbb"][wi], W["actTb"][f]], writes=[cx.bankb[4 + yi]])
            P.op("dve", lambda t, py=py, xe=xe, xo=xo: t.scalar_tensor_tensor(out=xo[:], in0=py[:], scalar=float(scale), in1=xe[:], op0=ALU.mult, op1=ALU.add),
                 reads=[cx.bankb[4 + yi], W["xeb"][ei]], writes=[W["xob"][ei]])
            P.dma("sp", xd[dc * 128:(dc + 1) * 128, tok0 + tt * 512: tok0 + (tt + 1) * 512], xo[:],
                  reads=[W["xob"][ei]], writes=[cx.xb(xdname, dc, gt)])


def load_actT(cx, W, src, nch, tok0):
    cfg, P = cx.cfg, cx.P
    sv = src.rearrange("(c p) t -> p c t", p=128)
    for c in range(nch):
        P.dma("sp", W["actT"][:, c, :], sv[:, c, tok0:tok0 + cfg.G], writes=[W["actTb"][c]])


def proj_phase(cx, W, wpt, gains, outs, tok0):
    cfg, P = cx.cfg, cx.P
    ntt = cfg.G // 512
    nsub = cfg.G // 128
    allh = W["hTb"]
    kinds = {}
    for h in range(cfg.HA):
        kinds[cfg.o_qa // 128 + h] = ("n", "qa", h, 0)
        kinds[cfg.o_ka // 128 + h] = ("n", "ka", h, 1)
    for h in range(cfg.HB):
        kinds[cfg.o_qb // 128 + h] = ("n", "qb", h, 2)
        kinds[cfg.o_kb // 128 + h] = ("n", "kb", h, 3)
    for j in range(cfg.NQI):
        kinds[cfg.o_qi // 128 + j] = ("p", "qi", j, 128)
    kinds[cfg.o_ki // 128] = ("p", "ki", 0, 64)
    for v0, nm, wdt in ((cfg.o_va // 128, "va", cfg.WA), (cfg.o_vb // 128, "vb", cfg.WB)):
        for j in range(wdt // 128):
            kinds[v0 + j] = ("v", nm, j, 0)
    gain_t, gain_b = gains
    for cc0 in range(0, cfg.NCC, 4):
        ncl = min(4, cfg.NCC - cc0)
        wi = W["cnt"]["wb"] % 2
        W["cnt"]["wb"] += 1
        wb = W["wb"][wi]
        wbv = wb[:, :4 * cfg.DC * 128].rearrange("p (j k c) -> p j k c", j=4, k=cfg.DC)
        P.dma("pool", wbv[:, :ncl], wpt[cc0:cc0 + ncl].rearrange("j p (k c) -> p j k c", k=cfg.DC), writes=[W["wbb"][wi]])
        j = 0
        while j < ncl:
            cc = cc0 + j
            kind = kinds[cc]
            if kind[0] == "v":
                j1 = j
                while j1 < ncl and kinds[cc0 + j1][0] == "v" and kinds[cc0 + j1][1] == kind[1]:
                    j1 += 1
                nv = j1 - j
                for sub in range(nsub):
                    gi = W["cnt"]["g"] % 2
                    W["cnt"]["g"] += 1
                    pb = cx.banks[gi]
                    for kc in range(cfg.DC):
                        P.op("pe", lambda t, pb=pb, kc=kc, sub=sub, j=j, nv=nv, wbv=wbv: t.matmul(
                            pb[:, :nv * 128], lhsT=W["hT"][:, kc, sub * 128:(sub + 1) * 128], rhs=wbv[:, j:j + nv, kc, :],
                            start=(kc == 0), stop=(kc == cfg.DC - 1)), reads=[W["wbb"][wi], allh[sub // 2]], writes=[cx.bankb[gi]])
                    oi = W["cnt"]["ot"] % 2
                    W["cnt"]["ot"] += 1
                    ot = W["ot"][oi]
                    P.op("act", lambda t, ot=ot, pb=pb, nv=nv: t.activation(out=ot[:, :nv * 128], in_=pb[:, :nv * 128], func=AF.Copy),
                         reads=[cx.bankb[gi]], writes=[W["otb"][oi]])
                    c0 = kind[2] * 128
                    P.dma("sp", outs[kind[1]][tok0 + sub * 128: tok0 + (sub + 1) * 128, c0:c0 + nv * 128], ot[:, :nv * 128], reads=[W["otb"][oi]])
                j = j1
                continue
            M = 128 if kind[0] == "n" else kind[3]
            for tt in range(ntt):
                gi = W["cnt"]["g"] % 2
                W["cnt"]["g"] += 1
                pb = cx.banks[gi]
                hb = [allh[2 * tt], allh[2 * tt + 1]]
                for kc in range(cfg.DC):
                    P.op("pe", lambda t, pb=pb, kc=kc, tt=tt, j=j, M=M, wbv=wbv: t.matmul(
                        pb[:M, :], lhsT=wbv[:, j, kc, :M], rhs=W["hT"][:, kc, tt * 512:(tt + 1) * 512],
                        start=(kc == 0), stop=(kc == cfg.DC - 1)), reads=[W["wbb"][wi]] + hb, writes=[cx.bankb[gi]])
                oi = W["cnt"]["ot"] % 2
                W["cnt"]["ot"] += 1
                ot = W["ot"][oi]
                tsl = slice(tok0 + tt * 512, tok0 + (tt + 1) * 512)
                if kind[0] == "n":
                    si = W["cnt"]["sq"] % 2
                    W["cnt"]["sq"] += 1
                    sq = W["sq"][si]
                    P.op("act", lambda t, sq=sq, pb=pb: t.activation(out=sq[:], in_=pb[:], func=AF.Square), reads=[cx.bankb[gi]], writes=[W["sqb"][si]])
                    ssq = cx.banks[6]
                    P.op("pe", lambda t, sq=sq: t.matmul(ssq[:], lhsT=cx.ones[:], rhs=sq[:], start=True, stop=True),
                         reads=[W["sqb"][si], cx.onesb], writes=[cx.bankb[6]])
                    rstd_from_ssq(cx, W, ssq[:], cx.bankb[6], 128, 512)
                    gcol = kind[3]
                    P.op("dve", lambda t, ot=ot, pb=pb, gcol=gcol: t.scalar_tensor_tensor(out=ot[:], in0=pb[:], scalar=gain_t[:, gcol:gcol + 1], in1=W["rstd"][:],
                                                                                        op0=ALU.mult, op1=ALU.mult),
                         reads=[cx.bankb[gi], W["rstdb"], gain_b], writes=[W["otb"][oi]])
                    P.dma("sp", outs[kind[1]][kind[2], :, tsl], ot[:], reads=[W["otb"][oi]])
                else:
                    P.op("act", lambda t, ot=ot, pb=pb, M=M: t.activation(out=ot[:M, :], in_=pb[:M, :], func=AF.Copy), reads=[cx.bankb[gi]], writes=[W["otb"][oi]])
                    if kind[1] == "qi":
                        P.dma("sp", outs["qi"][kind[2], :, tsl], ot[:], reads=[W["otb"][oi]])
                    else:
                        P.dma("sp", outs["ki"][:, tsl], ot[:64, :], reads=[W["otb"][oi]])
            if kind[1] == "ki":
                for sub in range(nsub):
                    gi = W["cnt"]["g"] % 2
                    W["cnt"]["g"] += 1
                    pb = cx.banks[gi]
                    for kc in range(cfg.DC):
                        P.op("pe", lambda t, pb=pb, kc=kc, sub=sub, j=j, wbv=wbv: t.matmul(
                            pb[:, :cfg.NIH], lhsT=W["hT"][:, kc, sub * 128:(sub + 1) * 128], rhs=wbv[:, j, kc, 64:64 + cfg.NIH],
                            start=(kc == 0), stop=(kc == cfg.DC - 1)), reads=[W["wbb"][wi], allh[sub // 2]], writes=[cx.bankb[gi]])
                    oi = W["cnt"]["wis"] % 2
                    W["cnt"]["wis"] += 1
                    ws = W["wis"][oi]
                    P.op("dve", lambda t, ws=ws, pb=pb: t.tensor_copy(out=ws[:, :cfg.NIH], in_=pb[:, :cfg.NIH]), reads=[cx.bankb[gi]], writes=[W["wisb"][oi]])
                    P.dma("sp", outs["wi"][tok0 + sub * 128: tok0 + (sub + 1) * 128, :], ws[:, :cfg.NIH], reads=[W["wisb"][oi]])
            j += 1


def common_consts(cx):
    es = cx.es
    cx.epsb = cx.sb(es, "epsb", [128, 1], F32)
    cx.epsbb = Buf()
    cx.P.op("dve", lambda t: t.memset(cx.epsb[:], EPS), writes=[cx.epsbb])


def declare_dense_inputs(cx, pre, with_ffn=True, with_proj=True):
    cfg = cx.cfg
    d = {}
    if with_ffn:
        d["g1"] = cx.din(pre + "g1", [128, cfg.DC], F32)
        d["w1t"] = cx.din(pre + "w1t", [cfg.FC, 128, cfg.DC * 256], F32)
        d["w2t"] = cx.din(pre + "w2t", [cfg.DC, 128, cfg.FC * 128], F32)
    if with_proj:
        d["gm"] = cx.din(pre + "gm", [128, cfg.DC], F32)
        d["wpt"] = cx.din(pre + "wpt", [cfg.NCC, 128, cfg.DC * 128], F32)
        d["hg"] = cx.din(pre + "hg", [128, 4], F32)
    return d


def declare_proj_outs(cx):
    cfg = cx.cfg
    o = {}
    o["qa"] = cx.dout("o_qa", [cfg.HA, 128, cfg.T], BF16)
    o["ka"] = cx.dout("o_ka", [cfg.HA, 128, cfg.T], BF16)
    o["va"] = cx.dout("o_va", [cfg.T, cfg.WA], BF16)
    o["qb"] = cx.dout("o_qb", [cfg.HB, 128, cfg.T], BF16)
    o["kb"] = cx.dout("o_kb", [cfg.HB, 128, cfg.T], BF16)
    o["vb"] = cx.dout("o_vb", [cfg.T, cfg.WB], BF16)
    o["qi"] = cx.dout("o_qi", [cfg.NQI, 128, cfg.T], BF16)
    o["ki"] = cx.dout("o_ki", [64, cfg.T], BF16)
    o["wi"] = cx.dout("o_wi", [cfg.T, cfg.NIH], F32)
    return o


def head_gains(cx, es, hg_ap):
    t, b = load_vec(cx, es, "hgs", hg_ap, 4)
    sc = 128.0 ** -0.5
    for col in (0, 2):
        cx.P.op("dve", lambda tt, col=col: tt.tensor_scalar(out=t[:, col:col + 1], in0=t[:, col:col + 1], scalar1=sc, scalar2=None, op0=ALU.mult),
                reads=[b], writes=[b])
    return t, b


def build_L1(cfg):
    cx = Ctx(cfg)
    common_consts(cx)
    x_in = cx.din("xT", [cfg.D, cfg.T], F32)
    di = declare_dense_inputs(cx, "")
    x_out = cx.dout("xT_out", [cfg.D, cfg.T], F32)
    outs = declare_proj_outs(cx)
    with ExitStack() as es2:
        W = alloc_dense(cx, es2)
        g1 = load_vec(cx, es2, "g1s", di["g1"], cfg.DC)
        gm = load_vec(cx, es2, "gms", di["gm"], cfg.DC)
        hg = head_gains(cx, es2, di["hg"])
        for g in range(cfg.T // cfg.G):
            tok0 = g * cfg.G
            norm_phase(cx, W, "xin", x_in, g1[0], g1[1], tok0)
            inproj_phase(cx, W, di["w1t"])
            linres_phase(cx, W, di["w2t"], cfg.FC, "xin", x_in, "xout", x_out, tok0, 0.5)
            norm_phase(cx, W, "xout", x_out, gm[0], gm[1], tok0)
            proj_phase(cx, W, di["wpt"], hg, outs, tok0)
        cx.P.finish()
    return cx


def tile_w1(w, cfg):
    a = w.reshape(cfg.DC, 128, 2, cfg.FC, 128).transpose(3, 1, 0, 2, 4)
    return np.ascontiguousarray(a).reshape(cfg.FC, 128, cfg.DC * 256)


def tile_w2(w, nch, cfg):
    a = w.reshape(nch, 128, cfg.DC, 128).transpose(2, 1, 0, 3)
    return np.ascontiguousarray(a).reshape(cfg.DC, 128, nch * 128)


def tile_wp(w, cfg):
    wpad = np.zeros((cfg.D, cfg.NCC * 128), np.float32)
    wpad[:, :cfg.PW] = w
    a = wpad.reshape(cfg.DC, 128, cfg.NCC, 128).transpose(2, 1, 0, 3)
    return np.ascontiguousarray(a).reshape(cfg.NCC, 128, cfg.DC * 128)


def vec_pc(v, cfg):
    return np.ascontiguousarray(v.reshape(cfg.DC, 128).T)


NBIS = 20
PEN = -30000.0


class DB:
    pass


class CtxA(Ctx):
    def __init__(self, cfg):
        self.cfg = cfg
        nc = bass.Bass("TRN2", target_bir_lowering=False)
        nc.dge_precook = False
        self.nc = nc
        self.es = ExitStack()
        self.P = Prog(nc, self.es)
        self.dbank = [self.es.enter_context(nc.psum_tensor("dbank%d" % i, [128, 1024], F32)) for i in range(4)]
        self.dbb = [Buf() for _ in range(4)]
        self.hb = [[Buf(), Buf()] for _ in range(4)]
        self.ones = self.es.enter_context(nc.sbuf_tensor("ones", [128, 128], BF16))
        self.onesb = Buf()
        self.P.op("dve", lambda t: t.memset(self.ones[:], 1.0), writes=[self.onesb])
        self.xbufs = {}


def dsa_phase(cx, es, I, out_mb):
    cfg, P = cx.cfg, cx.P
    HB, NIH, CPB = cfg.HB, cfg.NIH, cfg.CPB
    KT = 128 * CPB
    NQB = cfg.S // KT
    NN = 12 + CPB
    HW = HB * 128
    sb = lambda n, s, d: cx.sb(es, n, s, d)
    scores = sb("scores", [128, cfg.S], F32)
    scb = Buf()
    maskb = sb("maskb", [128, cfg.S], BF16)
    mkb = Buf()
    ki2 = sb("ki2", [128, cfg.S], BF16)
    ki2b = Buf()
    P.dma("sp", ki2[:], I["ki2"], writes=[ki2b])
    ident = sb("ident", [128, 128], BF16)
    identb = Buf()
    P.dma("sp", ident[:], I["ident"], writes=[identb])
    pen = sb("pen", [128, KT], F32)
    penb = Buf()
    qrel = sb("qrel", [128, 1], F32)
    P.dma("sp", qrel[:], I["qrel"], writes=[penb])
    P.dma("sp", pen[:], I["iota"], writes=[penb])
    P.op("dve", lambda t: t.tensor_scalar(out=pen[:], in0=pen[:], scalar1=qrel[:, 0:1], scalar2=PEN, op0=ALU.is_gt, op1=ALU.mult), reads=[penb], writes=[penb])
    cbt = sb("cbt", [128, HW], F32)
    cbb = Buf()
    P.dma("sp", cbt[:], I["cb"], writes=[cbb])
    qbt = [sb("qbt%d" % i, [128, HW], BF16) for i in range(2)]
    qbtb = [Buf(), Buf()]
    qit = [sb("qit%d" % i, [128, cfg.NQI * 128], BF16) for i in range(2)]
    qitb = [Buf(), Buf()]
    wit = [sb("wit%d" % i, [128, 3 * NIH], F32) for i in range(2)]
    witb = [Buf(), Buf()]
    R = [sb("R%d" % i, [128, KT], F32R) for i in range(4)]
    Rb = [Buf() for _ in range(4)]
    sm = sb("sm", [128, 8], F32)
    smb = Buf()
    sma = sb("sma", [128, 2], F32)
    smab = Buf()
    midb = Buf()
    mkb2 = Buf()
    Dg = [sb("Dg%d" % i, [128, NIH * 128], F32R) for i in range(2)]
    Dgb = [Buf(), Buf()]
    identf = sb("identf", [128, 128], F32)
    identfb = Buf()
    P.dma("sp", identf[:], I["identf"], writes=[identfb])
    kt_ = [sb("kt%d" % i, [128, HW], BF16) for i in range(2)]
    ktb = [Buf(), Buf()]
    vt_ = [sb("vt%d" % i, [128, HW], BF16) for i in range(2)]
    vtb = [Buf(), Buf()]
    E = [sb("E%d" % i, [128, HW], F32) for i in range(2)]
    Eb = [Buf(), Buf()]
    PT = [sb("PT%d" % i, [128, HW], BF16) for i in range(2)]
    PTb = [Buf(), Buf()]
    Ehb = [[Buf(), Buf()] for _ in range(2)]
    PThb = [[Buf(), Buf()] for _ in range(2)]
    tbt = [sb("tbt%d" % i, [128, HW], F32) for i in range(2)]
    tbb = [Buf(), Buf()]
    ob = sb("ob", [128, HW], BF16)
    obb = Buf()
    rl = sb("rl", [128, HW], F32)
    rlb = Buf()
    cnt = {"R": 0, "kv": 0, "E": 0, "tb": 0, "ps": 0, "pt": 0, "acc": 0}
    for m in range(NQB):
        qi_ = m % 2
        P.dma("sp", qbt[qi_][:], I["qb"][m], writes=[qbtb[qi_]])
        P.dma("sp", qit[qi_][:], I["qi"][m], writes=[qitb[qi_]])
        wt = wit[qi_]
        P.dma("sp", wt[:, :NIH], I["wi"][m], writes=[witb[qi_]])
        P.op("dve", lambda t, wt=wt: t.scalar_tensor_tensor(out=wt[:, NIH:2 * NIH], in0=wt[:, :NIH], scalar=-1.0, in1=wt[:, :NIH], op0=ALU.mult, op1=ALU.max), reads=[witb[qi_]], writes=[witb[qi_]])
        P.op("dve", lambda t, wt=wt: t.tensor_scalar(out=wt[:, 2 * NIH:3 * NIH], in0=wt[:, :NIH], scalar1=0.0, scalar2=2.0, op0=ALU.is_ge, op1=ALU.mult), reads=[witb[qi_]], writes=[witb[qi_]])
        P.op("dve", lambda t, wt=wt: t.tensor_scalar(out=wt[:, 2 * NIH:3 * NIH], in0=wt[:, 2 * NIH:3 * NIH], scalar1=-1.0, scalar2=None, op0=ALU.add), reads=[witb[qi_]], writes=[witb[qi_]])
        L = KT * (m + 1)
        Dq = Dg[qi_]
        for h in range(NIH):
            P.op("dve", lambda t, Dq=Dq, wt=wt, h=h: t.tensor_scalar(out=Dq[:, h * 128:(h + 1) * 128], in0=identf[:], scalar1=wt[:, h:h + 1], scalar2=None, op0=ALU.mult),
                 reads=[identfb, witb[qi_]], writes=[Dgb[qi_]])
        for kt in range(m + 1):
            k0 = kt * KT
            ai = 2 + (cnt["acc"] % 2)
            cnt["acc"] += 1
            accp = cx.dbank[ai][:, 0:KT]
            accb = cx.hb[ai][0]
            slots = {}

            def emit_a(h, k0=k0, qi_=qi_):
                bi = cnt["ps"] % 4
                cnt["ps"] += 1
                pb = cx.dbank[bi // 2][:, (bi % 2) * 512:(bi % 2) * 512 + KT]
                pbb = cx.hb[bi // 2][bi % 2]
                p0 = (h % 2) * 64
                P.op("pe", lambda t, pb=pb, h=h, p0=p0: t.matmul(pb, lhsT=qit[qi_][p0:p0 + 64, (h // 2) * 128:(h // 2) * 128 + 128], rhs=ki2[p0:p0 + 64, k0:k0 + KT],
                                                              start=True, stop=True), reads=[qitb[qi_], ki2b], writes=[pbb, cx.dbb[bi // 2]])
                ri = cnt["R"] % 4
                cnt["R"] += 1
                Rt = R[ri]
                P.op("act", lambda t, Rt=Rt, pb=pb: t.activation(out=Rt[:], in_=pb, func=AF.Relu), reads=[pbb], writes=[Rb[ri]])
                slots[h] = (Rt, ri)

            def emit_d(h, accp=accp, accb=accb, ai=ai, Dq=Dq, qi_=qi_):
                Rt, ri = slots[h]
                P.op("pe", lambda t, Rt=Rt, h=h: t.matmul(accp, lhsT=Dq[:, h * 128:(h + 1) * 128], rhs=Rt[:], start=(h == 0), stop=(h == NIH - 1)),
                     reads=[Dgb[qi_], Rb[ri]], writes=[accb, cx.dbb[ai]])

            DEPTH_A = 3
            for h in range(min(DEPTH_A, NIH)):
                emit_a(h)
            for h in range(NIH):
                emit_d(h)
                if h + DEPTH_A < NIH:
                    emit_a(h + DEPTH_A)
            P.op("dve", lambda t, accp=accp, k0=k0: t.tensor_copy(out=scores[:, k0:k0 + KT], in_=accp), reads=[accb, cx.dbb[ai]], writes=[scb])
        P.op("dve", lambda t, L=L: t.tensor_reduce(out=sm[:, 0:1], in_=scores[:, :L], axis=mybir.AxisListType.X, op=ALU.min), reads=[scb], writes=[smb])
        P.op("dve", lambda t, L=L: t.tensor_tensor(out=scores[:, L - KT:L], in0=scores[:, L - KT:L], in1=pen[:], op=ALU.add), reads=[scb, penb], writes=[scb])
        P.op("dve", lambda t, L=L: t.tensor_reduce(out=sm[:, 5:6], in_=scores[:, :L], axis=mybir.AxisListType.X, op=ALU.max), reads=[scb, smb], writes=[smb])
        P.op("dve", lambda t: t.scalar_tensor_tensor(out=sm[:, 1:2], in0=sm[:, 5:6], scalar=1e-6, in1=sm[:, 0:1], op0=ALU.add, op1=ALU.subtract), reads=[smb], writes=[smb])
        La = max(64, (int(L * 0.42) // 64) * 64)
        nact = L - La
        for it in range(NBIS):
            f = 2.0 ** -(it + 1)
            P.op("dve", lambda t, f=f: t.scalar_tensor_tensor(out=sm[:, 2:3], in0=sm[:, 1:2], scalar=f, in1=sm[:, 0:1], op0=ALU.mult, op1=ALU.add), reads=[smb], writes=[smb, midb])
            P.op("act", lambda t, L=L, La=La: t.activation(out=maskb[:, La:L], in_=scores[:, La:L], func=AF.Sign, scale=-1.0, bias=sm[:, 2:3], accum_out=sma[:, 0:1]),
                 reads=[scb, midb], writes=[mkb2, smab])
            P.op("dve", lambda t, La=La: t.tensor_scalar(out=maskb[:, :La], in0=scores[:, :La], scalar1=sm[:, 2:3], scalar2=None, op0=ALU.is_ge, op1=ALU.add, accum_out=sm[:, 3:4]),
                 reads=[scb, smb], writes=[mkb, smb])
            P.op("dve", lambda t: t.scalar_tensor_tensor(out=sm[:, 3:4], in0=sma[:, 0:1], scalar=-0.5, in1=sm[:, 3:4], op0=ALU.mult, op1=ALU.add), reads=[smb, smab], writes=[smb])
            P.op("dve", lambda t, f=f, nact=nact: t.tensor_scalar(out=sm[:, 4:5], in0=sm[:, 3:4], scalar1=255.5 - 0.5 * nact, scalar2=f, op0=ALU.is_ge, op1=ALU.mult), reads=[smb], writes=[smb])
            P.op("dve", lambda t: t.scalar_tensor_tensor(out=sm[:, 0:1], in0=sm[:, 4:5], scalar=sm[:, 1:2], in1=sm[:, 0:1], op0=ALU.mult, op1=ALU.add), reads=[smb], writes=[smb])
        P.op("dve", lambda t, L=L: t.tensor_scalar(out=maskb[:, :L], in0=scores[:, :L], scalar1=sm[:, 0:1], scalar2=None, op0=ALU.is_ge), reads=[scb, smb], writes=[mkb, mkb2])
        nkb = CPB * (m + 1)
        oacc, lacc, sbk, mtb = cx.dbank[2], cx.dbank[3], cx.dbank[0], cx.dbank[1]
        mt16 = mtb[:].bitcast(BF16)
        for kb in range(nkb):
            ki_ = cnt["kv"] % 2
            cnt["kv"] += 1
            P.dma("sp", kt_[ki_][:], I["kb"][kb], writes=[ktb[ki_]])
            P.dma("sp", vt_[ki_][:], I["vb"][kb], writes=[vtb[ki_]])
            mi = kb % 2
            mts = mt16[:, mi * 1024:mi * 1024 + 128]
            P.op("pe", lambda t, mts=mts, kb=kb: t.transpose(out=mts, in_=maskb[:, kb * 128:(kb + 1) * 128], identity=ident[:]),
                 reads=[mkb, mkb2, identb], writes=[cx.hb[1][mi]])
            ei = cnt["E"] % 2
            cnt["E"] += 1
            Et, PTt = E[ei], PT[ei]
            delta = (nkb - 1) - kb
            tb = None
            if delta < NN:
                ti = cnt["tb"] % 2
                cnt["tb"] += 1
                tb = tbt[ti]
                P.dma("sp", tb[:], I["tb"][delta], writes=[tbb[ti]])
                P.op("dve", lambda t, tb=tb: t.tensor_tensor(out=tb[:], in0=tb[:], in1=cbt[:], op=ALU.subtract), reads=[tbb[ti], cbb], writes=[tbb[ti]])
                P.op("act", lambda t, tb=tb: t.activation(out=tb[:], in_=tb[:], func=AF.Exp), reads=[tbb[ti]], writes=[tbb[ti]])
            for hh in range((HB + 3) // 4):
                nh = min(4, HB - 4 * hh)
                c0, c1 = hh * 512, hh * 512 + nh * 128
                for h in range(4 * hh, 4 * hh + nh):
                    P.op("pe", lambda t, h=h, ki_=ki_, qi_=qi_: t.matmul(sbk[:, h * 128:(h + 1) * 128], lhsT=kt_[ki_][:, h * 128:(h + 1) * 128], rhs=qbt[qi_][:, h * 128:(h + 1) * 128],
                                                                      start=True, stop=True), reads=[ktb[ki_], qbtb[qi_]], writes=[cx.hb[0][hh]])
                P.op("act", lambda t, Et=Et, c0=c0, c1=c1: t.activation(out=Et[:, c0:c1], in_=sbk[:, c0:c1], func=AF.Exp), reads=[cx.hb[0][hh]], writes=[Ehb[ei][hh]])
                if tb is not None:
                    P.op("dve", lambda t, tb=tb, Et=Et, c0=c0, c1=c1: t.tensor_tensor(out=Et[:, c0:c1], in0=Et[:, c0:c1], in1=tb[:, c0:c1], op=ALU.mult),
                         reads=[tbb[ti], Ehb[ei][hh]], writes=[Ehb[ei][hh]])
                mtbc = mts.unsqueeze(1).to_broadcast([128, nh, 128])
                P.op("dve", lambda t, Et=Et, PTt=PTt, mtbc=mtbc, c0=c0, c1=c1, nh=nh: t.tensor_tensor(out=PTt[:, c0:c1].rearrange("p (h t) -> p h t", h=nh), in0=Et[:, c0:c1].rearrange("p (h t) -> p h t", h=nh), in1=mtbc, op=ALU.mult),
                     reads=[Ehb[ei][hh], cx.hb[1][mi]], writes=[PThb[ei][hh]])
                for h in range(4 * hh, 4 * hh + nh):
                    P.op("pe", lambda t, h=h, ki_=ki_, PTt=PTt, kb=kb, nkb=nkb: t.matmul(oacc[:, h * 128:(h + 1) * 128], lhsT=vt_[ki_][:, h * 128:(h + 1) * 128], rhs=PTt[:, h * 128:(h + 1) * 128],
                                                                                      start=(kb == 0 and h % 4 == 0), stop=(kb == nkb - 1), skip_group_check=True), reads=[vtb[ki_], PThb[ei][hh]], writes=[cx.dbb[2]])
                P.op("pe", lambda t, PTt=PTt, kb=kb, c0=c0, c1=c1, nkb=nkb: t.matmul(lacc[:, c0:c1], lhsT=cx.ones[:], rhs=PTt[:, c0:c1], start=(kb == 0), stop=(kb == nkb - 1)),
                     reads=[cx.onesb, PThb[ei][hh]], writes=[cx.dbb[3]])
        P.op("dve", lambda t: t.reciprocal(out=rl[:], in_=lacc[:, :HW]), reads=[cx.dbb[3]], writes=[rlb])
        P.op("dve", lambda t: t.tensor_tensor(out=ob[:], in0=oacc[:, :HW], in1=rl[:], op=ALU.mult), reads=[cx.dbb[2], rlb], writes=[obb])
        P.dma("sp", out_mb[:, :, m * 128:(m + 1) * 128], ob[:].rearrange("p (h t) -> p h t", h=HB), reads=[obb])


def dil_phase(cx, es, I, out_ma):
    cfg, P = cx.cfg, cx.P
    HA = cfg.HA
    HW = HA * 128
    sb = lambda n, s, d: cx.sb(es, n, s, d)
    npf = sb("npf", [128, 1], F32)
    npfb = Buf()
    P.dma("sp", npf[:], I["npf"], writes=[npfb])
    EBA = [[sb("eba%d_%d" % (br, v), [128, HW], F32) for v in range(3)] for br in range(3)]
    ebab = Buf()
    tmpv = sb("aE0", [128, HW], F32)
    for br in range(3):
        for pc in range(2):
            e_ = EBA[br][pc]
            P.dma("sp", e_[:], I["tb"][br, pc], writes=[ebab])
            P.dma("sp", tmpv[:], I["vm"][br, pc], reads=[ebab], writes=[ebab])
            P.op("act", lambda t, e_=e_: t.activation(out=e_[:], in_=e_[:], func=AF.Exp), reads=[ebab], writes=[ebab])
            P.op("dve", lambda t, e_=e_: t.tensor_tensor(out=e_[:], in0=e_[:], in1=tmpv[:], op=ALU.mult), reads=[ebab], writes=[ebab])
        P.op("dve", lambda t, br=br: t.tensor_scalar(out=EBA[br][2][:], in0=EBA[br][0][:], scalar1=npf[:, 0:1], scalar2=None, op0=ALU.mult), reads=[ebab, npfb], writes=[ebab])
    oaccT = sb("oaccT", [128, HA, 2048], F32)
    laccT = sb("laccT", [128, HA, 2048], F32)
    accb = Buf()
    qt = [sb("aq%d" % i, [128, HW], BF16) for i in range(2)]
    kt = [sb("ak%d" % i, [128, 2, HW], BF16) for i in range(2)]
    vt = [sb("av%d" % i, [128, 2, HW], BF16) for i in range(2)]
    qkvb = [Buf(), Buf()]
    E0 = tmpv
    E = [E0, E0]
    Eb0 = ebab
    Eb = [Eb0, Eb0]
    PT = [sb("aPT%d" % i, [128, HW], BF16) for i in range(2)]
    PTb = [Buf(), Buf()]
    ob = [sb("aob%d" % i, [128, HA, 256], BF16) for i in range(2)]
    obb = [Buf(), Buf()]
    n = 0
    ne = 0
    for u in range(2):
        for br, dil in enumerate((1, 4, 16)):
            span = 128 * dil
            for ti in range(16):
                np_, r = ti // dil, ti % dil
                bi = n % 2
                n += 1
                P.dma("sp", qt[bi][:], I["q"][br, u, ti], writes=[qkvb[bi]])
                P.dma("sp", kt[bi][:], I["k"][br, u, ti].rearrange("c p x -> p c x"), writes=[qkvb[bi]])
                P.dma("sp", vt[bi][:], I["v"][br, u, ti].rearrange("c p x -> p c x"), writes=[qkvb[bi]])
                sbk = cx.dbank[bi]
                oacc, lacc = cx.dbank[2], cx.dbank[3]
                for pc in range(2):
                    for h in range(HA):
                        P.op("pe", lambda t, h=h, bi=bi, pc=pc, sbk=sbk: t.matmul(sbk[:, h * 128:(h + 1) * 128], lhsT=kt[bi][:, pc, h * 128:(h + 1) * 128], rhs=qt[bi][:, h * 128:(h + 1) * 128],
                                                                              start=True, stop=True), reads=[qkvb[bi]], writes=[cx.dbb[bi]])
                    ei = ne % 2
                    ne += 1
                    Et, PTt = E[ei], PT[ei]
                    P.op("act", lambda t, Et=Et, sbk=sbk: t.activation(out=Et[:], in_=sbk[:, :HW], func=AF.Exp), reads=[cx.dbb[bi]], writes=[Eb[ei]])
                    ev = EBA[br][2] if (pc == 0 and u == 0 and np_ == 0) else EBA[br][pc]
                    P.op("dve", lambda t, Et=Et, PTt=PTt, ev=ev: t.tensor_tensor(out=PTt[:], in0=Et[:], in1=ev[:], op=ALU.mult), reads=[Eb[ei], ebab], writes=[PTb[ei]])
                    for h in range(HA):
                        P.op("pe", lambda t, h=h, bi=bi, pc=pc, PTt=PTt: t.matmul(oacc[:, h * 128:(h + 1) * 128], lhsT=vt[bi][:, pc, h * 128:(h + 1) * 128], rhs=PTt[:, h * 128:(h + 1) * 128],
                                                                              start=(pc == 0 and h % 4 == 0), stop=(pc == 1), skip_group_check=True), reads=[qkvb[bi], PTb[ei]], writes=[cx.dbb[2]])
                    for c0 in range(0, HW, 512):
                        c1 = min(HW, c0 + 512)
                        P.op("pe", lambda t, PTt=PTt, pc=pc, c0=c0, c1=c1: t.matmul(lacc[:, c0:c1], lhsT=cx.ones[:], rhs=PTt[:, c0:c1], start=(pc == 0), stop=(pc == 1)),
                             reads=[cx.onesb, PTb[ei]], writes=[cx.dbb[3]])
                s0 = np_ * span + r
                dst_o = oaccT[:, :, s0:s0 + 127 * dil + 1:dil]
                dst_l = laccT[:, :, s0:s0 + 127 * dil + 1:dil]
                ov = oacc[:, :HW].rearrange("p (h t) -> p h t", h=HA)
                lv = lacc[:, :HW].rearrange("p (h t) -> p h t", h=HA)
                if br == 0:
                    P.op("dve", lambda t, dst_o=dst_o, ov=ov: t.tensor_copy(out=dst_o, in_=ov), reads=[cx.dbb[2]], writes=[accb])
                    P.op("act", lambda t, dst_l=dst_l, lv=lv: t.activation(out=dst_l, in_=lv, func=AF.Copy), reads=[cx.dbb[3]], writes=[accb])
                else:
                    P.op("dve", lambda t, dst_o=dst_o, ov=ov: t.tensor_tensor(out=dst_o, in0=dst_o, in1=ov, op=ALU.add), reads=[cx.dbb[2], accb], writes=[accb])
                    P.op("dve", lambda t, dst_l=dst_l, lv=lv: t.tensor_tensor(out=dst_l, in0=dst_l, in1=lv, op=ALU.add), reads=[cx.dbb[3], accb], writes=[accb])
        for c in range(8):
            sl = slice(c * 256, (c + 1) * 256)
            P.op("dve", lambda t, sl=sl: t.reciprocal(out=laccT[:, :, sl], in_=laccT[:, :, sl]), reads=[accb], writes=[accb])
            oi = c % 2
            P.op("dve", lambda t, sl=sl, oi=oi: t.tensor_tensor(out=ob[oi][:], in0=oaccT[:, :, sl], in1=laccT[:, :, sl], op=ALU.mult), reads=[accb], writes=[obb[oi]])
            P.dma("sp", out_ma[:, :, u * 2048 + c * 256: u * 2048 + (c + 1) * 256], ob[oi][:], reads=[obb[oi]])


def build_L2(cfg):
    cx = CtxA(cfg)
    HA, HB, KT = cfg.HA, cfg.HB, 128 * cfg.CPB
    NQB = cfg.S // KT
    NKB = cfg.S // 128
    A = {"npf": cx.din("a_npf", [128, 1], F32), "tb": cx.din("a_tb", [3, 2, 128, HA * 128], F32), "vm": cx.din("a_vm", [3, 2, 128, HA * 128], F32),
         "q": cx.din("a_q", [3, 2, 16, 128, HA * 128], BF16), "k": cx.din("a_k", [3, 2, 16, 2, 128, HA * 128], BF16),
         "v": cx.din("a_v", [3, 2, 16, 2, 128, HA * 128], BF16)}
    Bd = {"ki2": cx.din("b_ki2", [128, cfg.S], BF16), "ident": cx.din("b_ident", [128, 128], BF16), "identf": cx.din("b_identf", [128, 128], F32), "qrel": cx.din("b_qrel", [128, 1], F32),
          "iota": cx.din("b_iota", [128, KT], F32), "cb": cx.din("b_cb", [128, HB * 128], F32), "qb": cx.din("b_qb", [NQB, 128, HB * 128], BF16),
          "qi": cx.din("b_qi", [NQB, 128, cfg.NQI * 128], BF16), "wi": cx.din("b_wi", [NQB, 128, cfg.NIH], F32),
          "kb": cx.din("b_kb", [NKB, 128, HB * 128], BF16), "vb": cx.din("b_vb", [NKB, 128, HB * 128], BF16),
          "tb": cx.din("b_tb", [12 + cfg.CPB, 128, HB * 128], F32)}
    o_ma = cx.dout("o_ma", [128, HA, cfg.T], BF16)
    o_mb = cx.dout("o_mb", [128, HB, NQB * 128], BF16)
    with ExitStack() as es2:
        dil_phase(cx, es2, A, o_ma)
        cx.P.barrier()
    with ExitStack() as es3:
        dsa_phase(cx, es3, Bd, o_mb)
        cx.P.finish()
    return cx


def rel_bucket_np(dist):
    dist = np.asarray(dist, np.int64)
    df = np.maximum(dist, 1).astype(np.float32)
    large = 16 + (np.log(df / np.float32(16)) / np.float32(np.log(2048 / 16)) * np.float32(16)).astype(np.int32)
    large = np.minimum(large, 31)
    return np.where(dist < 16, np.maximum(dist, 0), large).astype(np.int64)


def core_tokens(cfg, c):
    r = c % cfg.CPB
    u0, u1 = r, cfg.NU - 1 - r
    return np.concatenate([np.arange(2048) + 2048 * u0, np.arange(2048) + 2048 * u1])


def gather_global(cfg, outs_per_core):
    G = []
    for b in range(cfg.B):
        g = {"qa": np.zeros((cfg.HA, 128, cfg.S), NPBF), "ka": np.zeros((cfg.HA, 128, cfg.S), NPBF), "va": np.zeros((cfg.S, cfg.WA), NPBF),
             "qb": np.zeros((cfg.HB, 128, cfg.S), NPBF), "kb": np.zeros((cfg.HB, 128, cfg.S), NPBF), "vb": np.zeros((cfg.S, cfg.WB), NPBF),
             "qi": np.zeros((cfg.NQI, 128, cfg.S), NPBF), "ki": np.zeros((64, cfg.S), NPBF), "wi": np.zeros((cfg.S, cfg.NIH), np.float32)}
        for r in range(cfg.CPB):
            c = b * cfg.CPB + r
            pos = core_tokens(cfg, c)
            o = outs_per_core[c]
            for k in ("qa", "ka", "qb", "kb", "qi", "ki"):
                g[k][..., pos] = o["o_" + k]
            for k in ("va", "vb", "wi"):
                g[k][pos] = o["o_" + k]
        G.append(g)
    return G


def l2_inputs(cfg, G, rel_bias, c):
    b, r = c // cfg.CPB, c % cfg.CPB
    g = G[b]
    HA, HB, CPB = cfg.HA, cfg.HB, cfg.CPB
    rel_bias = np.asarray(rel_bias, np.float32)
    ins = {}
    units = (r, cfg.NU - 1 - r)
    aq = np.zeros((3, 2, 16, 128, HA * 128), NPBF)
    ak = np.zeros((3, 2, 16, 2, 128, HA * 128), NPBF)
    av = np.zeros((3, 2, 16, 2, 128, HA * 128), NPBF)
    atb = np.zeros((3, 2, 128, HA, 128), np.float32)
    avm = np.zeros((3, 2, 128, HA, 128), np.float32)
    jj, ii = np.meshgrid(np.arange(128), np.arange(128), indexing="ij")
    for br, dil in enumerate((1, 4, 16)):
        span = 128 * dil
        for pc in range(2):
            step = ii - jj + (128 if pc == 0 else 0)
            valid = (step >= 0) & (step <= 128) & ((jj >= ii) if pc == 0 else (jj <= ii))
            bk = rel_bucket_np(np.clip(step, 0, 128) * dil)
            atb[br, pc] = np.where(valid[:, None, :], rel_bias[bk][:, :, :HA].transpose(0, 2, 1), 0.0)
            avm[br, pc] = valid[:, None, :].astype(np.float32)
        for u, gu in enumerate(units):
            for ti in range(16):
                np_, r_ = ti // dil, ti % dil
                p = 2048 * gu + np_ * span + r_ + dil * np.arange(128)
                aq[br, u, ti] = g["qa"][:, :, p].transpose(1, 0, 2).reshape(128, HA * 128)
                ak[br, u, ti, 1] = g["ka"][:, :, p].transpose(1, 0, 2).reshape(128, HA * 128)
                av[br, u, ti, 1] = g["va"][p]
                pp = p - span
                if pp[0] >= 0:
                    ak[br, u, ti, 0] = g["ka"][:, :, pp].transpose(1, 0, 2).reshape(128, HA * 128)
                    av[br, u, ti, 0] = g["va"][pp]
    ins.update({"a_q": aq, "a_k": ak, "a_v": av, "a_tb": atb.reshape(3, 2, 128, HA * 128), "a_vm": avm.reshape(3, 2, 128, HA * 128),
                "a_npf": np.full((128, 1), 0.0 if r == 0 else 1.0, np.float32)})
    KT = 128 * CPB
    NQB = cfg.S // KT
    NKB = cfg.S // 128
    gq = CPB * np.arange(NQB) + r
    qb = g["qb"].reshape(HB, 128, NKB, 128)[:, :, gq]
    ins["b_qb"] = np.ascontiguousarray(qb.transpose(2, 1, 0, 3)).reshape(NQB, 128, HB * 128)
    qi = g["qi"].reshape(cfg.NQI, 128, NKB, 128)[:, :, gq]
    ins["b_qi"] = np.ascontiguousarray(qi.transpose(2, 1, 0, 3)).reshape(NQB, 128, cfg.NQI * 128)
    ins["b_wi"] = np.ascontiguousarray(g["wi"].reshape(NKB, 128, cfg.NIH)[gq])
    ins["b_ki2"] = np.ascontiguousarray(np.concatenate([g["ki"], g["ki"]], 0))
    ins["b_kb"] = np.ascontiguousarray(g["kb"].reshape(HB, 128, NKB, 128).transpose(2, 1, 0, 3)).reshape(NKB, 128, HB * 128)
    ins["b_vb"] = np.ascontiguousarray(g["vb"].reshape(NKB, 128, HB * 128))
    ins["b_qrel"] = (r * 128 + np.arange(128, dtype=np.float32)).reshape(128, 1)
    ins["b_iota"] = np.ascontiguousarray(np.broadcast_to(np.arange(KT, dtype=np.float32), (128, KT)))
    ins["b_cb"] = np.ascontiguousarray(np.broadcast_to(rel_bias[31, HA:HA + HB][None, :, None], (128, HB, 128))).reshape(128, HB * 128)
    NN = 12 + CPB
    tb = np.zeros((NN, 128, HB, 128), np.float32)
    for d in range(NN):
        dist = 128 * (d - (CPB - 1 - r)) + ii - jj
        tb[d] = rel_bias[rel_bucket_np(np.maximum(dist, 0))][:, :, HA:HA + HB].transpose(0, 2, 1)
    ins["b_tb"] = tb.reshape(NN, 128, HB * 128)
    ins["b_ident"] = np.eye(128, dtype=np.float32).astype(NPBF)
    ins["b_identf"] = np.eye(128, dtype=np.float32)
    return ins


def scatter_mix(cfg, res_per_core):
    M = []
    KT = 128 * cfg.CPB
    NQB = cfg.S // KT
    for b in range(cfg.B):
        mix = np.zeros((cfg.S, cfg.WA + cfg.WB), NPBF)
        for r in range(cfg.CPB):
            c = b * cfg.CPB + r
            pos = core_tokens(cfg, c)
            ma = res_per_core[c]["o_ma"]
            mix[pos, :cfg.WA] = ma.transpose(2, 1, 0).reshape(cfg.T, cfg.WA)
            mb = res_per_core[c]["o_mb"].reshape(128, cfg.HB, NQB, 128)
            gq = cfg.CPB * np.arange(NQB) + r
            posb = (gq[:, None] * 128 + np.arange(128)[None, :]).reshape(-1)
            mix[posb, cfg.WA:] = mb.transpose(2, 3, 1, 0).reshape(NQB * 128, cfg.WB)
        M.append(mix)
    return M


def build_L3(cfg, with_next=True):
    cx = Ctx(cfg)
    common_consts(cx)
    NM = (cfg.WA + cfg.WB) // 128
    x_in = cx.din("xT", [cfg.D, cfg.T], F32)
    mixT = cx.din("mixT", [NM * 128, cfg.T], BF16)
    wot = cx.din("wot", [cfg.DC, 128, NM * 128], F32)
    f2 = declare_dense_inputs(cx, "f2_", with_proj=False)
    xa = cx.dint("xa", [cfg.D, cfg.T], F32)
    xb = cx.dout("xT_mid", [cfg.D, cfg.T], F32)
    if with_next:
        nx = declare_dense_inputs(cx, "nx_")
        xc = cx.dout("xT_out", [cfg.D, cfg.T], F32)
        outs = declare_proj_outs(cx)
    with ExitStack() as es2:
        W = alloc_dense(cx, es2)
        g2 = load_vec(cx, es2, "g2s", f2["g1"], cfg.DC)
        if with_next:
            g1 = load_vec(cx, es2, "g1s", nx["g1"], cfg.DC)
            gm = load_vec(cx, es2, "gms", nx["gm"], cfg.DC)
            hg = head_gains(cx, es2, nx["hg"])
        for g in range(cfg.T // cfg.G):
            tok0 = g * cfg.G
            load_actT(cx, W, mixT, NM, tok0)
            linres_phase(cx, W, wot, NM, "xin", x_in, "xa", xa, tok0, 1.0)
            norm_phase(cx, W, "xa", xa, g2[0], g2[1], tok0)
            inproj_phase(cx, W, f2["w1t"])
            linres_phase(cx, W, f2["w2t"], cfg.FC, "xa", xa, "xb", xb, tok0, 0.5)
            if not with_next:
                continue
            norm_phase(cx, W, "xb", xb, g1[0], g1[1], tok0)
            inproj_phase(cx, W, nx["w1t"])
            linres_phase(cx, W, nx["w2t"], cfg.FC, "xb", xb, "xc", xc, tok0, 0.5)
            norm_phase(cx, W, "xc", xc, gm[0], gm[1], tok0)
            proj_phase(cx, W, nx["wpt"], hg, outs, tok0)
        cx.P.finish()
    return cx


def _run(cx, in_maps, n):
    res = run_bass_kernel_spmd(cx.nc, in_maps, core_ids=list(range(n)))
    cx.es.close()
    return res.results


def layer_dense_inputs(cfg, inp, l, pre, with_ffn1=True):
    d = {}
    if with_ffn1:
        d[pre + "g1"] = vec_pc(inp["norm_ffn1"][l], cfg)
        d[pre + "w1t"] = tile_w1(inp["w_ffn1_in"][l], cfg)
        d[pre + "w2t"] = tile_w2(inp["w_ffn1_out"][l], cfg.FC, cfg)
    d[pre + "gm"] = vec_pc(inp["norm_mix"][l], cfg)
    d[pre + "wpt"] = tile_wp(inp["w_in"][l], cfg)
    d[pre + "hg"] = np.ascontiguousarray(np.stack([inp["q_norm_a"][l], inp["k_norm_a"][l], inp["q_norm_b"][l], inp["k_norm_b"][l]], 1))
    return d


def run_model(cfg, inp):
    inp = {k: np.asarray(v) for k, v in inp.items()}
    n = cfg.NCORES
    x = inp["x"]
    xT = [np.ascontiguousarray(x[c // cfg.CPB][core_tokens(cfg, c)].T) for c in range(n)]
    cx = build_L1(cfg)
    d0 = layer_dense_inputs(cfg, inp, 0, "")
    res = _run(cx, [dict(d0, xT=xT[c]) for c in range(n)], n)
    cxa = build_L2(cfg)
    cx3 = None
    for l in range(cfg.DEPTH):
        G = gather_global(cfg, res)
        resa = _run(cxa, [l2_inputs(cfg, G, inp["rel_bias"], c) for c in range(n)], n)
        if l + 1 < cfg.DEPTH:
            cxa = build_L2(cfg)
        mix = scatter_mix(cfg, resa)
        last = (l + 1 == cfg.DEPTH)
        cx3 = build_L3(cfg, with_next=not last)
        d3 = {} if last else layer_dense_inputs(cfg, inp, l + 1, "nx_")
        d3["wot"] = tile_w2(inp["w_out"][l], (cfg.WA + cfg.WB) // 128, cfg)
        d3["f2_g1"] = vec_pc(inp["norm_ffn2"][l], cfg)
        d3["f2_w1t"] = tile_w1(inp["w_ffn2_in"][l], cfg)
        d3["f2_w2t"] = tile_w2(inp["w_ffn2_out"][l], cfg.FC, cfg)
        ims = []
        for c in range(n):
            xin = res[c]["xT_out"]
            mt = np.ascontiguousarray(mix[c // cfg.CPB][core_tokens(cfg, c)].T)
            ims.append(dict(d3, xT=xin, mixT=mt))
        res = _run(cx3, ims, n)
    out = np.zeros((cfg.B, cfg.S, cfg.D), np.float32)
    for c in range(n):
        out[c // cfg.CPB, core_tokens(cfg, c)] = res[c]["xT_mid"].T
    return out


def kernel(**inputs):
    return run_model(Cfg(), inputs)
```

```python
import numpy as np
from contextlib import ExitStack
import ml_dtypes
import concourse.bass as bass
import concourse.mybir as mybir
from concourse.bass_utils import run_bass_kernel_spmd

F32 = mybir.dt.float32
F32R = mybir.dt.float32r
BF16 = mybir.dt.bfloat16
ALU = mybir.AluOpType
AF = mybir.ActivationFunctionType
NPBF = ml_dtypes.bfloat16
EPS = 1e-6


class Cfg:
    def __init__(s, D=2048, F=5632, HA=8, HB=8, NIH=16, B=2, S=16384, CPB=4, DEPTH=2):
        s.D, s.F, s.HA, s.HB, s.NIH, s.B, s.S, s.CPB, s.DEPTH = D, F, HA, HB, NIH, B, S, CPB, DEPTH
        s.DC, s.FC = D // 128, F // 128
        s.NCORES = B * CPB
        s.UNIT = 2048
        s.NU = S // s.UNIT
        assert s.NU == 2 * CPB
        s.T = 2 * s.UNIT
        s.G = 1024
        s.WA, s.WB, s.QI = HA * 128, HB * 128, NIH * 64
        s.PW = 3 * s.WA + 3 * s.WB + s.QI + 64 + NIH
        s.NCC = (s.PW + 127) // 128
        s.o_qa, s.o_ka, s.o_va = 0, s.WA, 2 * s.WA
        s.o_qb, s.o_kb, s.o_vb = 3 * s.WA, 3 * s.WA + s.WB, 3 * s.WA + 2 * s.WB
        s.o_qi = 3 * s.WA + 3 * s.WB
        s.o_ki = s.o_qi + s.QI
        s.o_wi = s.o_ki + 64
        assert s.o_ki % 128 == 0
        s.NQI = s.QI // 128


class Buf:
    __slots__ = ("lw", "rd")

    def __init__(self):
        self.lw = None
        self.rd = []


class EngS:
    def __init__(self, name):
        self.name = name
        self.ops = []
        self.count = 0
        self.waited = {}


class Prog:
    NDMASEM = 12

    def __init__(self, nc, es):
        self.nc = nc
        self.E = {n: EngS(n) for n in ("pe", "act", "dve", "pool", "sp")}
        self.sems = {}
        for n in self.E:
            self.sems[("e", n)] = es.enter_context(nc.semaphore("s_" + n))
        self.dq = {}
        for q in ("sp", "pool"):
            lst = []
            for i in range(self.NDMASEM):
                k = ("d", q, i)
                self.sems[k] = es.enter_context(nc.semaphore("d_%s_%d" % (q, i)))
                lst.append(k)
            self.dq[q] = {"keys": lst, "n": 0, "vals": [0] * self.NDMASEM}

    def _deps(self, reads, writes, extra=()):
        deps = list(extra)
        for b in reads:
            if b.lw is not None:
                deps.append(b.lw)
        for b in writes:
            if b.lw is not None:
                deps.append(b.lw)
            deps.extend(b.rd)
        return deps

    def _prune(self, e, deps):
        best = {}
        for (k, v) in deps:
            if e.name == "pe" and k == ("e", "pe"):
                continue
            if e.waited.get(k, 0) >= v:
                continue
            if best.get(k, 0) < v:
                best[k] = v
        for k, v in best.items():
            e.waited[k] = v
        return list(best.items())

    def _commit(self, tok, reads, writes):
        for b in reads:
            b.rd.append(tok)
        for b in writes:
            b.lw = tok
            b.rd = []

    def op(self, eng, fn, reads=(), writes=()):
        e = self.E[eng]
        waits = self._prune(e, self._deps(reads, writes))
        e.count += 1
        tok = (("e", eng), e.count)
        e.ops.append((waits, fn, (("e", eng), 1)))
        self._commit(tok, reads, writes)
        return tok

    def dma(self, q, out, in_, reads=(), writes=()):
        e = self.E[q]
        d = self.dq[q]
        i = d["n"] % self.NDMASEM
        d["n"] += 1
        key = d["keys"][i]
        prev = d["vals"][i]
        extra = [(key, prev)] if prev > 0 else []
        waits = self._prune(e, self._deps(reads, writes, extra))
        d["vals"][i] = prev + 16
        tok = (key, prev + 16)
        e.ops.append((waits, (lambda t, out=out, in_=in_: t.dma_start(out=out, in_=in_)), (key, 16)))
        self._commit(tok, reads, writes)
        return tok

    def all_tokens(self):
        toks = []
        for n, e in self.E.items():
            if e.count:
                toks.append((("e", n), e.count))
        for q, d in self.dq.items():
            for k, v in zip(d["keys"], d["vals"]):
                if v:
                    toks.append((k, v))
        return toks

    def barrier(self):
        toks = self.all_tokens()
        for n, e in self.E.items():
            waits = self._prune(e, toks)
            if waits:
                e.ops.append((waits, None, None))

    def finish(self):
        self.barrier()
        nc, sems, E = self.nc, self.sems, self.E

        def replay(engname, engobj):
            for (waits, fn, inc) in E[engname].ops:
                for (k, v) in waits:
                    engobj.wait_ge(sems[k], v)
                if fn is not None:
                    fn(engobj).then_inc(sems[inc[0]], inc[1])

        with nc.Block() as block:
            @block.tensor
            def _(t):
                replay("pe", t)

            @block.scalar
            def _(t):
                replay("act", t)

            @block.vector
            def _(t):
                replay("dve", t)

            @block.gpsimd
            def _(t):
                replay("pool", t)

            @block.sync
            def _(t):
                replay("sp", t)


class Ctx:
    def __init__(self, cfg):
        self.cfg = cfg
        nc = bass.Bass("TRN2", target_bir_lowering=False)
        nc.dge_precook = False
        self.nc = nc
        self.es = ExitStack()
        self.P = Prog(nc, self.es)
        self.banks = [self.es.enter_context(nc.psum_tensor("bank%d" % i, [128, 512], F32)) for i in range(8)]
        self.bankb = [Buf() for _ in range(8)]
        self.ones = self.es.enter_context(nc.sbuf_tensor("ones", [128, 128], BF16))
        self.onesb = Buf()
        self.P.op("dve", lambda t: t.memset(self.ones[:], 1.0), writes=[self.onesb])
        self.xbufs = {}
        self.n_in = {}

    def din(self, name, shape, dt):
        return self.nc.dram_tensor(name, list(shape), dt, kind="ExternalInput").ap()

    def dout(self, name, shape, dt):
        return self.nc.dram_tensor(name, list(shape), dt, kind="ExternalOutput").ap()

    def dint(self, name, shape, dt):
        return self.nc.dram_tensor(name, list(shape), dt, kind="Internal").ap()

    def sb(self, es, name, shape, dt):
        return es.enter_context(self.nc.sbuf_tensor(name, list(shape), dt))

    def xb(self, name, dc, tt):
        k = (name, dc, tt)
        if k not in self.xbufs:
            self.xbufs[k] = Buf()
        return self.xbufs[k]


def alloc_dense(cx, es):
    cfg = cx.cfg
    W = {}
    NCH = max(cfg.FC, cfg.DC)
    W["hT"] = cx.sb(es, "hT", [128, cfg.DC, cfg.G], BF16)
    W["hTb"] = [Buf() for _ in range(cfg.G // 256)]
    W["actT"] = cx.sb(es, "actT", [128, NCH, cfg.G], BF16)
    W["actTb"] = [Buf() for _ in range(NCH)]
    W["w1"] = [cx.sb(es, "w1_%d" % i, [128, cfg.DC * 256], BF16) for i in range(2)]
    W["w1b"] = [Buf(), Buf()]
    wbsz = max(NCH * 128, 4 * cfg.DC * 128)
    W["wb"] = [cx.sb(es, "wb_%d" % i, [128, wbsz], BF16) for i in range(2)]
    W["wbb"] = [Buf(), Buf()]
    W["xn"] = cx.sb(es, "xn", [128, cfg.DC, 256], F32)
    W["xnb"] = Buf()
    W["sq"] = [cx.sb(es, "sq%d" % i, [128, 512], BF16) for i in range(2)]
    W["sqb"] = [Buf(), Buf()]
    W["rstd"] = cx.sb(es, "rstd", [128, 512], F32)
    W["rstdb"] = Buf()
    W["sg"] = [cx.sb(es, "sg%d" % i, [128, 512], F32) for i in range(2)]
    W["sgb"] = [Buf(), Buf()]
    W["xe"] = [cx.sb(es, "xe%d" % i, [128, 512], F32) for i in range(2)]
    W["xeb"] = [Buf(), Buf()]
    W["xo"] = [cx.sb(es, "xo%d" % i, [128, 512], F32) for i in range(2)]
    W["xob"] = [Buf(), Buf()]
    W["ot"] = [cx.sb(es, "ot%d" % i, [128, 512], BF16) for i in range(2)]
    W["otb"] = [Buf(), Buf()]
    W["wis"] = [cx.sb(es, "wis%d" % i, [128, 16], F32) for i in range(2)]
    W["wisb"] = [Buf(), Buf()]
    W["cnt"] = {"g": 0, "y": 0, "w1": 0, "wb": 0, "sq": 0, "sg": 0, "xe": 0, "ot": 0, "wis": 0}
    return W


def load_vec(cx, es, name, dram_ap, ncol):
    t = cx.sb(es, name, [128, ncol], F32)
    b = Buf()
    cx.P.dma("sp", t[:], dram_ap, writes=[b])
    return t, b


def rstd_from_ssq(cx, W, ssq_ap, ssq_buf, n, width):
    P = cx.P
    rs = W["rstd"]
    P.op("act", lambda t: t.activation(out=rs[:, :width], in_=ssq_ap, func=AF.Sqrt, scale=1.0 / n, bias=cx.epsb[:, 0:1]),
         reads=[ssq_buf, cx.epsbb], writes=[W["rstdb"]])
    P.op("dve", lambda t: t.reciprocal(out=rs[:, :width], in_=rs[:, :width]), reads=[W["rstdb"]], writes=[W["rstdb"]])


def norm_phase(cx, W, xname, xap, gain, gainb, tok0):
    cfg, P = cx.cfg, cx.P
    xv = xap.rearrange("(c p) t -> p c t", p=128)
    for q in range(cfg.G // 256):
        t0 = tok0 + q * 256
        tt = t0 // 512
        xn = W["xn"]
        P.dma("sp", xn[:], xv[:, :, t0:t0 + 256], reads=[cx.xb(xname, dc, tt) for dc in range(cfg.DC)], writes=[W["xnb"]])
        ssq = cx.banks[6]
        for c in range(cfg.DC):
            i = W["cnt"]["sq"] % 2
            W["cnt"]["sq"] += 1
            sq = W["sq"][i]
            P.op("act", lambda t, sq=sq, c=c: t.activation(out=sq[:, :256], in_=xn[:, c, :], func=AF.Square),
                 reads=[W["xnb"]], writes=[W["sqb"][i]])
            P.op("pe", lambda t, sq=sq, c=c: t.matmul(ssq[:, :256], lhsT=cx.ones[:], rhs=sq[:, :256], start=(c == 0), stop=(c == cfg.DC - 1)),
                 reads=[W["sqb"][i], cx.onesb], writes=[cx.bankb[6]])
        rstd_from_ssq(cx, W, ssq[:, :256], cx.bankb[6], cfg.D, 256)
        for c in range(cfg.DC):
            P.op("dve", lambda t, c=c, q=q: t.scalar_tensor_tensor(out=W["hT"][:, c, q * 256:(q + 1) * 256], in0=xn[:, c, :], scalar=gain[:, c:c + 1],
                                                                 in1=W["rstd"][:, :256], op0=ALU.mult, op1=ALU.mult),
                 reads=[W["xnb"], W["rstdb"], gainb], writes=[W["hTb"][q]])


def inproj_phase(cx, W, w1t):
    cfg, P = cx.cfg, cx.P
    ntt = cfg.G // 512
    for f in range(cfg.FC):
        wi = W["cnt"]["w1"] % 2
        W["cnt"]["w1"] += 1
        w1 = W["w1"][wi]
        P.dma("pool", w1[:], w1t[f], writes=[W["w1b"][wi]])
        for tt in range(ntt):
            gi = W["cnt"]["g"] % 2
            W["cnt"]["g"] += 1
            pg, pu = cx.banks[gi], cx.banks[2 + gi]
            hb = [W["hTb"][2 * tt], W["hTb"][2 * tt + 1]]
            for gu, pb, bi in ((0, pg, gi), (1, pu, 2 + gi)):
                for kc in range(cfg.DC):
                    P.op("pe", lambda t, pb=pb, w1=w1, kc=kc, gu=gu, tt=tt: t.matmul(
                        pb[:], lhsT=w1[:, kc * 256 + gu * 128: kc * 256 + gu * 128 + 128], rhs=W["hT"][:, kc, tt * 512:(tt + 1) * 512],
                        start=(kc == 0), stop=(kc == cfg.DC - 1)), reads=[W["w1b"][wi]] + hb, writes=[cx.bankb[bi]])
            si = W["cnt"]["sg"] % 2
            W["cnt"]["sg"] += 1
            sg = W["sg"][si]
            P.op("act", lambda t, sg=sg, pg=pg: t.activation(out=sg[:], in_=pg[:], func=AF.Silu), reads=[cx.bankb[gi]], writes=[W["sgb"][si]])
            P.op("dve", lambda t, sg=sg, pu=pu, f=f, tt=tt: t.tensor_tensor(out=W["actT"][:, f, tt * 512:(tt + 1) * 512], in0=sg[:], in1=pu[:], op=ALU.mult),
                 reads=[W["sgb"][si], cx.bankb[2 + gi]], writes=[W["actTb"][f]])


def linres_phase(cx, W, w2t, nch, xsname, xs, xdname, xd, tok0, scale):
    cfg, P = cx.cfg, cx.P
    ntt = cfg.G // 512
    for dc in range(cfg.DC):
        wi = W["cnt"]["wb"] % 2
        W["cnt"]["wb"] += 1
        wb = W["wb"][wi]
        P.dma("pool", wb[:, :nch * 128], w2t[dc], writes=[W["wbb"][wi]])
        for tt in range(ntt):
            yi = W["cnt"]["y"] % 2
            W["cnt"]["y"] += 1
            py = cx.banks[4 + yi]
            gt = (tok0 + tt * 512) // 512
            ei = W["cnt"]["xe"] % 2
            W["cnt"]["xe"] += 1
            xe, xo = W["xe"][ei], W["xo"][ei]
            P.dma("sp", xe[:], xs[dc * 128:(dc + 1) * 128, tok0 + tt * 512: tok0 + (tt + 1) * 512],
                  reads=[cx.xb(xsname, dc, gt)], writes=[W["xeb"][ei]])
            for f in range(nch):
                P.op("pe", lambda t, py=py, wb=wb, f=f, tt=tt: t.matmul(py[:], lhsT=wb[:, f * 128:(f + 1) * 128], rhs=W["actT"][:, f, tt * 512:(tt + 1) * 512],
                                                                      start=(f == 0), stop=(f == nch - 1)),
                     reads=[W["wbb"][wi], W["actTb"][f]], writes=[cx.bankb[4 + yi]])
            P.op("dve", lambda t, py=py, xe=xe, xo=xo: t.scalar_tensor_tensor(out=xo[:], in0=py[:], scalar=float(scale), in1=xe[:], op0=ALU.mult, op1=ALU.add),
                 reads=[cx.bankb[4 + yi], W["xeb"][ei]], writes=[W["xob"][ei]])
            P.dma("sp", xd[dc * 128:(dc + 1) * 128, tok0 + tt * 512: tok0 + (tt + 1) * 512], xo[:],
                  reads=[W["xob"][ei]], writes=[cx.xb(xdname, dc, gt)])


def load_actT(cx, W, src, nch, tok0):
    cfg, P = cx.cfg, cx.P
    sv = src.rearrange("(c p) t -> p c t", p=128)
    for c in range(nch):
        P.dma("sp", W["actT"][:, c, :], sv[:, c, tok0:tok0 + cfg.G], writes=[W["actTb"][c]])


def proj_phase(cx, W, wpt, gains, outs, tok0):
    cfg, P = cx.cfg, cx.P
    ntt = cfg.G // 512
    nsub = cfg.G // 128
    allh = W["hTb"]
    kinds = {}
    for h in range(cfg.HA):
        kinds[cfg.o_qa // 128 + h] = ("n", "qa", h, 0)
        kinds[cfg.o_ka // 128 + h] = ("n", "ka", h, 1)
    for h in range(cfg.HB):
        kinds[cfg.o_qb // 128 + h] = ("n", "qb", h, 2)
        kinds[cfg.o_kb // 128 + h] = ("n", "kb", h, 3)
    for j in range(cfg.NQI):
        kinds[cfg.o_qi // 128 + j] = ("p", "qi", j, 128)
    kinds[cfg.o_ki // 128] = ("p", "ki", 0, 64)
    for v0, nm, wdt in ((cfg.o_va // 128, "va", cfg.WA), (cfg.o_vb // 128, "vb", cfg.WB)):
        for j in range(wdt // 128):
            kinds[v0 + j] = ("v", nm, j, 0)
    gain_t, gain_b = gains
    for cc0 in range(0, cfg.NCC, 4):
        ncl = min(4, cfg.NCC - cc0)
        wi = W["cnt"]["wb"] % 2
        W["cnt"]["wb"] += 1
        wb = W["wb"][wi]
        wbv = wb[:, :4 * cfg.DC * 128].rearrange("p (j k c) -> p j k c", j=4, k=cfg.DC)
        P.dma("pool", wbv[:, :ncl], wpt[cc0:cc0 + ncl].rearrange("j p (k c) -> p j k c", k=cfg.DC), writes=[W["wbb"][wi]])
        j = 0
        while j < ncl:
            cc = cc0 + j
            kind = kinds[cc]
            if kind[0] == "v":
                j1 = j
                while j1 < ncl and kinds[cc0 + j1][0] == "v" and kinds[cc0 + j1][1] == kind[1]:
                    j1 += 1
                nv = j1 - j
                for sub in range(nsub):
                    gi = W["cnt"]["g"] % 2
                    W["cnt"]["g"] += 1
                    pb = cx.banks[gi]
                    for kc in range(cfg.DC):
                        P.op("pe", lambda t, pb=pb, kc=kc, sub=sub, j=j, nv=nv, wbv=wbv: t.matmul(
                            pb[:, :nv * 128], lhsT=W["hT"][:, kc, sub * 128:(sub + 1) * 128], rhs=wbv[:, j:j + nv, kc, :],
                            start=(kc == 0), stop=(kc == cfg.DC - 1)), reads=[W["wbb"][wi], allh[sub // 2]], writes=[cx.bankb[gi]])
                    oi = W["cnt"]["ot"] % 2
                    W["cnt"]["ot"] += 1
                    ot = W["ot"][oi]
                    P.op("act", lambda t, ot=ot, pb=pb, nv=nv: t.activation(out=ot[:, :nv * 128], in_=pb[:, :nv * 128], func=AF.Copy),
                         reads=[cx.bankb[gi]], writes=[W["otb"][oi]])
                    c0 = kind[2] * 128
                    P.dma("sp", outs[kind[1]][tok0 + sub * 128: tok0 + (sub + 1) * 128, c0:c0 + nv * 128], ot[:, :nv * 128], reads=[W["otb"][oi]])
                j = j1
                continue
            M = 128 if kind[0] == "n" else kind[3]
            for tt in range(ntt):
                gi = W["cnt"]["g"] % 2
                W["cnt"]["g"] += 1
                pb = cx.banks[gi]
                hb = [allh[2 * tt], allh[2 * tt + 1]]
                for kc in range(cfg.DC):
                    P.op("pe", lambda t, pb=pb, kc=kc, tt=tt, j=j, M=M, wbv=wbv: t.matmul(
                        pb[:M, :], lhsT=wbv[:, j, kc, :M], rhs=W["hT"][:, kc, tt * 512:(tt + 1) * 512],
                        start=(kc == 0), stop=(kc == cfg.DC - 1)), reads=[W["wbb"][wi]] + hb, writes=[cx.bankb[gi]])
                oi = W["cnt"]["ot"] % 2
                W["cnt"]["ot"] += 1
                ot = W["ot"][oi]
                tsl = slice(tok0 + tt * 512, tok0 + (tt + 1) * 512)
                if kind[0] == "n":
                    si = W["cnt"]["sq"] % 2
                    W["cnt"]["sq"] += 1
                    sq = W["sq"][si]
                    P.op("act", lambda t, sq=sq, pb=pb: t.activation(out=sq[:], in_=pb[:], func=AF.Square), reads=[cx.bankb[gi]], writes=[W["sqb"][si]])
                    ssq = cx.banks[6]
                    P.op("pe", lambda t, sq=sq: t.matmul(ssq[:], lhsT=cx.ones[:], rhs=sq[:], start=True, stop=True),
                         reads=[W["sqb"][si], cx.onesb], writes=[cx.bankb[6]])
                    rstd_from_ssq(cx, W, ssq[:], cx.bankb[6], 128, 512)
                    gcol = kind[3]
                    P.op("dve", lambda t, ot=ot, pb=pb, gcol=gcol: t.scalar_tensor_tensor(out=ot[:], in0=pb[:], scalar=gain_t[:, gcol:gcol + 1], in1=W["rstd"][:],
                                                                                        op0=ALU.mult, op1=ALU.mult),
                         reads=[cx.bankb[gi], W["rstdb"], gain_b], writes=[W["otb"][oi]])
                    P.dma("sp", outs[kind[1]][kind[2], :, tsl], ot[:], reads=[W["otb"][oi]])
                else:
                    P.op("act", lambda t, ot=ot, pb=pb, M=M: t.activation(out=ot[:M, :], in_=pb[:M, :], func=AF.Copy), reads=[cx.bankb[gi]], writes=[W["otb"][oi]])
                    if kind[1] == "qi":
                        P.dma("sp", outs["qi"][kind[2], :, tsl], ot[:], reads=[W["otb"][oi]])
                    else:
                        P.dma("sp", outs["ki"][:, tsl], ot[:64, :], reads=[W["otb"][oi]])
            if kind[1] == "ki":
                for sub in range(nsub):
                    gi = W["cnt"]["g"] % 2
                    W["cnt"]["g"] += 1
                    pb = cx.banks[gi]
                    for kc in range(cfg.DC):
                        P.op("pe", lambda t, pb=pb, kc=kc, sub=sub, j=j, wbv=wbv: t.matmul(
                            pb[:, :cfg.NIH], lhsT=W["hT"][:, kc, sub * 128:(sub + 1) * 128], rhs=wbv[:, j, kc, 64:64 + cfg.NIH],
                            start=(kc == 0), stop=(kc == cfg.DC - 1)), reads=[W["wbb"][wi], allh[sub // 2]], writes=[cx.bankb[gi]])
                    oi = W["cnt"]["wis"] % 2
                    W["cnt"]["wis"] += 1
                    ws = W["wis"][oi]
                    P.op("dve", lambda t, ws=ws, pb=pb: t.tensor_copy(out=ws[:, :cfg.NIH], in_=pb[:, :cfg.NIH]), reads=[cx.bankb[gi]], writes=[W["wisb"][oi]])
                    P.dma("sp", outs["wi"][tok0 + sub * 128: tok0 + (sub + 1) * 128, :], ws[:, :cfg.NIH], reads=[W["wisb"][oi]])
            j += 1


def common_consts(cx):
    es = cx.es
    cx.epsb = cx.sb(es, "epsb", [128, 1], F32)
    cx.epsbb = Buf()
    cx.P.op("dve", lambda t: t.memset(cx.epsb[:], EPS), writes=[cx.epsbb])


def declare_dense_inputs(cx, pre, with_ffn=True, with_proj=True):
    cfg = cx.cfg
    d = {}
    if with_ffn:
        d["g1"] = cx.din(pre + "g1", [128, cfg.DC], F32)
        d["w1t"] = cx.din(pre + "w1t", [cfg.FC, 128, cfg.DC * 256], F32)
        d["w2t"] = cx.din(pre + "w2t", [cfg.DC, 128, cfg.FC * 128], F32)
    if with_proj:
        d["gm"] = cx.din(pre + "gm", [128, cfg.DC], F32)
        d["wpt"] = cx.din(pre + "wpt", [cfg.NCC, 128, cfg.DC * 128], F32)
        d["hg"] = cx.din(pre + "hg", [128, 4], F32)
    return d


def declare_proj_outs(cx):
    cfg = cx.cfg
    o = {}
    o["qa"] = cx.dout("o_qa", [cfg.HA, 128, cfg.T], BF16)
    o["ka"] = cx.dout("o_ka", [cfg.HA, 128, cfg.T], BF16)
    o["va"] = cx.dout("o_va", [cfg.T, cfg.WA], BF16)
    o["qb"] = cx.dout("o_qb", [cfg.HB, 128, cfg.T], BF16)
    o["kb"] = cx.dout("o_kb", [cfg.HB, 128, cfg.T], BF16)
    o["vb"] = cx.dout("o_vb", [cfg.T, cfg.WB], BF16)
    o["qi"] = cx.dout("o_qi", [cfg.NQI, 128, cfg.T], BF16)
    o["ki"] = cx.dout("o_ki", [64, cfg.T], BF16)
    o["wi"] = cx.dout("o_wi", [cfg.T, cfg.NIH], F32)
    return o


def head_gains(cx, es, hg_ap):
    t, b = load_vec(cx, es, "hgs", hg_ap, 4)
    sc = 128.0 ** -0.5
    for col in (0, 2):
        cx.P.op("dve", lambda tt, col=col: tt.tensor_scalar(out=t[:, col:col + 1], in0=t[:, col:col + 1], scalar1=sc, scalar2=None, op0=ALU.mult),
                reads=[b], writes=[b])
    return t, b


def build_L1(cfg):
    cx = Ctx(cfg)
    common_consts(cx)
    x_in = cx.din("xT", [cfg.D, cfg.T], F32)
    di = declare_dense_inputs(cx, "")
    x_out = cx.dout("xT_out", [cfg.D, cfg.T], F32)
    outs = declare_proj_outs(cx)
    with ExitStack() as es2:
        W = alloc_dense(cx, es2)
        g1 = load_vec(cx, es2, "g1s", di["g1"], cfg.DC)
        gm = load_vec(cx, es2, "gms", di["gm"], cfg.DC)
        hg = head_gains(cx, es2, di["hg"])
        for g in range(cfg.T // cfg.G):
            tok0 = g * cfg.G
            norm_phase(cx, W, "xin", x_in, g1[0], g1[1], tok0)
            inproj_phase(cx, W, di["w1t"])
            linres_phase(cx, W, di["w2t"], cfg.FC, "xin", x_in, "xout", x_out, tok0, 0.5)
            norm_phase(cx, W, "xout", x_out, gm[0], gm[1], tok0)
            proj_phase(cx, W, di["wpt"], hg, outs, tok0)
        cx.P.finish()
    return cx


def tile_w1(w, cfg):
    a = w.reshape(cfg.DC, 128, 2, cfg.FC, 128).transpose(3, 1, 0, 2, 4)
    return np.ascontiguousarray(a).reshape(cfg.FC, 128, cfg.DC * 256)


def tile_w2(w, nch, cfg):
    a = w.reshape(nch, 128, cfg.DC, 128).transpose(2, 1, 0, 3)
    return np.ascontiguousarray(a).reshape(cfg.DC, 128, nch * 128)


def tile_wp(w, cfg):
    wpad = np.zeros((cfg.D, cfg.NCC * 128), np.float32)
    wpad[:, :cfg.PW] = w
    a = wpad.reshape(cfg.DC, 128, cfg.NCC, 128).transpose(2, 1, 0, 3)
    return np.ascontiguousarray(a).reshape(cfg.NCC, 128, cfg.DC * 128)


def vec_pc(v, cfg):
    return np.ascontiguousarray(v.reshape(cfg.DC, 128).T)


NBIS = 20
PEN = -30000.0


class DB:
    pass


class CtxA(Ctx):
    def __init__(self, cfg):
        self.cfg = cfg
        nc = bass.Bass("TRN2", target_bir_lowering=False)
        nc.dge_precook = False
        self.nc = nc
        self.es = ExitStack()
        self.P = Prog(nc, self.es)
        self.dbank = [self.es.enter_context(nc.psum_tensor("dbank%d" % i, [128, 1024], F32)) for i in range(4)]
        self.dbb = [Buf() for _ in range(4)]
        self.hb = [[Buf(), Buf()] for _ in range(4)]
        self.ones = self.es.enter_context(nc.sbuf_tensor("ones", [128, 128], BF16))
        self.onesb = Buf()
        self.P.op("dve", lambda t: t.memset(self.ones[:], 1.0), writes=[self.onesb])
        self.xbufs = {}


def dsa_phase(cx, es, I, out_mb):
    cfg, P = cx.cfg, cx.P
    HB, NIH, CPB = cfg.HB, cfg.NIH, cfg.CPB
    KT = 128 * CPB
    NQB = cfg.S // KT
    NN = 12 + CPB
    HW = HB * 128
    sb = lambda n, s, d: cx.sb(es, n, s, d)
    scores = sb("scores", [128, cfg.S], F32)
    scb = Buf()
    maskb = sb("maskb", [128, cfg.S], BF16)
    mkb = Buf()
    ki2 = sb("ki2", [128, cfg.S], BF16)
    ki2b = Buf()
    P.dma("sp", ki2[:], I["ki2"], writes=[ki2b])
    ident = sb("ident", [128, 128], BF16)
    identb = Buf()
    P.dma("sp", ident[:], I["ident"], writes=[identb])
    pen = sb("pen", [128, KT], F32)
    penb = Buf()
    qrel = sb("qrel", [128, 1], F32)
    P.dma("sp", qrel[:], I["qrel"], writes=[penb])
    P.dma("sp", pen[:], I["iota"], writes=[penb])
    P.op("dve", lambda t: t.tensor_scalar(out=pen[:], in0=pen[:], scalar1=qrel[:, 0:1], scalar2=PEN, op0=ALU.is_gt, op1=ALU.mult), reads=[penb], writes=[penb])
    cbt = sb("cbt", [128, HW], F32)
    cbb = Buf()
    P.dma("sp", cbt[:], I["cb"], writes=[cbb])
    qbt = [sb("qbt%d" % i, [128, HW], BF16) for i in range(2)]
    qbtb = [Buf(), Buf()]
    qit = [sb("qit%d" % i, [128, cfg.NQI * 128], BF16) for i in range(2)]
    qitb = [Buf(), Buf()]
    wit = [sb("wit%d" % i, [128, 3 * NIH], F32) for i in range(2)]
    witb = [Buf(), Buf()]
    R = [sb("R%d" % i, [128, KT], F32) for i in range(4)]
    Rb = [Buf() for _ in range(4)]
    sm = sb("sm", [128, 8], F32)
    smb = Buf()
    sma = sb("sma", [128, 2], F32)
    smab = Buf()
    midb = Buf()
    mkb2 = Buf()
    Dg = [sb("Dg%d" % i, [128, NIH * 128], F32R) for i in range(2)]
    Dgb = [Buf(), Buf()]
    identf = sb("identf", [128, 128], F32)
    identfb = Buf()
    P.dma("sp", identf[:], I["identf"], writes=[identfb])
    kt_ = [sb("kt%d" % i, [128, HW], BF16) for i in range(2)]
    ktb = [Buf(), Buf()]
    vt_ = [sb("vt%d" % i, [128, HW], BF16) for i in range(2)]
    vtb = [Buf(), Buf()]
    E = [sb("E%d" % i, [128, HW], F32) for i in range(2)]
    Eb = [Buf(), Buf()]
    PT = [sb("PT%d" % i, [128, HW], BF16) for i in range(2)]
    PTb = [Buf(), Buf()]
    Ehb = [[Buf(), Buf()] for _ in range(2)]
    PThb = [[Buf(), Buf()] for _ in range(2)]
    tbt = [sb("tbt%d" % i, [128, HW], F32) for i in range(2)]
    tbb = [Buf(), Buf()]
    ob = sb("ob", [128, HW], BF16)
    obb = Buf()
    rl = sb("rl", [128, HW], F32)
    rlb = Buf()
    cnt = {"R": 0, "kv": 0, "E": 0, "tb": 0, "ps": 0, "pt": 0, "acc": 0}
    for m in range(NQB):
        qi_ = m % 2
        P.dma("sp", qbt[qi_][:], I["qb"][m], writes=[qbtb[qi_]])
        P.dma("sp", qit[qi_][:], I["qi"][m], writes=[qitb[qi_]])
        wt = wit[qi_]
        P.dma("sp", wt[:, :NIH], I["wi"][m], writes=[witb[qi_]])
        P.op("dve", lambda t, wt=wt: t.scalar_tensor_tensor(out=wt[:, NIH:2 * NIH], in0=wt[:, :NIH], scalar=-1.0, in1=wt[:, :NIH], op0=ALU.mult, op1=ALU.max), reads=[witb[qi_]], writes=[witb[qi_]])
        P.op("dve", lambda t, wt=wt: t.tensor_scalar(out=wt[:, 2 * NIH:3 * NIH], in0=wt[:, :NIH], scalar1=0.0, scalar2=2.0, op0=ALU.is_ge, op1=ALU.mult), reads=[witb[qi_]], writes=[witb[qi_]])
        P.op("dve", lambda t, wt=wt: t.tensor_scalar(out=wt[:, 2 * NIH:3 * NIH], in0=wt[:, 2 * NIH:3 * NIH], scalar1=-1.0, scalar2=None, op0=ALU.add), reads=[witb[qi_]], writes=[witb[qi_]])
        L = KT * (m + 1)
        for kt in range(m + 1):
            k0 = kt * KT
            for h in range(NIH):
                bi = cnt["ps"] % 4
                cnt["ps"] += 1
                pb = cx.dbank[bi // 2][:, (bi % 2) * 512:(bi % 2) * 512 + KT]
                pbb = cx.hb[bi // 2][bi % 2]
                p0 = (h % 2) * 64
                P.op("pe", lambda t, pb=pb, h=h, p0=p0, k0=k0, qi_=qi_: t.matmul(pb, lhsT=qit[qi_][p0:p0 + 64, (h // 2) * 128:(h // 2) * 128 + 128], rhs=ki2[p0:p0 + 64, k0:k0 + KT],
                                                                              start=True, stop=True), reads=[qitb[qi_], ki2b], writes=[pbb, cx.dbb[bi // 2]])
                ri = cnt["R"] % 4
                cnt["R"] += 1
                Rt = R[ri]
                P.op("act", lambda t, Rt=Rt, pb=pb, wt=wt, h=h: t.activation(out=Rt[:], in_=pb, func=AF.Relu, scale=wt[:, NIH + h:NIH + h + 1]),
                     reads=[pbb, witb[qi_]], writes=[Rb[ri]])
                sc = scores[:, k0:k0 + KT]
                if h == 0:
                    P.op("dve", lambda t, Rt=Rt, sc=sc, wt=wt, h=h: t.tensor_scalar(out=sc, in0=Rt[:], scalar1=wt[:, 2 * NIH + h:2 * NIH + h + 1], scalar2=None, op0=ALU.mult),
                         reads=[Rb[ri], witb[qi_]], writes=[scb])
                else:
                    P.op("dve", lambda t, Rt=Rt, sc=sc, wt=wt, h=h: t.scalar_tensor_tensor(out=sc, in0=Rt[:], scalar=wt[:, 2 * NIH + h:2 * NIH + h + 1], in1=sc, op0=ALU.mult, op1=ALU.add),
                         reads=[Rb[ri], witb[qi_], scb], writes=[scb])
        P.op("dve", lambda t, L=L: t.tensor_reduce(out=sm[:, 0:1], in_=scores[:, :L], axis=mybir.AxisListType.X, op=ALU.min), reads=[scb], writes=[smb])
        P.op("dve", lambda t, L=L: t.tensor_tensor(out=scores[:, L - KT:L], in0=scores[:, L - KT:L], in1=pen[:], op=ALU.add), reads=[scb, penb], writes=[scb])
        P.op("dve", lambda t, L=L: t.tensor_reduce(out=sm[:, 5:6], in_=scores[:, :L], axis=mybir.AxisListType.X, op=ALU.max), reads=[scb, smb], writes=[smb])
        P.op("dve", lambda t: t.scalar_tensor_tensor(out=sm[:, 1:2], in0=sm[:, 5:6], scalar=1e-6, in1=sm[:, 0:1], op0=ALU.add, op1=ALU.subtract), reads=[smb], writes=[smb])
        La = max(64, (int(L * 0.42) // 64) * 64)
        nact = L - La
        for it in range(NBIS):
            f = 2.0 ** -(it + 1)
            P.op("dve", lambda t, f=f: t.scalar_tensor_tensor(out=sm[:, 2:3], in0=sm[:, 1:2], scalar=f, in1=sm[:, 0:1], op0=ALU.mult, op1=ALU.add), reads=[smb], writes=[smb, midb])
            P.op("act", lambda t, L=L, La=La: t.activation(out=maskb[:, La:L], in_=scores[:, La:L], func=AF.Sign, scale=-1.0, bias=sm[:, 2:3], accum_out=sma[:, 0:1]),
                 reads=[scb, midb], writes=[mkb2, smab])
            P.op("dve", lambda t, La=La: t.tensor_scalar(out=maskb[:, :La], in0=scores[:, :La], scalar1=sm[:, 2:3], scalar2=None, op0=ALU.is_ge, op1=ALU.add, accum_out=sm[:, 3:4]),
                 reads=[scb, smb], writes=[mkb, smb])
            P.op("dve", lambda t: t.scalar_tensor_tensor(out=sm[:, 3:4], in0=sma[:, 0:1], scalar=-0.5, in1=sm[:, 3:4], op0=ALU.mult, op1=ALU.add), reads=[smb, smab], writes=[smb])
            P.op("dve", lambda t, f=f, nact=nact: t.tensor_scalar(out=sm[:, 4:5], in0=sm[:, 3:4], scalar1=255.5 - 0.5 * nact, scalar2=f, op0=ALU.is_ge, op1=ALU.mult), reads=[smb], writes=[smb])
            P.op("dve", lambda t: t.scalar_tensor_tensor(out=sm[:, 0:1], in0=sm[:, 4:5], scalar=sm[:, 1:2], in1=sm[:, 0:1], op0=ALU.mult, op1=ALU.add), reads=[smb], writes=[smb])
        P.op("dve", lambda t, L=L: t.tensor_scalar(out=maskb[:, :L], in0=scores[:, :L], scalar1=sm[:, 0:1], scalar2=None, op0=ALU.is_ge), reads=[scb, smb], writes=[mkb, mkb2])
        nkb = CPB * (m + 1)
        oacc, lacc, sbk, mtb = cx.dbank[2], cx.dbank[3], cx.dbank[0], cx.dbank[1]
        mt16 = mtb[:].bitcast(BF16)
        for kb in range(nkb):
            ki_ = cnt["kv"] % 2
            cnt["kv"] += 1
            P.dma("sp", kt_[ki_][:], I["kb"][kb], writes=[ktb[ki_]])
            P.dma("sp", vt_[ki_][:], I["vb"][kb], writes=[vtb[ki_]])
            mi = kb % 2
            mts = mt16[:, mi * 1024:mi * 1024 + 128]
            P.op("pe", lambda t, mts=mts, kb=kb: t.transpose(out=mts, in_=maskb[:, kb * 128:(kb + 1) * 128], identity=ident[:]),
                 reads=[mkb, mkb2, identb], writes=[cx.hb[1][mi]])
            ei = cnt["E"] % 2
            cnt["E"] += 1
            Et, PTt = E[ei], PT[ei]
            delta = (nkb - 1) - kb
            tb = None
            if delta < NN:
                ti = cnt["tb"] % 2
                cnt["tb"] += 1
                tb = tbt[ti]
                P.dma("sp", tb[:], I["tb"][delta], writes=[tbb[ti]])
                P.op("dve", lambda t, tb=tb: t.tensor_tensor(out=tb[:], in0=tb[:], in1=cbt[:], op=ALU.subtract), reads=[tbb[ti], cbb], writes=[tbb[ti]])
                P.op("act", lambda t, tb=tb: t.activation(out=tb[:], in_=tb[:], func=AF.Exp), reads=[tbb[ti]], writes=[tbb[ti]])
            for hh in range((HB + 3) // 4):
                nh = min(4, HB - 4 * hh)
                c0, c1 = hh * 512, hh * 512 + nh * 128
                for h in range(4 * hh, 4 * hh + nh):
                    P.op("pe", lambda t, h=h, ki_=ki_, qi_=qi_: t.matmul(sbk[:, h * 128:(h + 1) * 128], lhsT=kt_[ki_][:, h * 128:(h + 1) * 128], rhs=qbt[qi_][:, h * 128:(h + 1) * 128],
                                                                      start=True, stop=True), reads=[ktb[ki_], qbtb[qi_]], writes=[cx.hb[0][hh]])
                P.op("act", lambda t, Et=Et, c0=c0, c1=c1: t.activation(out=Et[:, c0:c1], in_=sbk[:, c0:c1], func=AF.Exp), reads=[cx.hb[0][hh]], writes=[Ehb[ei][hh]])
                if tb is not None:
                    P.op("dve", lambda t, tb=tb, Et=Et, c0=c0, c1=c1: t.tensor_tensor(out=Et[:, c0:c1], in0=Et[:, c0:c1], in1=tb[:, c0:c1], op=ALU.mult),
                         reads=[tbb[ti], Ehb[ei][hh]], writes=[Ehb[ei][hh]])
                mtbc = mts.unsqueeze(1).to_broadcast([128, nh, 128])
                P.op("dve", lambda t, Et=Et, PTt=PTt, mtbc=mtbc, c0=c0, c1=c1, nh=nh: t.tensor_tensor(out=PTt[:, c0:c1].rearrange("p (h t) -> p h t", h=nh), in0=Et[:, c0:c1].rearrange("p (h t) -> p h t", h=nh), in1=mtbc, op=ALU.mult),
                     reads=[Ehb[ei][hh], cx.hb[1][mi]], writes=[PThb[ei][hh]])
                for h in range(4 * hh, 4 * hh + nh):
                    P.op("pe", lambda t, h=h, ki_=ki_, PTt=PTt, kb=kb, nkb=nkb: t.matmul(oacc[:, h * 128:(h + 1) * 128], lhsT=vt_[ki_][:, h * 128:(h + 1) * 128], rhs=PTt[:, h * 128:(h + 1) * 128],
                                                                                      start=(kb == 0 and h % 4 == 0), stop=(kb == nkb - 1), skip_group_check=True), reads=[vtb[ki_], PThb[ei][hh]], writes=[cx.dbb[2]])
                P.op("pe", lambda t, PTt=PTt, kb=kb, c0=c0, c1=c1, nkb=nkb: t.matmul(lacc[:, c0:c1], lhsT=cx.ones[:], rhs=PTt[:, c0:c1], start=(kb == 0), stop=(kb == nkb - 1)),
                     reads=[cx.onesb, PThb[ei][hh]], writes=[cx.dbb[3]])
        P.op("dve", lambda t: t.reciprocal(out=rl[:], in_=lacc[:, :HW]), reads=[cx.dbb[3]], writes=[rlb])
        P.op("dve", lambda t: t.tensor_tensor(out=ob[:], in0=oacc[:, :HW], in1=rl[:], op=ALU.mult), reads=[cx.dbb[2], rlb], writes=[obb])
        P.dma("sp", out_mb[:, :, m * 128:(m + 1) * 128], ob[:].rearrange("p (h t) -> p h t", h=HB), reads=[obb])


def dil_phase(cx, es, I, out_ma):
    cfg, P = cx.cfg, cx.P
    HA = cfg.HA
    HW = HA * 128
    sb = lambda n, s, d: cx.sb(es, n, s, d)
    npf = sb("npf", [128, 1], F32)
    npfb = Buf()
    P.dma("sp", npf[:], I["npf"], writes=[npfb])
    EBA = [[sb("eba%d_%d" % (br, v), [128, HW], F32) for v in range(3)] for br in range(3)]
    ebab = Buf()
    tmpv = sb("aE0", [128, HW], F32)
    for br in range(3):
        for pc in range(2):
            e_ = EBA[br][pc]
            P.dma("sp", e_[:], I["tb"][br, pc], writes=[ebab])
            P.dma("sp", tmpv[:], I["vm"][br, pc], reads=[ebab], writes=[ebab])
            P.op("act", lambda t, e_=e_: t.activation(out=e_[:], in_=e_[:], func=AF.Exp), reads=[ebab], writes=[ebab])
            P.op("dve", lambda t, e_=e_: t.tensor_tensor(out=e_[:], in0=e_[:], in1=tmpv[:], op=ALU.mult), reads=[ebab], writes=[ebab])
        P.op("dve", lambda t, br=br: t.tensor_scalar(out=EBA[br][2][:], in0=EBA[br][0][:], scalar1=npf[:, 0:1], scalar2=None, op0=ALU.mult), reads=[ebab, npfb], writes=[ebab])
    oaccT = sb("oaccT", [128, HA, 2048], F32)
    laccT = sb("laccT", [128, HA, 2048], F32)
    accb = Buf()
    qt = [sb("aq%d" % i, [128, HW], BF16) for i in range(2)]
    kt = [sb("ak%d" % i, [128, 2, HW], BF16) for i in range(2)]
    vt = [sb("av%d" % i, [128, 2, HW], BF16) for i in range(2)]
    qkvb = [Buf(), Buf()]
    E0 = tmpv
    E = [E0, E0]
    Eb0 = ebab
    Eb = [Eb0, Eb0]
    PT = [sb("aPT%d" % i, [128, HW], BF16) for i in range(2)]
    PTb = [Buf(), Buf()]
    ob = [sb("aob%d" % i, [128, HA, 256], BF16) for i in range(2)]
    obb = [Buf(), Buf()]
    n = 0
    ne = 0
    for u in range(2):
        for br, dil in enumerate((1, 4, 16)):
            span = 128 * dil
            for ti in range(16):
                np_, r = ti // dil, ti % dil
                bi = n % 2
                n += 1
                P.dma("sp", qt[bi][:], I["q"][br, u, ti], writes=[qkvb[bi]])
                P.dma("sp", kt[bi][:], I["k"][br, u, ti].rearrange("c p x -> p c x"), writes=[qkvb[bi]])
                P.dma("sp", vt[bi][:], I["v"][br, u, ti].rearrange("c p x -> p c x"), writes=[qkvb[bi]])
                sbk = cx.dbank[bi]
                oacc, lacc = cx.dbank[2], cx.dbank[3]
                for pc in range(2):
                    for h in range(HA):
                        P.op("pe", lambda t, h=h, bi=bi, pc=pc, sbk=sbk: t.matmul(sbk[:, h * 128:(h + 1) * 128], lhsT=kt[bi][:, pc, h * 128:(h + 1) * 128], rhs=qt[bi][:, h * 128:(h + 1) * 128],
                                                                              start=True, stop=True), reads=[qkvb[bi]], writes=[cx.dbb[bi]])
                    ei = ne % 2
                    ne += 1
                    Et, PTt = E[ei], PT[ei]
                    P.op("act", lambda t, Et=Et, sbk=sbk: t.activation(out=Et[:], in_=sbk[:, :HW], func=AF.Exp), reads=[cx.dbb[bi]], writes=[Eb[ei]])
                    ev = EBA[br][2] if (pc == 0 and u == 0 and np_ == 0) else EBA[br][pc]
                    P.op("dve", lambda t, Et=Et, PTt=PTt, ev=ev: t.tensor_tensor(out=PTt[:], in0=Et[:], in1=ev[:], op=ALU.mult), reads=[Eb[ei], ebab], writes=[PTb[ei]])
                    for h in range(HA):
                        P.op("pe", lambda t, h=h, bi=bi, pc=pc, PTt=PTt: t.matmul(oacc[:, h * 128:(h + 1) * 128], lhsT=vt[bi][:, pc, h * 128:(h + 1) * 128], rhs=PTt[:, h * 128:(h + 1) * 128],
                                                                              start=(pc == 0 and h % 4 == 0), stop=(pc == 1), skip_group_check=True), reads=[qkvb[bi], PTb[ei]], writes=[cx.dbb[2]])
                    for c0 in range(0, HW, 512):
                        c1 = min(HW, c0 + 512)
                        P.op("pe", lambda t, PTt=PTt, pc=pc, c0=c0, c1=c1: t.matmul(lacc[:, c0:c1], lhsT=cx.ones[:], rhs=PTt[:, c0:c1], start=(pc == 0), stop=(pc == 1)),
                             reads=[cx.onesb, PTb[ei]], writes=[cx.dbb[3]])
                s0 = np_ * span + r
                dst_o = oaccT[:, :, s0:s0 + 127 * dil + 1:dil]
                dst_l = laccT[:, :, s0:s0 + 127 * dil + 1:dil]
                ov = oacc[:, :HW].rearrange("p (h t) -> p h t", h=HA)
                lv = lacc[:, :HW].rearrange("p (h t) -> p h t", h=HA)
                if br == 0:
                    P.op("dve", lambda t, dst_o=dst_o, ov=ov: t.tensor_copy(out=dst_o, in_=ov), reads=[cx.dbb[2]], writes=[accb])
                    P.op("act", lambda t, dst_l=dst_l, lv=lv: t.activation(out=dst_l, in_=lv, func=AF.Copy), reads=[cx.dbb[3]], writes=[accb])
                else:
                    P.op("dve", lambda t, dst_o=dst_o, ov=ov: t.tensor_tensor(out=dst_o, in0=dst_o, in1=ov, op=ALU.add), reads=[cx.dbb[2], accb], writes=[accb])
                    P.op("dve", lambda t, dst_l=dst_l, lv=lv: t.tensor_tensor(out=dst_l, in0=dst_l, in1=lv, op=ALU.add), reads=[cx.dbb[3], accb], writes=[accb])
        for c in range(8):
            sl = slice(c * 256, (c + 1) * 256)
            P.op("dve", lambda t, sl=sl: t.reciprocal(out=laccT[:, :, sl], in_=laccT[:, :, sl]), reads=[accb], writes=[accb])
            oi = c % 2
            P.op("dve", lambda t, sl=sl, oi=oi: t.tensor_tensor(out=ob[oi][:], in0=oaccT[:, :, sl], in1=laccT[:, :, sl], op=ALU.mult), reads=[accb], writes=[obb[oi]])
            P.dma("sp", out_ma[:, :, u * 2048 + c * 256: u * 2048 + (c + 1) * 256], ob[oi][:], reads=[obb[oi]])


def build_L2(cfg):
    cx = CtxA(cfg)
    HA, HB, KT = cfg.HA, cfg.HB, 128 * cfg.CPB
    NQB = cfg.S // KT
    NKB = cfg.S // 128
    A = {"npf": cx.din("a_npf", [128, 1], F32), "tb": cx.din("a_tb", [3, 2, 128, HA * 128], F32), "vm": cx.din("a_vm", [3, 2, 128, HA * 128], F32),
         "q": cx.din("a_q", [3, 2, 16, 128, HA * 128], BF16), "k": cx.din("a_k", [3, 2, 16, 2, 128, HA * 128], BF16),
         "v": cx.din("a_v", [3, 2, 16, 2, 128, HA * 128], BF16)}
    Bd = {"ki2": cx.din("b_ki2", [128, cfg.S], BF16), "ident": cx.din("b_ident", [128, 128], BF16), "identf": cx.din("b_identf", [128, 128], F32), "qrel": cx.din("b_qrel", [128, 1], F32),
          "iota": cx.din("b_iota", [128, KT], F32), "cb": cx.din("b_cb", [128, HB * 128], F32), "qb": cx.din("b_qb", [NQB, 128, HB * 128], BF16),
          "qi": cx.din("b_qi", [NQB, 128, cfg.NQI * 128], BF16), "wi": cx.din("b_wi", [NQB, 128, cfg.NIH], F32),
          "kb": cx.din("b_kb", [NKB, 128, HB * 128], BF16), "vb": cx.din("b_vb", [NKB, 128, HB * 128], BF16),
          "tb": cx.din("b_tb", [12 + cfg.CPB, 128, HB * 128], F32)}
    o_ma = cx.dout("o_ma", [128, HA, cfg.T], BF16)
    o_mb = cx.dout("o_mb", [128, HB, NQB * 128], BF16)
    with ExitStack() as es2:
        dil_phase(cx, es2, A, o_ma)
        cx.P.barrier()
    with ExitStack() as es3:
        dsa_phase(cx, es3, Bd, o_mb)
        cx.P.finish()
    return cx


def rel_bucket_np(dist):
    dist = np.asarray(dist, np.int64)
    df = np.maximum(dist, 1).astype(np.float32)
    large = 16 + (np.log(df / np.float32(16)) / np.float32(np.log(2048 / 16)) * np.float32(16)).astype(np.int32)
    large = np.minimum(large, 31)
    return np.where(dist < 16, np.maximum(dist, 0), large).astype(np.int64)


def core_tokens(cfg, c):
    r = c % cfg.CPB
    u0, u1 = r, cfg.NU - 1 - r
    return np.concatenate([np.arange(2048) + 2048 * u0, np.arange(2048) + 2048 * u1])


def gather_global(cfg, outs_per_core):
    G = []
    for b in range(cfg.B):
        g = {"qa": np.zeros((cfg.HA, 128, cfg.S), NPBF), "ka": np.zeros((cfg.HA, 128, cfg.S), NPBF), "va": np.zeros((cfg.S, cfg.WA), NPBF),
             "qb": np.zeros((cfg.HB, 128, cfg.S), NPBF), "kb": np.zeros((cfg.HB, 128, cfg.S), NPBF), "vb": np.zeros((cfg.S, cfg.WB), NPBF),
             "qi": np.zeros((cfg.NQI, 128, cfg.S), NPBF), "ki": np.zeros((64, cfg.S), NPBF), "wi": np.zeros((cfg.S, cfg.NIH), np.float32)}
        for r in range(cfg.CPB):
            c = b * cfg.CPB + r
            pos = core_tokens(cfg, c)
            o = outs_per_core[c]
            for k in ("qa", "ka", "qb", "kb", "qi", "ki"):
                g[k][..., pos] = o["o_" + k]
            for k in ("va", "vb", "wi"):
                g[k][pos] = o["o_" + k]
        G.append(g)
    return G


def l2_inputs(cfg, G, rel_bias, c):
    b, r = c // cfg.CPB, c % cfg.CPB
    g = G[b]
    HA, HB, CPB = cfg.HA, cfg.HB, cfg.CPB
    rel_bias = np.asarray(rel_bias, np.float32)
    ins = {}
    units = (r, cfg.NU - 1 - r)
    aq = np.zeros((3, 2, 16, 128, HA * 128), NPBF)
    ak = np.zeros((3, 2, 16, 2, 128, HA * 128), NPBF)
    av = np.zeros((3, 2, 16, 2, 128, HA * 128), NPBF)
    atb = np.zeros((3, 2, 128, HA, 128), np.float32)
    avm = np.zeros((3, 2, 128, HA, 128), np.float32)
    jj, ii = np.meshgrid(np.arange(128), np.arange(128), indexing="ij")
    for br, dil in enumerate((1, 4, 16)):
        span = 128 * dil
        for pc in range(2):
            step = ii - jj + (128 if pc == 0 else 0)
            valid = (step >= 0) & (step <= 128) & ((jj >= ii) if pc == 0 else (jj <= ii))
            bk = rel_bucket_np(np.clip(step, 0, 128) * dil)
            atb[br, pc] = np.where(valid[:, None, :], rel_bias[bk][:, :, :HA].transpose(0, 2, 1), 0.0)
            avm[br, pc] = valid[:, None, :].astype(np.float32)
        for u, gu in enumerate(units):
            for ti in range(16):
                np_, r_ = ti // dil, ti % dil
                p = 2048 * gu + np_ * span + r_ + dil * np.arange(128)
                aq[br, u, ti] = g["qa"][:, :, p].transpose(1, 0, 2).reshape(128, HA * 128)
                ak[br, u, ti, 1] = g["ka"][:, :, p].transpose(1, 0, 2).reshape(128, HA * 128)
                av[br, u, ti, 1] = g["va"][p]
                pp = p - span
                if pp[0] >= 0:
                    ak[br, u, ti, 0] = g["ka"][:, :, pp].transpose(1, 0, 2).reshape(128, HA * 128)
                    av[br, u, ti, 0] = g["va"][pp]
    ins.update({"a_q": aq, "a_k": ak, "a_v": av, "a_tb": atb.reshape(3, 2, 128, HA * 128), "a_vm": avm.reshape(3, 2, 128, HA * 128),
                "a_npf": np.full((128, 1), 0.0 if r == 0 else 1.0, np.float32)})
    KT = 128 * CPB
    NQB = cfg.S // KT
    NKB = cfg.S // 128
    gq = CPB * np.arange(NQB) + r
    qb = g["qb"].reshape(HB, 128, NKB, 128)[:, :, gq]
    ins["b_qb"] = np.ascontiguousarray(qb.transpose(2, 1, 0, 3)).reshape(NQB, 128, HB * 128)
    qi = g["qi"].reshape(cfg.NQI, 128, NKB, 128)[:, :, gq]
    ins["b_qi"] = np.ascontiguousarray(qi.transpose(2, 1, 0, 3)).reshape(NQB, 128, cfg.NQI * 128)
    ins["b_wi"] = np.ascontiguousarray(g["wi"].reshape(NKB, 128, cfg.NIH)[gq])
    ins["b_ki2"] = np.ascontiguousarray(np.concatenate([g["ki"], g["ki"]], 0))
    ins["b_kb"] = np.ascontiguousarray(g["kb"].reshape(HB, 128, NKB, 128).transpose(2, 1, 0, 3)).reshape(NKB, 128, HB * 128)
    ins["b_vb"] = np.ascontiguousarray(g["vb"].reshape(NKB, 128, HB * 128))
    ins["b_qrel"] = (r * 128 + np.arange(128, dtype=np.float32)).reshape(128, 1)
    ins["b_iota"] = np.ascontiguousarray(np.broadcast_to(np.arange(KT, dtype=np.float32), (128, KT)))
    ins["b_cb"] = np.ascontiguousarray(np.broadcast_to(rel_bias[31, HA:HA + HB][None, :, None], (128, HB, 128))).reshape(128, HB * 128)
    NN = 12 + CPB
    tb = np.zeros((NN, 128, HB, 128), np.float32)
    for d in range(NN):
        dist = 128 * (d - (CPB - 1 - r)) + ii - jj
        tb[d] = rel_bias[rel_bucket_np(np.maximum(dist, 0))][:, :, HA:HA + HB].transpose(0, 2, 1)
    ins["b_tb"] = tb.reshape(NN, 128, HB * 128)
    ins["b_ident"] = np.eye(128, dtype=np.float32).astype(NPBF)
    ins["b_identf"] = np.eye(128, dtype=np.float32)
    return ins


def scatter_mix(cfg, res_per_core):
    M = []
    KT = 128 * cfg.CPB
    NQB = cfg.S // KT
    for b in range(cfg.B):
        mix = np.zeros((cfg.S, cfg.WA + cfg.WB), NPBF)
        for r in range(cfg.CPB):
            c = b * cfg.CPB + r
            pos = core_tokens(cfg, c)
            ma = res_per_core[c]["o_ma"]
            mix[pos, :cfg.WA] = ma.transpose(2, 1, 0).reshape(cfg.T, cfg.WA)
            mb = res_per_core[c]["o_mb"].reshape(128, cfg.HB, NQB, 128)
            gq = cfg.CPB * np.arange(NQB) + r
            posb = (gq[:, None] * 128 + np.arange(128)[None, :]).reshape(-1)
            mix[posb, cfg.WA:] = mb.transpose(2, 3, 1, 0).reshape(NQB * 128, cfg.WB)
        M.append(mix)
    return M


def build_L3(cfg, with_next=True):
    cx = Ctx(cfg)
    common_consts(cx)
    NM = (cfg.WA + cfg.WB) // 128
    x_in = cx.din("xT", [cfg.D, cfg.T], F32)
    mixT = cx.din("mixT", [NM * 128, cfg.T], BF16)
    wot = cx.din("wot", [cfg.DC, 128, NM * 128], F32)
    f2 = declare_dense_inputs(cx, "f2_", with_proj=False)
    xa = cx.dint("xa", [cfg.D, cfg.T], F32)
    xb = cx.dout("xT_mid", [cfg.D, cfg.T], F32)
    if with_next:
        nx = declare_dense_inputs(cx, "nx_")
        xc = cx.dout("xT_out", [cfg.D, cfg.T], F32)
        outs = declare_proj_outs(cx)
    with ExitStack() as es2:
        W = alloc_dense(cx, es2)
        g2 = load_vec(cx, es2, "g2s", f2["g1"], cfg.DC)
        if with_next:
            g1 = load_vec(cx, es2, "g1s", nx["g1"], cfg.DC)
            gm = load_vec(cx, es2, "gms", nx["gm"], cfg.DC)
            hg = head_gains(cx, es2, nx["hg"])
        for g in range(cfg.T // cfg.G):
            tok0 = g * cfg.G
            load_actT(cx, W, mixT, NM, tok0)
            linres_phase(cx, W, wot, NM, "xin", x_in, "xa", xa, tok0, 1.0)
            norm_phase(cx, W, "xa", xa, g2[0], g2[1], tok0)
            inproj_phase(cx, W, f2["w1t"])
            linres_phase(cx, W, f2["w2t"], cfg.FC, "xa", xa, "xb", xb, tok0, 0.5)
            if not with_next:
                continue
            norm_phase(cx, W, "xb", xb, g1[0], g1[1], tok0)
            inproj_phase(cx, W, nx["w1t"])
            linres_phase(cx, W, nx["w2t"], cfg.FC, "xb", xb, "xc", xc, tok0, 0.5)
            norm_phase(cx, W, "xc", xc, gm[0], gm[1], tok0)
            proj_phase(cx, W, nx["wpt"], hg, outs, tok0)
        cx.P.finish()
    return cx


def _run(cx, in_maps, n):
    res = run_bass_kernel_spmd(cx.nc, in_maps, core_ids=list(range(n)))
    cx.es.close()
    return res.results


def layer_dense_inputs(cfg, inp, l, pre, with_ffn1=True):
    d = {}
    if with_ffn1:
        d[pre + "g1"] = vec_pc(inp["norm_ffn1"][l], cfg)
        d[pre + "w1t"] = tile_w1(inp["w_ffn1_in"][l], cfg)
        d[pre + "w2t"] = tile_w2(inp["w_ffn1_out"][l], cfg.FC, cfg)
    d[pre + "gm"] = vec_pc(inp["norm_mix"][l], cfg)
    d[pre + "wpt"] = tile_wp(inp["w_in"][l], cfg)
    d[pre + "hg"] = np.ascontiguousarray(np.stack([inp["q_norm_a"][l], inp["k_norm_a"][l], inp["q_norm_b"][l], inp["k_norm_b"][l]], 1))
    return d


def run_model(cfg, inp):
    inp = {k: np.asarray(v) for k, v in inp.items()}
    n = cfg.NCORES
    x = inp["x"]
    xT = [np.ascontiguousarray(x[c // cfg.CPB][core_tokens(cfg, c)].T) for c in range(n)]
    cx = build_L1(cfg)
    d0 = layer_dense_inputs(cfg, inp, 0, "")
    res = _run(cx, [dict(d0, xT=xT[c]) for c in range(n)], n)
    cxa = build_L2(cfg)
    cx3 = None
    for l in range(cfg.DEPTH):
        G = gather_global(cfg, res)
        resa = _run(cxa, [l2_inputs(cfg, G, inp["rel_bias"], c) for c in range(n)], n)
        if l + 1 < cfg.DEPTH:
            cxa = build_L2(cfg)
        mix = scatter_mix(cfg, resa)
        last = (l + 1 == cfg.DEPTH)
        cx3 = build_L3(cfg, with_next=not last)
        d3 = {} if last else layer_dense_inputs(cfg, inp, l + 1, "nx_")
        d3["wot"] = tile_w2(inp["w_out"][l], (cfg.WA + cfg.WB) // 128, cfg)
        d3["f2_g1"] = vec_pc(inp["norm_ffn2"][l], cfg)
        d3["f2_w1t"] = tile_w1(inp["w_ffn2_in"][l], cfg)
        d3["f2_w2t"] = tile_w2(inp["w_ffn2_out"][l], cfg.FC, cfg)
        ims = []
        for c in range(n):
            xin = res[c]["xT_out"]
            mt = np.ascontiguousarray(mix[c // cfg.CPB][core_tokens(cfg, c)].T)
            ims.append(dict(d3, xT=xin, mixT=mt))
        res = _run(cx3, ims, n)
    out = np.zeros((cfg.B, cfg.S, cfg.D), np.float32)
    for c in range(n):
        out[c // cfg.CPB, core_tokens(cfg, c)] = res[c]["xT_mid"].T
    return out


def kernel(**inputs):
    return run_model(Cfg(), inputs)
```
